# Optimizing a Trainium2 kernel written in Bass

```python
import math
import numpy as np
import jax
import jax.numpy as jnp
from jax import lax

D_MODEL = 1024
BATCH = 4
SEQ = 4096
DEPTH = 2

GRID_W = 64
CTX_LEN = 256
N_EVEN = (DEPTH + 1) // 2
N_ODD = DEPTH // 2
N_MOD = 6
NORM_EPS = 1e-6
ROPE_BASE = 10000.0

MLA_HEADS = 8
MLA_NOPE = 64
MLA_ROPE = 32
MLA_V = 64
Q_LORA = 384
KV_LORA = 256
MLA_Q_BLOCK = 128
MLA_SCALE = (MLA_NOPE + MLA_ROPE) ** -0.5

NA_HEADS = 8
NA_HEAD_DIM = 64
NA_WIN_H = 8
NA_WIN_W = 16
NA_SCALE = NA_HEAD_DIM ** -0.5

EVEN_IN = Q_LORA + KV_LORA + MLA_ROPE + 3 * NA_HEADS * NA_HEAD_DIM
EVEN_OUT = MLA_HEADS * MLA_V + NA_HEADS * NA_HEAD_DIM

S5_WIDTH = D_MODEL
S5_GROUP = 16
S5_GROUPS = S5_WIDTH // S5_GROUP
S5_STATE = 64

D_FF = 2816
N_EXPERTS = 8
TOP_K = 2
D_FF_EXPERT = 3584

kernel_name = 'hybrid_mla_natten_s5_moe_dit'


def rms_norm(x, g):
    xf = x.astype(jnp.float32)
    y = xf * lax.rsqrt(jnp.mean(xf * xf, axis=-1, keepdims=True) + NORM_EPS)
    return (y * g.astype(jnp.float32)).astype(x.dtype)


def modulate(x, g, shift, scale):
    return rms_norm(x, g) * (1 + scale) + shift


def adaln_params(cond, w, b):
    m = (jax.nn.silu(cond) @ w + b)[..., None, :]
    return jnp.split(m, N_MOD, axis=-1)


def softmax_f32(s):
    return jax.nn.softmax(s.astype(jnp.float32), axis=-1)


def dense_attend(s, v):
    p = softmax_f32(s).astype(v.dtype)
    return jnp.einsum('bhqk,bkhd->bqhd', p, v)


def axial_rope(x, row, col):
    half = x.shape[-1] // 2
    quarter = half // 2
    inv_freq = ROPE_BASE ** (-jnp.arange(quarter, dtype=jnp.float32) / quarter)
    bshape = (1, x.shape[1]) + (1,) * (x.ndim - 3) + (quarter,)

    def rotate(xa, pos):
        ang = pos.astype(jnp.float32)[:, None] * inv_freq[None, :]
        cos = jnp.cos(ang).reshape(bshape).astype(x.dtype)
        sin = jnp.sin(ang).reshape(bshape).astype(x.dtype)
        x1, x2 = xa[..., :quarter], xa[..., quarter:]
        return jnp.concatenate([x1 * cos - x2 * sin, x1 * sin + x2 * cos], axis=-1)

    return jnp.concatenate([rotate(x[..., :half], row), rotate(x[..., half:], col)], axis=-1)


def even_project(t, w_in, q_norm_g, w_qb, kv_norm_g, w_kvb):
    b, l, _ = t.shape
    p = t @ w_in
    cq, ckv, kr, qkv = jnp.split(p, [Q_LORA, Q_LORA + KV_LORA, Q_LORA + KV_LORA + MLA_ROPE], axis=-1)
    q = (rms_norm(cq, q_norm_g) @ w_qb).reshape(b, l, MLA_HEADS, MLA_NOPE + MLA_ROPE)
    kv = (rms_norm(ckv, kv_norm_g) @ w_kvb).reshape(b, l, MLA_HEADS, MLA_NOPE + MLA_V)
    qkv = qkv.reshape(b, l, 3, NA_HEADS, NA_HEAD_DIM)
    return (q[..., :MLA_NOPE], q[..., MLA_NOPE:], kv[..., :MLA_NOPE], kr, kv[..., MLA_NOPE:],
            qkv[:, :, 0], qkv[:, :, 1], qkv[:, :, 2])


def mla_scores(qn, qr, kn, kr):
    return (jnp.einsum('bqhd,bkhd->bhqk', qn, kn) + jnp.einsum('bqhr,bkr->bhqk', qr, kr)) * MLA_SCALE


def mla_latent(qn, qr, kn, kr, v, kn_c, kr_c, v_c):
    b, l, h, _ = qn.shape
    nb = l // MLA_Q_BLOCK

    def to_blocks(t):
        return t.reshape((b, nb, MLA_Q_BLOCK) + t.shape[2:]).swapaxes(0, 1)

    def block(qs):
        qn_b, qr_b = qs
        s = jnp.concatenate([mla_scores(qn_b, qr_b, kn, kr), mla_scores(qn_b, qr_b, kn_c, kr_c)], axis=-1)
        p = softmax_f32(s).astype(v.dtype)
        return (jnp.einsum('bhqk,bkhd->bqhd', p[..., :l], v)
                + jnp.einsum('bhqk,bkhd->bqhd', p[..., l:], v_c))

    out = lax.map(block, (to_blocks(qn), to_blocks(qr)))
    return out.swapaxes(0, 1).reshape(b, l, h * MLA_V)


def na_latent(q, k, v, k_c, v_c, rpb):
    b, l, h, d = q.shape
    rows = l // GRID_W
    kh = min(NA_WIN_H, rows)
    r = np.arange(rows)
    row_start = np.clip(r - kh // 2, 0, rows - kh)
    row_off = row_start[:, None] + np.arange(kh)[None, :] - r[:, None] + (NA_WIN_H - 1)
    cidx = np.arange(GRID_W)
    col_start = np.clip(cidx - NA_WIN_W // 2, 0, GRID_W - NA_WIN_W)
    col_idx = col_start[:, None] + np.arange(NA_WIN_W)[None, :]
    col_off = col_idx - cidx[:, None] + (NA_WIN_W - 1)
    bias = rpb[:, row_off][..., col_off].transpose(1, 0, 3, 2, 4)
    n_win = kh * NA_WIN_W
    qg = (q * NA_SCALE).reshape(b, rows, GRID_W, h, d).swapaxes(0, 1)
    kg = k.reshape(b, rows, GRID_W, h, d)
    vg = v.reshape(b, rows, GRID_W, h, d)

    def row_block(xs):
        q_r, start, bias_r = xs
        k_win = lax.dynamic_slice_in_dim(kg, start, kh, axis=1)[:, :, col_idx]
        v_win = lax.dynamic_slice_in_dim(vg, start, kh, axis=1)[:, :, col_idx]
        s_win = jnp.einsum('bqhd,biqjhd->bhqij', q_r, k_win) + bias_r
        s_ctx = jnp.einsum('bqhd,bkhd->bhqk', q_r, k_c)
        s = jnp.concatenate([s_win.reshape(b, h, GRID_W, n_win), s_ctx], axis=-1)
        p = softmax_f32(s).astype(v.dtype)
        p_win = p[..., :n_win].reshape(b, h, GRID_W, kh, NA_WIN_W)
        return (jnp.einsum('bhqij,biqjhd->bqhd', p_win, v_win)
                + jnp.einsum('bhqk,bkhd->bqhd', p[..., n_win:], v_c))

    out = lax.map(row_block, (qg, jnp.asarray(row_start, jnp.int32), bias))
    return out.swapaxes(0, 1).reshape(b, l, h * d)


def even_mixer(h, hc, w_in, q_norm_g, w_qb, kv_norm_g, w_kvb, rpb, w_out, need_ctx):
    b, l, _ = h.shape
    qn, qr, kn, kr, v, nq, nk, nv = even_project(h, w_in, q_norm_g, w_qb, kv_norm_g, w_kvb)
    qn_c, qr_c, kn_c, kr_c, v_c, nq_c, nk_c, nv_c = even_project(hc, w_in, q_norm_g, w_qb, kv_norm_g, w_kvb)
    t = jnp.arange(l)
    row, col = t // GRID_W, t % GRID_W
    qr = axial_rope(qr, row, col)
    kr = axial_rope(kr, row, col)
    mla = mla_latent(qn, qr, kn, kr, v, kn_c, kr_c, v_c)
    na = na_latent(nq, nk, nv, nk_c, nv_c, rpb)
    y = jnp.concatenate([mla, na], axis=-1) @ w_out
    if not need_ctx:
        return y, None
    lc = hc.shape[1]
    mla_c = dense_attend(mla_scores(qn_c, qr_c, kn_c, kr_c), v_c).reshape(b, lc, MLA_HEADS * MLA_V)
    na_c = dense_attend(jnp.einsum('bqhd,bkhd->bhqk', nq_c * NA_SCALE, nk_c), nv_c).reshape(b, lc, NA_HEADS * NA_HEAD_DIM)
    yc = jnp.concatenate([mla_c, na_c], axis=-1) @ w_out
    return y, yc


def s5_discretize(a_re, a_im, log_step, b_re, b_im):
    dt = jnp.exp(log_step)[:, None]
    decay = jnp.exp(a_re * dt)
    ab_re = decay * jnp.cos(a_im * dt)
    ab_im = decay * jnp.sin(a_im * dt)
    den = a_re * a_re + a_im * a_im
    f_re = ((ab_re - 1) * a_re + ab_im * a_im) / den
    f_im = (ab_im * a_re - (ab_re - 1) * a_im) / den
    bb_re = f_re[..., None] * b_re - f_im[..., None] * b_im
    bb_im = f_re[..., None] * b_im + f_im[..., None] * b_re
    return ab_re, ab_im, bb_re, bb_im


def linear_recurrence(e1, e2):
    a1r, a1i, b1r, b1i = e1
    a2r, a2i, b2r, b2i = e2
    return (a2r * a1r - a2i * a1i, a2r * a1i + a2i * a1r,
            a2r * b1r - a2i * b1i + b2r, a2r * b1i + a2i * b1r + b2i)


def ssm_scan(bu_re, bu_im, ab_re, ab_im, h0, reverse):
    if reverse:
        bu_re, bu_im = jnp.flip(bu_re, axis=1), jnp.flip(bu_im, axis=1)
    if h0 is not None:
        h0r, h0i = h0
        bu_re = bu_re.at[:, 0].add(ab_re * h0r - ab_im * h0i)
        bu_im = bu_im.at[:, 0].add(ab_re * h0i + ab_im * h0r)
    l = bu_re.shape[1]
    a_re = jnp.broadcast_to(ab_re, (1, l) + ab_re.shape)
    a_im = jnp.broadcast_to(ab_im, (1, l) + ab_im.shape)
    _, _, hr, hi = lax.associative_scan(linear_recurrence, (a_re, a_im, bu_re, bu_im), axis=1)
    if reverse:
        hr, hi = jnp.flip(hr, axis=1), jnp.flip(hi, axis=1)
    return hr, hi


def glu(t, w_glu):
    z = t @ w_glu
    return z[..., :D_MODEL] * jax.nn.sigmoid(z[..., D_MODEL:])


def s5_mixer(h, hc, w_in, a_re, a_im, log_step, b_re, b_im, c_re, c_im, d_skip, w_glu, need_ctx):
    b, l, _ = h.shape
    lc = hc.shape[1]
    u = h @ w_in
    u_c = hc @ w_in
    ug = u.reshape(b, l, S5_GROUPS, S5_GROUP)
    ug_c = u_c.reshape(b, lc, S5_GROUPS, S5_GROUP)
    y = u * d_skip
    y_c = u_c * d_skip if need_ctx else None
    for direction in range(2):
        reverse = direction == 1
        ab_re, ab_im, bb_re, bb_im = s5_discretize(a_re[direction], a_im[direction], log_step[direction],
                                                   b_re[direction], b_im[direction])
        cr, ci = c_re[direction], c_im[direction]

        def drive(t):
            return (jnp.einsum('blgi,gpi->blgp', t, bb_re), jnp.einsum('blgi,gpi->blgp', t, bb_im))

        def readout(hr, hi):
            out = jnp.einsum('blgp,gip->blgi', hr, cr) - jnp.einsum('blgp,gip->blgi', hi, ci)
            return out.reshape(hr.shape[0], hr.shape[1], S5_WIDTH)

        hc_re, hc_im = ssm_scan(*drive(ug_c), ab_re, ab_im, None, reverse)
        end = 0 if reverse else -1
        h_re, h_im = ssm_scan(*drive(ug), ab_re, ab_im, (hc_re[:, end], hc_im[:, end]), reverse)
        y = y + readout(h_re, h_im)
        if need_ctx:
            y_c = y_c + readout(hc_re, hc_im)
    out = glu(jax.nn.gelu(y), w_glu)
    out_c = glu(jax.nn.gelu(y_c), w_glu) if need_ctx else None
    return out, out_c


def swiglu(t, w_gate, w_up, w_down):
    return (jax.nn.silu(t @ w_gate) * (t @ w_up)) @ w_down


def moe_swiglu(t, w_router, w_gate, w_up, w_down):
    logits = (t @ w_router).astype(jnp.float32)
    top_val, top_idx = lax.top_k(logits, TOP_K)
    top_w = jax.nn.softmax(top_val, axis=-1)
    comb = jnp.sum(jax.nn.one_hot(top_idx, N_EXPERTS, dtype=jnp.float32) * top_w[..., None], axis=-2).astype(t.dtype)
    out = comb[..., 0:1] * swiglu(t, w_gate[0], w_up[0], w_down[0])
    for e in range(1, N_EXPERTS):
        out = out + comb[..., e:e + 1] * swiglu(t, w_gate[e], w_up[e], w_down[e])
    return out


def setup_inputs(seed: int = 0) -> dict:
    key = jax.random.key(seed)
    ks = iter(jax.random.split(key, 48))
    D = D_MODEL

    def nrm(shape, scale):
        return scale * jax.random.normal(next(ks), shape, jnp.float32)

    def gain(shape):
        return 1.0 + nrm(shape, 0.05)

    inp = {}
    inp['x'] = nrm((BATCH, SEQ, D), 1.0)
    inp['c'] = nrm((BATCH, D), 1.0)
    inp['ctx'] = nrm((BATCH, CTX_LEN, D), 1.0)
    inp['c_ctx'] = nrm((D,), 1.0)
    inp['mod_w'] = nrm((DEPTH, D, N_MOD * D), 0.5 * D ** -0.5)
    inp['mod_b'] = nrm((DEPTH, N_MOD * D), 0.02)
    inp['norm1_g'] = gain((DEPTH, D))
    inp['norm2_g'] = gain((DEPTH, D))
    inp['ev_w_in'] = nrm((N_EVEN, D, EVEN_IN), D ** -0.5)
    inp['ev_q_norm_g'] = gain((N_EVEN, Q_LORA))
    inp['ev_w_qb'] = nrm((N_EVEN, Q_LORA, MLA_HEADS * (MLA_NOPE + MLA_ROPE)), Q_LORA ** -0.5)
    inp['ev_kv_norm_g'] = gain((N_EVEN, KV_LORA))
    inp['ev_w_kvb'] = nrm((N_EVEN, KV_LORA, MLA_HEADS * (MLA_NOPE + MLA_V)), KV_LORA ** -0.5)
    inp['ev_na_rpb'] = nrm((N_EVEN, NA_HEADS, 2 * NA_WIN_H - 1, 2 * NA_WIN_W - 1), 0.2)
    inp['ev_w_out'] = nrm((N_EVEN, EVEN_OUT, D), EVEN_OUT ** -0.5)
    inp['ev_ffn_w_gate'] = nrm((N_EVEN, D, D_FF), D ** -0.5)
    inp['ev_ffn_w_up'] = nrm((N_EVEN, D, D_FF), D ** -0.5)
    inp['ev_ffn_w_down'] = nrm((N_EVEN, D_FF, D), D_FF ** -0.5)
    inp['od_w_in'] = nrm((N_ODD, D, S5_WIDTH), D ** -0.5)
    inp['od_a_re'] = -0.5 + nrm((N_ODD, 2, S5_GROUPS, S5_STATE), 0.01)
    inp['od_a_im'] = math.pi * jnp.arange(S5_STATE, dtype=jnp.float32) + nrm((N_ODD, 2, S5_GROUPS, S5_STATE), 0.01)
    inp['od_log_step'] = jax.random.uniform(next(ks), (N_ODD, 2, S5_GROUPS), jnp.float32,
                                            math.log(1e-3), math.log(1e-1))
    inp['od_b_re'] = nrm((N_ODD, 2, S5_GROUPS, S5_STATE, S5_GROUP), (2 * S5_GROUP) ** -0.5)
    inp['od_b_im'] = nrm((N_ODD, 2, S5_GROUPS, S5_STATE, S5_GROUP), (2 * S5_GROUP) ** -0.5)
    inp['od_c_re'] = nrm((N_ODD, 2, S5_GROUPS, S5_GROUP, S5_STATE), S5_STATE ** -0.5)
    inp['od_c_im'] = nrm((N_ODD, 2, S5_GROUPS, S5_GROUP, S5_STATE), S5_STATE ** -0.5)
    inp['od_d'] = nrm((N_ODD, S5_WIDTH), 1.0)
    inp['od_w_glu'] = nrm((N_ODD, S5_WIDTH, 2 * D), S5_WIDTH ** -0.5)
    inp['moe_w_router'] = nrm((N_ODD, D, N_EXPERTS), D ** -0.5)
    inp['moe_w_gate'] = nrm((N_ODD, N_EXPERTS, D, D_FF_EXPERT), D ** -0.5)
    inp['moe_w_up'] = nrm((N_ODD, N_EXPERTS, D, D_FF_EXPERT), D ** -0.5)
    inp['moe_w_down'] = nrm((N_ODD, N_EXPERTS, D_FF_EXPERT, D), D_FF_EXPERT ** -0.5)
    inp['final_g'] = gain((D,))
    return inp


def reference(x, c, ctx, c_ctx, mod_w, mod_b, norm1_g, norm2_g,
              ev_w_in, ev_q_norm_g, ev_w_qb, ev_kv_norm_g, ev_w_kvb, ev_na_rpb, ev_w_out,
              ev_ffn_w_gate, ev_ffn_w_up, ev_ffn_w_down,
              od_w_in, od_a_re, od_a_im, od_log_step, od_b_re, od_b_im, od_c_re, od_c_im, od_d, od_w_glu,
              moe_w_router, moe_w_gate, moe_w_up, moe_w_down, final_g):
    xc = ctx
    for layer in range(DEPTH):
        last = layer == DEPTH - 1
        i = layer // 2
        sh1, sc1, g1, sh2, sc2, g2 = adaln_params(c, mod_w[layer], mod_b[layer])
        sh1c, sc1c, g1c, sh2c, sc2c, g2c = adaln_params(c_ctx, mod_w[layer], mod_b[layer])
        h = modulate(x, norm1_g[layer], sh1, sc1)
        hc = modulate(xc, norm1_g[layer], sh1c, sc1c)
        if layer % 2 == 0:
            y, yc = even_mixer(h, hc, ev_w_in[i], ev_q_norm_g[i], ev_w_qb[i], ev_kv_norm_g[i],
                               ev_w_kvb[i], ev_na_rpb[i], ev_w_out[i], not last)

            def channel_mix(t):
                return swiglu(t, ev_ffn_w_gate[i], ev_ffn_w_up[i], ev_ffn_w_down[i])
        else:
            y, yc = s5_mixer(h, hc, od_w_in[i], od_a_re[i], od_a_im[i], od_log_step[i], od_b_re[i],
                             od_b_im[i], od_c_re[i], od_c_im[i], od_d[i], od_w_glu[i], not last)

            def channel_mix(t):
                return moe_swiglu(t, moe_w_router[i], moe_w_gate[i], moe_w_up[i], moe_w_down[i])
        x = x + g1 * y
        x = x + g2 * channel_mix(modulate(x, norm2_g[layer], sh2, sc2))
        if not last:
            xc = xc + g1c * yc
            xc = xc + g2c * channel_mix(modulate(xc, norm2_g[layer], sh2c, sc2c))
    return rms_norm(x, final_g)
```

```python
import numpy as np
import concourse.bass as bass
import concourse.mybir as mybir
from concourse.bass_utils import run_bass_kernel_spmd
from contextlib import ExitStack
import ml_dtypes

F32 = mybir.dt.float32
BF16 = mybir.dt.bfloat16
I32 = mybir.dt.int32
U32 = mybir.dt.uint32
AF = mybir.ActivationFunctionType
ALU = mybir.AluOpType
AX = mybir.AxisListType
NPBF = ml_dtypes.bfloat16

NCORES = 8
D = 1024
B = 4
L = 4096
LC = 256
EPS = 1e-6


class Buf:
    __slots__ = ("w", "rs", "name")

    def __init__(self, name=""):
        self.w = None
        self.rs = {}
        self.name = name


class Tile:
    __slots__ = ("t", "b")

    def __init__(self, t, b):
        self.t = t
        self.b = b

    def __getitem__(self, idx):
        return self.t[idx]


COMPUTE = ("pe", "act", "dve", "pool")


class View:
    def __init__(self, ap, b):
        self.ap = ap
        self.b = b

    def __getitem__(self, idx):
        return self.ap


class Prog:
    def __init__(self, ndma=8):
        self.nc = bass.Bass("TRN2", target_bir_lowering=False)
        nc = self.nc
        self.es = ExitStack()
        self.eng = {"pe": nc.tensor, "act": nc.scalar, "dve": nc.vector,
                    "pool": nc.gpsimd, "sp": nc.sync}
        self.sems = {}
        self.cnt = {}
        for e in COMPUTE:
            self.sems[e] = self.es.enter_context(nc.semaphore("c_" + e))
            self.cnt[e] = 0
        self.waited = {e: {} for e in self.eng}
        self.dpool = {}
        self.didx = {}
        self.dval = {}
        for q in ("sp", "act", "pool"):
            n = ndma if q == "sp" else 4
            self.dpool[q] = []
            for i in range(n):
                k = ("d", q, i)
                self.sems[k] = self.es.enter_context(nc.semaphore("d_%s_%d" % (q, i)))
                self.dval[k] = 0
                self.dpool[q].append(k)
            self.didx[q] = 0
        self.ndram = 0

    def sb(self, name, shape, dt):
        t = self.es.enter_context(self.nc.sbuf_tensor("s_" + name, list(shape), dt))
        return Tile(t, Buf(name))

    def ps(self, name, shape, dt):
        t = self.es.enter_context(self.nc.psum_tensor("p_" + name, list(shape), dt))
        return Tile(t, Buf(name))

    def dram(self, name, shape, dt, kind):
        t = self.nc.dram_tensor(name, list(shape), dt, kind=kind)
        return Tile(t.ap(), Buf(name))

    def _wait(self, e, reads, writes):
        need = {}
        for b in reads:
            if b.w is not None:
                k, v = b.w
                if not (k == e and e == "pe"):
                    need[k] = max(need.get(k, 0), v)
        for b in writes:
            if b.w is not None:
                k, v = b.w
                if not (k == e and e == "pe"):
                    need[k] = max(need.get(k, 0), v)
            for k, v in b.rs.items():
                if not (k == e and e == "pe"):
                    need[k] = max(need.get(k, 0), v)
        w = self.waited[e]
        for k, v in need.items():
            if w.get(k, 0) < v:
                self.eng[e].wait_ge(self.sems[k], v)
                w[k] = v

    def _mark(self, tok, reads, writes):
        k, v = tok
        for b in reads:
            if b.rs.get(k, 0) < v:
                b.rs[k] = v
        for b in writes:
            b.w = tok
            b.rs = {}

    def op(self, e, fn, reads=(), writes=()):
        reads = [r.b if isinstance(r, (Tile, View)) else r for r in reads]
        writes = [r.b if isinstance(r, (Tile, View)) else r for r in writes]
        self._wait(e, reads, writes)
        inst = fn(self.eng[e])
        self.cnt[e] += 1
        inst.then_inc(self.sems[e], 1)
        self._mark((e, self.cnt[e]), reads, writes)

    def dma(self, q, out, in_, reads=(), writes=(), **kw):
        reads = [r.b if isinstance(r, (Tile, View)) else r for r in reads]
        writes = [r.b if isinstance(r, (Tile, View)) else r for r in writes]
        pool = self.dpool[q]
        k = pool[self.didx[q] % len(pool)]
        self.didx[q] += 1
        pv = self.dval[k]
        w = self.waited[q]
        if pv and w.get(k, 0) < pv:
            self.eng[q].wait_ge(self.sems[k], pv)
            w[k] = pv
        self._wait(q, reads, writes)
        self.eng[q].dma_start(out=out, in_=in_, **kw).then_inc(self.sems[k], 16)
        self.dval[k] = pv + 16
        self._mark((k, pv + 16), reads, writes)

    def barrier(self):
        for e in self.eng:
            w = self.waited[e]
            for k in COMPUTE:
                v = self.cnt[k]
                if v and k != e and w.get(k, 0) < v:
                    self.eng[e].wait_ge(self.sems[k], v)
                    w[k] = v
            for k, v in self.dval.items():
                if v and w.get(k, 0) < v:
                    self.eng[e].wait_ge(self.sems[k], v)
                    w[k] = v

    def finish(self):
        for k, v in self.dval.items():
            if v and self.waited["sp"].get(k, 0) < v:
                self.nc.sync.wait_ge(self.sems[k], v)
        self.es.close()
        return self.nc


TIMES = []


def run_prog(nc, in_maps, trace=False):
    res = run_bass_kernel_spmd(nc, in_maps, core_ids=list(range(len(in_maps))), trace=trace)
    if trace:
        TIMES.append(res.exec_time_ns)
    return res.results


def f32(a):
    return np.ascontiguousarray(a, dtype=np.float32)


def build_adaln():
    p = Prog()
    cond = p.dram("cond", [128, 8, 5], F32, "ExternalInput")
    w = p.dram("w", [2, 1024, 768], F32, "ExternalInput")
    bias5 = p.dram("bias5", [2, 5, 768], F32, "ExternalInput")
    gain5 = p.dram("gain5", [2, 5, 768], F32, "ExternalInput")
    m_out = p.dram("m", [2, 5, 768], F32, "ExternalOutput")
    gs_out = p.dram("gs", [2, 5, 768], F32, "ExternalOutput")
    ct = p.sb("ct", [128, 8, 5], F32)
    st = p.sb("st", [128, 8, 5], F32)
    p.dma("sp", ct[:], cond[:], [cond], [ct])
    p.op("act", lambda e: e.activation(out=st[:], in_=ct[:], func=AF.Silu), [ct], [st])
    pss = [p.ps("ps%d" % i, [128, 512], F32) for i in range(2)]
    for l in range(2):
        wt = p.sb("wt%d" % l, [128, 8, 768], F32)
        bt = p.sb("bt%d" % l, [5, 768], F32)
        gt = p.sb("gt%d" % l, [5, 768], F32)
        mt = p.sb("mt%d" % l, [5, 768], F32)
        gst = p.sb("gst%d" % l, [5, 768], F32)
        p.dma("sp", wt[:], w[l].rearrange("(c p) n -> p c n", p=128), [w], [wt])
        p.dma("sp", bt[:], bias5[l], [bias5], [bt])
        p.dma("sp", gt[:], gain5[l], [gain5], [gt])
        for nb in range(2):
            ps = pss[nb]
            cs = slice(nb * 384, (nb + 1) * 384)
            for c in range(8):
                p.op("pe", lambda e, c=c, cs=cs, ps=ps: e.matmul(
                    ps[0:5, 0:384], lhsT=st[:, c, :], rhs=wt[:, c, cs],
                    start=(c == 0), stop=(c == 7)), [st, wt], [ps])
            p.op("dve", lambda e, cs=cs, ps=ps: e.tensor_tensor(
                out=mt[:, cs], in0=ps[0:5, 0:384], in1=bt[:, cs], op=ALU.add), [ps, bt], [mt])
        p.op("dve", lambda e: e.scalar_tensor_tensor(
            out=gst[:], in0=mt[:], scalar=1.0, in1=gt[:], op0=ALU.add, op1=ALU.mult), [mt, gt], [gst])
        p.dma("sp", m_out[l], mt[:], [mt], [m_out])
        p.dma("sp", gs_out[l], gst[:], [gst], [gs_out])
    return p.finish()


def run_adaln(c, c_ctx, mod_w, mod_b, norm1_g, norm2_g):
    cond_all = np.concatenate([f32(c), f32(c_ctx)[None]], 0)
    condT = np.ascontiguousarray(cond_all.T.reshape(8, 128, 5).transpose(1, 0, 2))
    gain = np.zeros((2, 6144), np.float32)
    gain[:, 1024:2048] = f32(norm1_g)
    gain[:, 4096:5120] = f32(norm2_g)
    in_maps = []
    for j in range(NCORES):
        cs = slice(768 * j, 768 * j + 768)
        in_maps.append({
            "cond": condT,
            "w": np.ascontiguousarray(f32(mod_w)[:, :, cs]),
            "bias5": np.ascontiguousarray(np.broadcast_to(f32(mod_b)[:, None, cs], (2, 5, 768))),
            "gain5": np.ascontiguousarray(np.broadcast_to(gain[:, None, cs], (2, 5, 768))),
        })
    res = run_prog(build_adaln(), in_maps)
    m = np.concatenate([r["m"] for r in res], axis=2)
    gs = np.concatenate([r["gs"] for r in res], axis=2)
    return m, gs


class Stage:
    def __init__(self, p, width, n=2, name="stg"):
        self.p = p
        self.slots = [p.sb("%s%d" % (name, i), [128, width], F32) for i in range(n)]
        self.i = 0
        self.ce = 0

    def load(self, dst_ap, dst_buf, src_ap, src_buf, rows, n, eng=None, scale=None):
        p = self.p
        s = self.slots[self.i % len(self.slots)]
        self.i += 1
        p.dma("sp", s[0:rows, 0:n], src_ap, [src_buf], [s])
        if scale is not None:
            sc_ap, sc_t = scale
            p.op("dve", lambda e: e.tensor_scalar(out=dst_ap, in0=s[0:rows, 0:n], scalar1=sc_ap, scalar2=None,
                                                  op0=ALU.mult), [s, sc_t], [dst_buf])
            return
        if eng is None:
            eng = ("pool", "dve")[self.ce % 2]
            self.ce += 1
        p.op(eng, lambda e: e.tensor_copy(out=dst_ap, in_=s[0:rows, 0:n]), [s], [dst_buf])


def load_w(p, stg, name, src, kc, n, eng=None, rowscale=None):
    dst = p.sb(name, [128, kc, n], BF16)
    for c in range(kc):
        sc = None if rowscale is None else (rowscale[:, c:c + 1], rowscale)
        stg.load(dst[:, c, :], dst.b, src[c * 128:(c + 1) * 128, :], src.b, 128, n, eng, sc)
    return dst


class NormT:
    def __init__(self, p, ident, nslots=2):
        self.p = p
        self.ident = ident
        self.xt = [p.sb("nx%d" % i, [128, 1024], F32) for i in range(nslots)]
        self.xn = [p.sb("nn%d" % i, [128, 1024], BF16) for i in range(nslots)]
        self.junk = p.sb("njunk", [128, 1024], BF16)
        self.ss = [p.sb("nss%d" % i, [128, 2], F32) for i in range(nslots)]
        self.pt = [p.ps("npt%d" % i, [128, 1024], BF16) for i in range(2)]
        self.i = 0
        self.eps = mk_eps(p)

    def run(self, x_ap, x_buf, gsT, shT, cond, hT, col0, x_loaded=None):
        p = self.p
        k = self.i % len(self.xt)
        self.i += 1
        xt, xn, ss, pt = self.xt[k], self.xn[k], self.ss[k], self.pt[k % 2]
        if x_loaded is None:
            p.dma("sp", xt[:], x_ap, [x_buf], [xt])
        else:
            xt = x_loaded
        p.op("act", lambda e: e.activation(out=self.junk[:], in_=xt[:], func=AF.Square,
                                           accum_out=ss[:, 0:1]), [xt], [self.junk, ss])
        p.op("act", lambda e: e.activation(out=ss[:, 1:2], in_=ss[:, 0:1], func=AF.Sqrt,
                                           scale=1.0 / D, bias=self.eps[:, 0:1]), [ss, self.eps], [ss])
        p.op("dve", lambda e: e.reciprocal(out=ss[:, 0:1], in_=ss[:, 1:2]), [ss], [ss])
        p.op("dve", lambda e: e.tensor_scalar(out=xn[:], in0=xt[:], scalar1=ss[:, 0:1], scalar2=None,
                                              op0=ALU.mult), [xt, ss], [xn])
        for c in range(8):
            p.op("pe", lambda e, c=c: e.transpose(out=pt[:, c * 128:(c + 1) * 128],
                                                   in_=xn[:, c * 128:(c + 1) * 128],
                                                   identity=self.ident[:]), [xn, self.ident], [pt])
        for c in range(8):
            p.op("dve" if c % 2 == 0 else "act",
                 (lambda e, c=c: e.tensor_scalar(out=hT[:, c, col0:col0 + 128], in0=pt[:, c * 128:(c + 1) * 128],
                                                 scalar1=gsT[:, c, cond:cond + 1], scalar2=shT[:, c, cond:cond + 1],
                                                 op0=ALU.mult, op1=ALU.add)) if c % 2 == 0 else
                 (lambda e, c=c: e.activation(out=hT[:, c, col0:col0 + 128], in_=pt[:, c * 128:(c + 1) * 128],
                                              func=AF.Identity, scale=gsT[:, c, cond:cond + 1],
                                              bias=shT[:, c, cond:cond + 1])),
                 [pt, gsT, shT], [hT])


def mk_eps(p, val=EPS):
    t = p.sb("epsc", [128, 1], F32)
    p.op("pool", lambda e: e.memset(t[:], val), [], [t])
    return t


T2 = 2176
BLK2 = [(0, 512), (512, 512), (1024, 512), (1536, 512), (2048, 128)]
NA_SCALE = 64 ** -0.5
MLA_SCALE = 96 ** -0.5


def build_l2(stop=None):
    p = Prog()
    x = p.dram("x", [T2, D], F32, "ExternalInput")
    w1 = p.dram("w1", [D, 2368], F32, "ExternalInput")
    wq = p.dram("wq", [384, 1536], F32, "ExternalInput")
    wkv = p.dram("wkv", [256, 1024], F32, "ExternalInput")
    cos_d = p.dram("cos96", [96, T2], F32, "ExternalInput")
    sin_d = p.dram("sin96", [96, T2], F32, "ExternalInput")
    gsT_d = p.dram("gsT", [128, 8, 2], F32, "ExternalInput")
    shT_d = p.dram("shT", [128, 8, 2], F32, "ExternalInput")
    gq_d = p.dram("gq", [128, 3], F32, "ExternalInput")
    gkv_d = p.dram("gkv", [128, 2], F32, "ExternalInput")
    ident_d = p.dram("ident", [128, 128], BF16, "ExternalInput")
    QT = p.dram("QT", [96, 8, T2], BF16, "ExternalOutput")
    KT = p.dram("KT", [96, 8, T2], BF16, "ExternalOutput")
    V = p.dram("V", [T2, 512], BF16, "ExternalOutput")
    NQT = p.dram("NQT", [64, 8, T2], BF16, "ExternalOutput")
    NKT = p.dram("NKT", [64, 8, T2], BF16, "ExternalOutput")
    NV = p.dram("NV", [T2, 512], BF16, "ExternalOutput")

    ident = p.sb("ident", [128, 128], BF16)
    p.dma("sp", ident[:], ident_d[:], [ident_d], [ident])
    ones = p.sb("ones", [128, 128], BF16)
    p.op("pool", lambda e: e.memset(ones[:], 1.0), [], [ones])
    cos = p.sb("cos", [96, T2], F32)
    sin = p.sb("sin", [96, T2], F32)
    p.dma("sp", cos[:], cos_d[:], [cos_d], [cos])
    p.dma("sp", sin[:], sin_d[:], [sin_d], [sin])
    gsT = p.sb("gsT", [128, 8, 2], F32)
    shT = p.sb("shT", [128, 8, 2], F32)
    gq = p.sb("gq", [128, 3], F32)
    gkv = p.sb("gkv", [128, 2], F32)
    for a, b_ in ((gsT, gsT_d), (shT, shT_d), (gq, gq_d), (gkv, gkv_d)):
        p.dma("sp", a[:], b_[:], [b_], [a])
    stg = Stage(p, 2368)
    W1 = load_w(p, stg, "W1", w1, 8, 2368)
    WQ = load_w(p, stg, "WQ", wq, 3, 1536, rowscale=gq)
    WKV = load_w(p, stg, "WKV", wkv, 2, 1024, rowscale=gkv)
    O_CQ, O_CKV, O_KR, O_KRR, O_NQ, O_NK, O_NV = 0, 384, 640, 736, 832, 1344, 1856
    nt = NormT(p, ident)
    eps = nt.eps
    hT = p.sb("hT", [128, 8, 512], BF16)
    cqg = p.sb("cqg", [128, 3, 512], BF16)
    sqq = p.sb("sqq", [128, 3, 512], BF16)
    ckvg = p.sb("ckvg", [128, 2, 512], BF16)
    sqkv = p.sb("sqkv", [128, 2, 512], BF16)
    rq = p.sb("rq", [128, 512], F32)
    rkv = p.sb("rkv", [128, 512], F32)
    rtok = p.sb("rtok", [128, 8], F32)
    tmpa = [p.sb("tmpa%d" % i, [128, 512], F32) for i in range(2)]
    tmpb = [p.sb("tmpb%d" % i, [128, 512], F32) for i in range(2)]
    krt = p.sb("krt", [96, 512], BF16)
    Qb = p.sb("Qb", [96, 8, 512], BF16)
    Kb = p.sb("Kb", [96, 8, 512], BF16)
    NQb = p.sb("NQb", [64, 8, 512], BF16)
    NKb = p.sb("NKb", [64, 8, 512], BF16)
    Vb = p.sb("Vb", [128, 4, 512], BF16)
    NVb = p.sb("NVb", [128, 4, 512], BF16)
    pp = [p.ps("pp%d" % i, [128, 512], F32) for i in range(6)]
    ppi = [0]

    def bank():
        ppi[0] += 1
        return pp[ppi[0] % 6]

    def mm(ps_ap, ps_t, pairs, rd):
        n = len(pairs)
        for i, (l, r) in enumerate(pairs):
            p.op("pe", lambda e, l=l, r=r, i=i: e.matmul(ps_ap, lhsT=l, rhs=r, start=(i == 0), stop=(i == n - 1)),
                 rd, [ps_t])

    for (c0, n) in BLK2:
        ntile = n // 128
        for ti in range(ntile):
            t0 = c0 + ti * 128
            cond = 0 if t0 < 2048 else 1
            nt.run(x[t0:t0 + 128, :], x.b, gsT, shT, cond, hT, ti * 128)
        if stop == 'norm':
            break
        for (dst, sq, g, off, nch, rbc, dim) in ((cqg, sqq, gq, O_CQ, 3, rq, 384), (ckvg, sqkv, gkv, O_CKV, 2, rkv, 256)):
            for c3 in range(nch):
                ps = bank()
                mm(ps[:, 0:n], ps, [(W1[:, c, off + c3 * 128: off + (c3 + 1) * 128], hT[:, c, 0:n]) for c in range(8)], [W1, hT])
                if stop == 'cq_mm':
                    continue
                p.op("dve", lambda e, ps=ps, c3=c3, dst=dst: e.tensor_copy(out=dst[:, c3, 0:n], in_=ps[:, 0:n]), [ps], [dst])
                p.op("act", lambda e, c3=c3, sq=sq, dst=dst: e.activation(out=sq[:, c3, 0:n], in_=dst[:, c3, 0:n], func=AF.Square), [dst], [sq])
            if stop in ('cq_mm', 'cq_dve', 'cq_act', 'cq_act2'):
                continue
            ps = bank()
            mm(ps[:, 0:n], ps, [(ones[:], sq[:, c3, 0:n]) for c3 in range(nch)], [ones, sq])
            if stop == 'cq_ones':
                continue
            p.op("act", lambda e, ps=ps, rbc=rbc, dim=dim: e.activation(out=rbc[:, 0:n], in_=ps[:, 0:n], func=AF.Sqrt,
                                                                      scale=1.0 / dim, bias=eps[:, 0:1]), [ps, eps], [rbc])
            p.op("dve", lambda e, rbc=rbc: e.reciprocal(out=rbc[:, 0:n], in_=rbc[:, 0:n]), [rbc], [rbc])
        if stop in ('cq', 'cq_mm', 'cq_dve', 'cq_act', 'cq_ones', 'cq_act2'):
            break
        ps = bank()
        for ti in range(ntile):
            mm(ps[:, ti:ti + 1], ps, [(sqkv[:, c2, ti * 128:(ti + 1) * 128], ones[:, 0:1]) for c2 in range(2)], [sqkv, ones])
        p.op("act", lambda e, ps=ps: e.activation(out=rtok[:, 0:ntile], in_=ps[:, 0:ntile], func=AF.Sqrt,
                                                  scale=1.0 / 256, bias=eps[:, 0:1]), [ps, eps], [rtok])
        p.op("dve", lambda e: e.reciprocal(out=rtok[:, 0:ntile], in_=rtok[:, 0:ntile]), [rtok], [rtok])
        if stop == 'rtok':
            break
        def rope(pa, pb, out_ap, out_t, scale_ap=None, scale_t=None, k=0):
            ta, tb = tmpa[k % 2], tmpb[k % 2]
            p.op("dve", lambda e: e.tensor_tensor(out=ta[0:96, 0:n], in0=pa[0:96, 0:n], in1=cos[:, c0:c0 + n], op=ALU.mult), [pa, cos], [ta])
            p.op("dve", lambda e: e.tensor_tensor(out=tb[0:96, 0:n], in0=pb[0:96, 0:n], in1=sin[:, c0:c0 + n], op=ALU.mult), [pb, sin], [tb])
            if scale_ap is None:
                p.op("pool", lambda e: e.tensor_tensor(out=out_ap, in0=ta[0:96, 0:n], in1=tb[0:96, 0:n], op=ALU.add), [ta, tb], [out_t])
            else:
                p.op("pool", lambda e: e.tensor_tensor(out=ta[0:96, 0:n], in0=ta[0:96, 0:n], in1=tb[0:96, 0:n], op=ALU.add), [ta, tb], [ta])
                p.op("pool", lambda e: e.tensor_tensor(out=out_ap, in0=ta[0:96, 0:n], in1=scale_ap, op=ALU.mult), [ta, scale_t], [out_t])
        for h in range(8):
            pa, pb = bank(), bank()
            mm(pa[0:96, 0:n], pa, [(WQ[:, c3, h * 96:(h + 1) * 96], cqg[:, c3, 0:n]) for c3 in range(3)], [WQ, cqg])
            mm(pb[0:96, 0:n], pb, [(WQ[:, c3, 768 + h * 96: 768 + (h + 1) * 96], cqg[:, c3, 0:n]) for c3 in range(3)], [WQ, cqg])
            rope(pa, pb, Qb[:, h, 0:n], Qb, rq[0:96, 0:n], rq, k=h)
        if stop == 'q':
            break
        pa, pb = bank(), bank()
        mm(pa[0:96, 0:n], pa, [(W1[:, c, O_KR:O_KR + 96], hT[:, c, 0:n]) for c in range(8)], [W1, hT])
        mm(pb[0:96, 0:n], pb, [(W1[:, c, O_KRR:O_KRR + 96], hT[:, c, 0:n]) for c in range(8)], [W1, hT])
        rope(pa, pb, krt[:, 0:n], krt)
        if stop == 'kr':
            break
        for h in range(8):
            ps = bank()
            mm(ps[0:64, 0:n], ps, [(WKV[:, c2, h * 64:(h + 1) * 64], ckvg[:, c2, 0:n]) for c2 in range(2)], [WKV, ckvg])
            p.op("dve", lambda e, ps=ps, h=h: e.tensor_tensor(out=Kb[0:64, h, 0:n], in0=ps[0:64, 0:n], in1=rkv[0:64, 0:n], op=ALU.mult), [ps, rkv], [Kb])
            p.op("pool", lambda e, h=h: e.tensor_copy(out=Kb[64:96, h, 0:n], in_=krt[64:96, 0:n]), [krt], [Kb])
        for ti in range(ntile):
            ps = bank()
            mm(ps[:, :], ps, [(ckvg[:, c2, ti * 128:(ti + 1) * 128], WKV[:, c2, 512:1024]) for c2 in range(2)], [WKV, ckvg])
            p.op("dve", lambda e, ps=ps, ti=ti: e.tensor_scalar(out=Vb[:, ti, :], in0=ps[:, :], scalar1=rtok[:, ti:ti + 1], scalar2=None, op0=ALU.mult), [ps, rtok], [Vb])
        if stop == 'kv':
            break
        for h in range(8):
            ps = bank()
            mm(ps[0:64, 0:n], ps, [(W1[:, c, O_NQ + h * 64:O_NQ + (h + 1) * 64], hT[:, c, 0:n]) for c in range(8)], [W1, hT])
            p.op("act", lambda e, ps=ps, h=h: e.mul(out=NQb[:, h, 0:n], in_=ps[0:64, 0:n], mul=NA_SCALE), [ps], [NQb])
            ps = bank()
            mm(ps[0:64, 0:n], ps, [(W1[:, c, O_NK + h * 64:O_NK + (h + 1) * 64], hT[:, c, 0:n]) for c in range(8)], [W1, hT])
            p.op("dve", lambda e, ps=ps, h=h: e.tensor_copy(out=NKb[:, h, 0:n], in_=ps[0:64, 0:n]), [ps], [NKb])
        for ti in range(ntile):
            ps = bank()
            mm(ps[:, :], ps, [(hT[:, c, ti * 128:(ti + 1) * 128], W1[:, c, O_NV:O_NV + 512]) for c in range(8)], [W1, hT])
            p.op("act", lambda e, ps=ps, ti=ti: e.copy(out=NVb[:, ti, :], in_=ps[:, :]), [ps], [NVb])
        if stop == 'na':
            break
        p.dma("pool", QT[:, :, c0:c0 + n], Qb[:, :, 0:n], [Qb], [QT])
        p.dma("pool", KT[:, :, c0:c0 + n], Kb[:, :, 0:n], [Kb], [KT])
        p.dma("pool", NQT[:, :, c0:c0 + n], NQb[:, :, 0:n], [NQb], [NQT])
        p.dma("pool", NKT[:, :, c0:c0 + n], NKb[:, :, 0:n], [NKb], [NKT])
        p.dma("pool", V[c0:c0 + n, :].rearrange("(t p) f -> p t f", p=128), Vb[:, 0:ntile, :], [Vb], [V])
        p.dma("pool", NV[c0:c0 + n, :].rearrange("(t p) f -> p t f", p=128), NVb[:, 0:ntile, :], [NVb], [NV])
    return p.finish()


def rope_tables(pos):
    T = len(pos)
    cos = np.ones((96, T), np.float64)
    sin = np.zeros((96, T), np.float64)
    invf = 10000.0 ** (-np.arange(8) / 8.0)
    valid = pos >= 0
    row = (pos // 64).astype(np.float64)
    col = (pos % 64).astype(np.float64)
    for j in range(32):
        pp_ = row if j < 16 else col
        ang = (pp_.astype(np.float32) * invf[j % 8].astype(np.float32)).astype(np.float64)
        cj = np.where(valid, np.cos(ang), 1.0)
        sj = np.where(valid, np.sin(ang), 0.0)
        cos[64 + j] = cj
        sin[64 + j] = -sj if (j % 16) < 8 else sj
    return cos.astype(np.float32), sin.astype(np.float32)


ROPE_PERM = np.array([j + 8 if (j % 16) < 8 else j - 8 for j in range(32)])


def fm(vec, nch):
    return np.ascontiguousarray(f32(vec).reshape(nch, 128).T)


def l2_inputs(xtok, pos, gs_lat, sh_lat, gs_ctx, sh_ctx, ev_w_in, q_norm_g, w_qb, kv_norm_g, w_kvb):
    w_in = f32(ev_w_in)
    kr = w_in[:, 640:672]
    z64 = np.zeros((D, 64), np.float32)
    w1 = np.concatenate([w_in[:, 0:640], z64, kr, z64, kr[:, ROPE_PERM], w_in[:, 672:2208]], axis=1)
    wqb = f32(w_qb).reshape(384, 8, 96)
    wq_rot = np.concatenate([np.zeros((384, 8, 64), np.float32), wqb[:, :, 64:][:, :, ROPE_PERM]], axis=2)
    wq = np.concatenate([wqb.reshape(384, 768), wq_rot.reshape(384, 768)], axis=1)
    wkvb = f32(w_kvb).reshape(256, 8, 128)
    wkv = np.concatenate([wkvb[:, :, :64].reshape(256, 512), wkvb[:, :, 64:].reshape(256, 512)], axis=1)
    cos96, sin96 = rope_tables(pos)
    ident = np.eye(128, dtype=np.float32).astype(NPBF)
    return {
        "x": f32(xtok), "w1": np.ascontiguousarray(w1), "wq": np.ascontiguousarray(wq), "wkv": np.ascontiguousarray(wkv),
        "cos96": cos96, "sin96": sin96,
        "gsT": np.ascontiguousarray(np.stack([fm(gs_lat, 8), fm(gs_ctx, 8)], axis=2)),
        "shT": np.ascontiguousarray(np.stack([fm(sh_lat, 8), fm(sh_ctx, 8)], axis=2)),
        "gq": fm(q_norm_g, 3), "gkv": fm(kv_norm_g, 2), "ident": ident,
    }


NKEY = 4352
NAK = 40 * 64 + 256


def build_l3():
    p = Prog()
    QT = p.dram("QT", [96, 8, T2], BF16, "ExternalInput")
    KT = p.dram("KT", [96, 8, NKEY], BF16, "ExternalInput")
    VA = p.dram("VA", [NKEY, 8, 65], BF16, "ExternalInput")
    NQT = p.dram("NQT", [64, 8, T2], BF16, "ExternalInput")
    NKT = p.dram("NKT", [64, 8, NAK], BF16, "ExternalInput")
    NVA = p.dram("NVA", [NAK, 8, 65], BF16, "ExternalInput")
    NB = p.dram("NB", [8, 128, 18, 256], F32, "ExternalInput")
    AO = p.dram("AO", [T2, D], BF16, "ExternalOutput")
    attn = p.sb("attn", [128, 17, D], BF16)
    S = [p.ps("S%d" % i, [128, 512], F32) for i in range(2)]
    O = [p.ps("O%d" % i, [128, 512], F32) for i in range(4)]
    PT = [p.sb("PT%d" % i, [128, 512], BF16) for i in range(3)]
    rden = [p.sb("rden%d" % i, [128, 1], F32) for i in range(4)]
    tmp = [p.sb("tmpf%d" % i, [128, 256], F32) for i in range(2)]
    cnt = {"s": 0, "pt": 0, "tm": 0}

    def attend(kt, ktoff, q, q0, nq, keytiles, v, scale, bias=None, tile0=0, col0=0):
        nqs = nq // 128
        nk = len(keytiles)

        def score(i):
            kb, bj = keytiles[i]
            ps = S[cnt["s"] % 2]
            cnt["s"] += 1
            pt = PT[cnt["pt"] % 3]
            cnt["pt"] += 1
            p.op("pe", lambda e: e.matmul(ps[:, 0:nq], lhsT=kt[:, kb * 128:(kb + 1) * 128], rhs=q[:, q0:q0 + nq],
                                          start=True, stop=True), [kt, q], [ps])
            if bj is None:
                p.op("act", lambda e: e.activation(out=pt[:, 0:nq], in_=ps[:, 0:nq], func=AF.Exp, scale=scale), [ps], [pt])
            else:
                tm = tmp[cnt["tm"] % 2]
                cnt["tm"] += 1
                p.op("dve", lambda e: e.tensor_tensor(out=tm[:, 0:nq], in0=ps[:, 0:nq], in1=bias[:, bj, 0:nq], op=ALU.add), [ps, bias], [tm])
                p.op("act", lambda e: e.activation(out=pt[:, 0:nq], in_=tm[:, 0:nq], func=AF.Exp, scale=scale), [tm], [pt])
            return pt

        pts = [score(0)]
        for i, (kb, bj) in enumerate(keytiles):
            if i + 1 < nk:
                pts.append(score(i + 1))
            pt = pts[i]
            for qs in range(nqs):
                p.op("pe", lambda e, qs=qs: e.matmul(O[qs][:, 0:65], lhsT=pt[:, qs * 128:(qs + 1) * 128], rhs=v[:, kb, :],
                                                     start=(i == 0), stop=(i == nk - 1)), [pt, v], [O[qs]])
        for qs in range(nqs):
            p.op("dve", lambda e, qs=qs: e.reciprocal(out=rden[qs][:], in_=O[qs][:, 64:65]), [O[qs]], [rden[qs]])
            p.op("dve", lambda e, qs=qs: e.tensor_scalar(out=attn[:, tile0 + qs, col0:col0 + 64], in0=O[qs][:, 0:64],
                                                         scalar1=rden[qs][:, 0:1], scalar2=None, op0=ALU.mult),
                 [O[qs], rden[qs]], [attn])

    kth = [p.sb("kth%d" % i, [96, NKEY], BF16) for i in range(2)]
    vh = [p.sb("vh%d" % i, [128, 34, 65], BF16) for i in range(2)]
    qh = [p.sb("qh%d" % i, [96, T2], BF16) for i in range(2)]
    for h in range(8):
        k_, v_, q_ = kth[h % 2], vh[h % 2], qh[h % 2]
        p.dma("sp", k_[:], KT[:, h, :], [KT], [k_])
        p.dma("sp", v_[:], VA[:, h, :].rearrange("(t p) f -> p t f", p=128), [VA], [v_])
        p.dma("sp", q_[:], QT[:, h, :], [QT], [q_])
        for qb in range(4):
            attend(k_, 0, q_, qb * 512, 512, [(kb, None) for kb in range(34)], v_, MLA_SCALE, tile0=qb * 4, col0=h * 64)
        attend(k_, 0, q_, 2048, 128, [(32, None), (33, None)], v_, MLA_SCALE, tile0=16, col0=h * 64)
    nkh = [p.sb("nkh%d" % i, [64, NAK], BF16) for i in range(2)]
    nvh = [p.sb("nvh%d" % i, [128, 22, 65], BF16) for i in range(2)]
    nqh = [p.sb("nqh%d" % i, [64, T2], BF16) for i in range(2)]
    nbh = [p.sb("nbh%d" % i, [128, 18, 256], F32) for i in range(2)]
    for h in range(8):
        k_, v_, q_, b_ = nkh[h % 2], nvh[h % 2], nqh[h % 2], nbh[h % 2]
        p.dma("sp", k_[:], NKT[:, h, :], [NKT], [k_])
        p.dma("sp", v_[:], NVA[:, h, :].rearrange("(t p) f -> p t f", p=128), [NVA], [v_])
        p.dma("sp", q_[:], NQT[:, h, :], [NQT], [q_])
        p.dma("sp", b_[:], NB[h], [NB], [b_])
        for qt in range(8):
            slot = 0 if qt == 0 else (2 if qt == 7 else 1)
            kts = [(2 * qt + j, slot * 6 + j) for j in range(6)] + [(20, None), (21, None)]
            attend(k_, 0, q_, qt * 256, 256, kts, v_, 1.0, bias=b_, tile0=qt * 2, col0=512 + h * 64)
        attend(k_, 0, q_, 2048, 128, [(20, None), (21, None)], v_, 1.0, tile0=16, col0=512 + h * 64)
    p.dma("sp", AO.t.rearrange("(t p) f -> p t f", p=128), attn[:], [attn], [AO])
    return p.finish()


def na_bias(rpb, hf, qt):
    rpb = f32(rpb)
    j = np.arange(6)[:, None, None, None, None]
    krl = np.arange(2)[None, :, None, None, None]
    kc = np.arange(64)[None, None, :, None, None]
    qrl = np.arange(4)[None, None, None, :, None]
    qc = np.arange(64)[None, None, None, None, :]
    kr = 32 * hf + 4 * qt - 4 + 2 * j + krl
    r = 32 * hf + 4 * qt + qrl
    rs = np.clip(r - 4, 0, 56)
    cs = np.clip(qc - 8, 0, 48)
    ok = (kr >= 0) & (kr < 64) & (kr >= rs) & (kr < rs + 8) & (kc >= cs) & (kc < cs + 16)
    ro = np.clip(kr - r + 7, 0, 14) + 0 * kc + 0 * qc
    co = np.clip(kc - qc + 15, 0, 30) + 0 * kr + 0 * r
    ok = np.broadcast_to(ok, ro.shape)
    out = np.where(ok[None], rpb[:, ro, co], np.float32(-30000.0))
    return out.reshape(8, 6, 128, 256).astype(np.float32)


def l3_inputs(b, hf, l2res, rpb):
    r0, r1 = l2res[2 * b], l2res[2 * b + 1]
    own = l2res[2 * b + hf]
    KT = np.concatenate([r0["KT"][:, :, :2048], r1["KT"][:, :, :2048], r0["KT"][:, :, 2048:], r1["KT"][:, :, 2048:]], axis=2)
    Vall = np.concatenate([r0["V"][:2048], r1["V"][:2048], r0["V"][2048:], r1["V"][2048:]], axis=0).reshape(NKEY, 8, 64)
    VA = np.concatenate([Vall, np.ones((NKEY, 8, 1), NPBF)], axis=2)
    nk_lat = np.concatenate([r0["NKT"][:, :, :2048], r1["NKT"][:, :, :2048]], axis=2)
    nk_ctx = np.concatenate([r0["NKT"][:, :, 2048:], r1["NKT"][:, :, 2048:]], axis=2)
    nv_lat = np.concatenate([r0["NV"][:2048], r1["NV"][:2048]], axis=0).reshape(4096, 8, 64)
    nv_ctx = np.concatenate([r0["NV"][2048:], r1["NV"][2048:]], axis=0).reshape(256, 8, 64)
    NK = np.zeros((64, 8, NAK), NPBF)
    NVv = np.zeros((NAK, 8, 64), NPBF)
    for i in range(40):
        gr = 32 * hf - 4 + i
        if 0 <= gr < 64:
            NK[:, :, i * 64:(i + 1) * 64] = nk_lat[:, :, gr * 64:(gr + 1) * 64]
            NVv[i * 64:(i + 1) * 64] = nv_lat[gr * 64:(gr + 1) * 64]
    NK[:, :, 2560:] = nk_ctx
    NVv[2560:] = nv_ctx
    NVA = np.concatenate([NVv, np.ones((NAK, 8, 1), NPBF)], axis=2)
    nb = np.stack([na_bias(rpb, hf, qt) for qt in (0, 3, 7)], axis=1)
    NB = np.ascontiguousarray(nb.reshape(8, 18, 128, 256).transpose(0, 2, 1, 3))
    return {"QT": own["QT"], "KT": np.ascontiguousarray(KT), "VA": np.ascontiguousarray(VA), "NQT": own["NQT"],
            "NKT": NK, "NVA": np.ascontiguousarray(NVA), "NB": NB}


class View:
    def __init__(self, ap, b):
        self.ap = ap
        self.b = b

    def __getitem__(self, idx):
        return self.ap


class Panels:
    def __init__(self, p, nslots=4):
        self.p = p
        self.st = [p.sb("pst%d" % i, [128, 8, 128], F32) for i in range(nslots)]
        self.bf = [p.sb("pbf%d" % i, [128, 8, 128], BF16) for i in range(nslots)]
        self.i = 0

    def get(self, w, col0):
        p = self.p
        k = self.i % len(self.st)
        self.i += 1
        st, bf = self.st[k], self.bf[k]
        if len(w.t.shape) == 4:
            p.dma("sp", st[:], w[col0 // 128], [w], [st])
        else:
            p.dma("sp", st[:], w[:, col0:col0 + 128].rearrange("(c p) n -> p c n", p=128), [w], [st])
        p.op("pool" if k % 2 == 0 else "dve", lambda e: e.tensor_copy(out=bf[:], in_=st[:]), [st], [bf])
        return bf


def ffn_phase1(p, pan, banks, hT, n, wg, wu, nf, actT, sg, f0=0, between=None):
    for f in range(f0, f0 + nf):
        g_, u_ = pan.get(wg, f * 128), pan.get(wu, f * 128)
        if between is not None:
            between(f - f0)
        for n0 in range(0, n, 512):
            nn = min(512, n - n0)
            pg, pu = banks(), banks()
            for (ps, w_) in ((pg, g_), (pu, u_)):
                for c in range(8):
                    p.op("pe", lambda e, ps=ps, w_=w_, c=c: e.matmul(ps[:, 0:nn], lhsT=w_[:, c, :], rhs=hT[:, c, n0:n0 + nn],
                                                                   start=(c == 0), stop=(c == 7)), [w_, hT], [ps])
            s = sg[(f + n0 // 512) % 2]
            p.op("act", lambda e, s=s, pg=pg: e.activation(out=s[:, 0:nn], in_=pg[:, 0:nn], func=AF.Silu), [pg], [s])
            p.op("dve", lambda e, s=s, pu=pu, f=f: e.tensor_tensor(out=actT[:, f - f0, n0:n0 + nn], in0=s[:, 0:nn], in1=pu[:, 0:nn],
                                                                 op=ALU.mult), [s, pu], [actT])


def build_l4():
    p = Prog()
    x = p.dram("x", [T2, D], F32, "ExternalInput")
    aT = p.dram("aT", [D, T2], BF16, "ExternalInput")
    wo = p.dram("wo", [D, D], F32, "ExternalInput")
    wg = p.dram("wg", [22, 128, 8, 128], F32, "ExternalInput")
    wu = p.dram("wu", [22, 128, 8, 128], F32, "ExternalInput")
    wd = p.dram("wd", [2816, D], F32, "ExternalInput")
    g1_d = p.dram("g1bc", [2, 128, D], F32, "ExternalInput")
    g2_d = p.dram("g2bc", [2, 128, D], F32, "ExternalInput")
    gsT_d = p.dram("gsT", [128, 8, 2], F32, "ExternalInput")
    shT_d = p.dram("shT", [128, 8, 2], F32, "ExternalInput")
    ident_d = p.dram("ident", [128, 128], BF16, "ExternalInput")
    xo = p.dram("xo", [T2, D], F32, "ExternalOutput")
    ident = p.sb("ident", [128, 128], BF16)
    p.dma("sp", ident[:], ident_d[:], [ident_d], [ident])
    gsT = p.sb("gsT", [128, 8, 2], F32)
    shT = p.sb("shT", [128, 8, 2], F32)
    p.dma("sp", gsT[:], gsT_d[:], [gsT_d], [gsT])
    p.dma("sp", shT[:], shT_d[:], [shT_d], [shT])
    stg = Stage(p, 1024)
    WO = load_w(p, stg, "WO", wo, 8, 1024)
    WD = load_w(p, stg, "WD", wd, 22, 1024)
    nt = NormT(p, ident, nslots=1)
    pan = Panels(p)
    hT = p.sb("hT", [128, 8, 512], BF16)
    at = p.sb("at", [128, 8, 512], BF16)
    actT = p.sb("actT", [128, 22, 512], BF16)
    xs = p.sb("xs", [128, 4, D], F32)
    g1 = p.sb("g1", [128, D], F32)
    g2 = p.sb("g2", [128, D], F32)
    sg = [p.sb("sg%d" % i, [128, 512], F32) for i in range(2)]
    tm = [p.sb("tm%d" % i, [128, 512], F32) for i in range(2)]
    pp = [p.ps("pp%d" % i, [128, 512], F32) for i in range(6)]
    ppi = [0]

    def banks():
        ppi[0] += 1
        return pp[ppi[0] % 6]

    tmi = [0]

    def resid(ps, gt, ti, cb):
        t = tm[tmi[0] % 2]
        tmi[0] += 1
        p.op("dve", lambda e: e.tensor_tensor(out=t[:], in0=ps[:], in1=gt[:, cb * 512:(cb + 1) * 512], op=ALU.mult), [ps, gt], [t])
        p.op("pool", lambda e: e.tensor_tensor(out=xs[:, ti, cb * 512:(cb + 1) * 512], in0=xs[:, ti, cb * 512:(cb + 1) * 512],
                                               in1=t[:], op=ALU.add), [xs, t], [xs])

    for (c0, n) in BLK2:
        ntile = n // 128
        cond = 0 if c0 < 2048 else 1
        p.dma("sp", g1[:], g1_d[cond], [g1_d], [g1])
        p.dma("sp", g2[:], g2_d[cond], [g2_d], [g2])
        p.dma("sp", xs[:, 0:ntile, :], x[c0:c0 + n, :].rearrange("(t p) f -> p t f", p=128), [x], [xs])
        p.dma("sp", at[:, :, 0:n], aT[:, c0:c0 + n].rearrange("(c p) t -> p c t", p=128), [aT], [at])
        for ti in range(ntile):
            for cb in range(2):
                ps = banks()
                for c in range(8):
                    p.op("pe", lambda e, c=c, ps=ps: e.matmul(ps[:], lhsT=at[:, c, ti * 128:(ti + 1) * 128],
                                                             rhs=WO[:, c, cb * 512:(cb + 1) * 512], start=(c == 0), stop=(c == 7)),
                         [at, WO], [ps])
                resid(ps, g1, ti, cb)
            nt.run(None, None, gsT, shT, cond, hT, ti * 128, x_loaded=View(xs[:, ti, :], xs.b))
        ffn_phase1(p, pan, banks, hT, n, wg, wu, 22, actT, sg)
        for ti in range(ntile):
            for cb in range(2):
                ps = banks()
                for f in range(22):
                    p.op("pe", lambda e, f=f, ps=ps: e.matmul(ps[:], lhsT=actT[:, f, ti * 128:(ti + 1) * 128],
                                                             rhs=WD[:, f, cb * 512:(cb + 1) * 512], start=(f == 0), stop=(f == 21)),
                         [actT, WD], [ps])
                resid(ps, g2, ti, cb)
        p.dma("pool", xo[c0:c0 + n, :].rearrange("(t p) f -> p t f", p=128), xs[:, 0:ntile, :], [xs], [xo])
    return p.finish()


def pretile(w):
    w = f32(w)
    F = w.shape[1]
    return np.ascontiguousarray(w.reshape(8, 128, F // 128, 128).transpose(2, 1, 0, 3))


def bc128(v):
    return np.ascontiguousarray(np.broadcast_to(f32(v)[None, :], (128, len(v))))


def build_l5():
    p = Prog()
    x = p.dram("x", [T2, D], F32, "ExternalInput")
    wi = p.dram("wi", [D, D], F32, "ExternalInput")
    gsT_d = p.dram("gsT", [128, 8, 2], F32, "ExternalInput")
    shT_d = p.dram("shT", [128, 8, 2], F32, "ExternalInput")
    ident_d = p.dram("ident", [128, 128], BF16, "ExternalInput")
    uo = p.dram("u", [T2, D], F32, "ExternalOutput")
    ident = p.sb("ident", [128, 128], BF16)
    p.dma("sp", ident[:], ident_d[:], [ident_d], [ident])
    gsT = p.sb("gsT", [128, 8, 2], F32)
    shT = p.sb("shT", [128, 8, 2], F32)
    p.dma("sp", gsT[:], gsT_d[:], [gsT_d], [gsT])
    p.dma("sp", shT[:], shT_d[:], [shT_d], [shT])
    stg = Stage(p, 1024)
    WI = load_w(p, stg, "WI", wi, 8, 1024)
    nt = NormT(p, ident)
    hT = [p.sb("hT%d" % i, [128, 8, 128], BF16) for i in range(2)]
    us = [p.sb("us%d" % i, [128, D], F32) for i in range(2)]
    pp = [p.ps("pp%d" % i, [128, 512], F32) for i in range(4)]
    k = 0
    for ti in range(T2 // 128):
        cond = 0 if ti < 16 else 1
        h_, u_ = hT[ti % 2], us[ti % 2]
        nt.run(x[ti * 128:(ti + 1) * 128, :], x.b, gsT, shT, cond, h_, 0)
        for cb in range(2):
            ps = pp[k % 4]
            k += 1
            for c in range(8):
                p.op("pe", lambda e, c=c, ps=ps: e.matmul(ps[:], lhsT=h_[:, c, :], rhs=WI[:, c, cb * 512:(cb + 1) * 512],
                                                         start=(c == 0), stop=(c == 7)), [h_, WI], [ps])
            p.op("act" if cb else "dve", (lambda e, ps=ps: e.copy(out=u_[:, cb * 512:(cb + 1) * 512], in_=ps[:])) if cb else
                 (lambda e, ps=ps: e.tensor_copy(out=u_[:, cb * 512:(cb + 1) * 512], in_=ps[:])), [ps], [u_])
        p.dma("pool", uo[ti * 128:(ti + 1) * 128, :], u_[:], [u_], [uo])
    return p.finish()


NCH = 544
TWO_PI = 2.0 * np.pi


def build_l6():
    p = Prog()
    U = p.dram("U", [64, 128, NCH], F32, "ExternalInput")
    prm = p.dram("prm", [3, 128, 64], F32, "ExternalInput")
    bri = p.dram("bri", [2, 128, 64, 16], F32, "ExternalInput")
    cri = p.dram("cri", [2, 128, 64, 16], F32, "ExternalInput")
    sel = p.dram("sel", [128, 2], F32, "ExternalInput")
    dq_d = p.dram("dq", [128, 64], F32, "ExternalInput")
    identf_d = p.dram("identf", [128, 128], F32, "ExternalInput")
    ident_d = p.dram("ident", [128, 128], BF16, "ExternalInput")
    Y = p.dram("Y", [64, 128, 512], F32, "ExternalOutput")

    def ld(name, shape, src, dt=F32):
        t = p.sb(name, shape, dt)
        p.dma("sp", t[:], src, [src] if isinstance(src, Tile) else [], [t])
        return t

    AR = ld("AR", [128, 64], prm[0]); AI = ld("AI", [128, 64], prm[1]); LS = ld("LS", [128, 64], prm[2])
    BR = ld("BR", [128, 64, 16], bri[0]); BI = ld("BI", [128, 64, 16], bri[1])
    CR = ld("CR", [128, 64, 16], cri[0]); CI = ld("CI", [128, 64, 16], cri[1])
    SEL = ld("SEL", [128, 2], sel[:]); DQ = ld("DQ", [128, 64], dq_d[:])
    IDF = ld("IDF", [128, 128], identf_d[:]); IDB = ld("IDB", [128, 128], ident_d[:], BF16)
    sa, sb_ = SEL[:, 0:1], SEL[:, 1:2]
    n_ = [0]

    def T(shape=(128, 64), dt=F32):
        n_[0] += 1
        return p.sb("g%d" % n_[0], list(shape), dt)

    def tt(out, a, b, op, eng="dve"):
        p.op(eng, lambda e: e.tensor_tensor(out=out[:], in0=a[:], in1=b[:], op=op), [a, b], [out])
        return out

    def ts(out, a, s1, op0, s2=None, op1=None, rd=()):
        if op1 is None:
            p.op("dve", lambda e: e.tensor_scalar(out=out[:], in0=a[:], scalar1=s1, scalar2=None, op0=op0), [a] + list(rd), [out])
        else:
            p.op("dve", lambda e: e.tensor_scalar(out=out[:], in0=a[:], scalar1=s1, scalar2=s2, op0=op0, op1=op1), [a] + list(rd), [out])
        return out

    def stt(out, a, s, b, op0, op1, rd=()):
        p.op("dve", lambda e: e.scalar_tensor_tensor(out=out[:], in0=a[:], scalar=s, in1=b[:], op0=op0, op1=op1), [a, b] + list(rd), [out])
        return out

    def act(out, a, func, scale=1.0):
        p.op("act", lambda e: e.activation(out=out[:], in_=a[:], func=func, scale=scale), [a], [out])
        return out

    dt_ = act(T(), LS, AF.Exp)
    xd = tt(T(), AR, dt_, ALU.mult)
    th = tt(T(), AI, dt_, ALU.mult)
    ki = p.sb("ki", [128, 64], I32)
    kf, m1 = T(), T()

    def reduce_(r):
        ts(kf, r, 1.0 / TWO_PI, ALU.mult)
        p.op("dve", lambda e: e.tensor_copy(out=ki[:], in_=kf[:]), [kf], [ki])
        p.op("dve", lambda e: e.tensor_copy(out=kf[:], in_=ki[:]), [ki], [kf])
        stt(r, kf, -TWO_PI, r, ALU.mult, ALU.add)
        wrap(r)

    def wrap(r):
        ts(m1, r, float(np.pi), ALU.is_gt)
        stt(r, m1, -TWO_PI, r, ALU.mult, ALU.add)
        ts(m1, r, -float(np.pi), ALU.is_lt)
        stt(r, m1, TWO_PI, r, ALU.mult, ALU.add)

    lr, li = [None] * 9, [None] * 9
    for k in range(9):
        lr[k], li[k] = T(), T()
        if k == 0:
            p.op("pool", lambda e: e.memset(lr[0][:], 1.0), [], [lr[0]])
            p.op("pool", lambda e: e.memset(li[0][:], 0.0), [], [li[0]])
            continue
        ek = act(T(), xd, AF.Exp, scale=float(k))
        ph = ts(T(), th, float(k), ALU.mult)
        reduce_(ph)
        sk = act(T(), ph, AF.Sin)
        ts(ph, ph, float(np.pi / 2), ALU.add)
        wrap(ph)
        ck = act(T(), ph, AF.Sin)
        tt(lr[k], ek, ck, ALU.mult)
        tt(li[k], ek, sk, ALU.mult)
        if k == 8:
            ek8, ck8, sk8 = ek, ck, sk
    den = tt(T(), AR, AR, ALU.mult)
    t0 = tt(T(), AI, AI, ALU.mult)
    tt(den, den, t0, ALU.add)
    p.op("dve", lambda e: e.reciprocal(out=den[:], in_=den[:]), [den], [den])
    lm1 = ts(T(), lr[1], -1.0, ALU.add)
    fr = tt(T(), lm1, AR, ALU.mult); tt(t0, li[1], AI, ALU.mult); tt(fr, fr, t0, ALU.add); tt(fr, fr, den, ALU.mult)
    fi = tt(T(), li[1], AR, ALU.mult); tt(t0, lm1, AI, ALU.mult); tt(fi, fi, t0, ALU.subtract); tt(fi, fi, den, ALU.mult)
    al, be = [None] * 8, [None] * 8
    wr, wi_, t1 = T(), T(), T()
    for k in range(8):
        tt(wr, lr[k], fr, ALU.mult); tt(t0, li[k], fi, ALU.mult); tt(wr, wr, t0, ALU.subtract)
        tt(wi_, lr[k], fi, ALU.mult); tt(t0, li[k], fr, ALU.mult); tt(wi_, wi_, t0, ALU.add)
        al[k], be[k] = T(), T()
        ts(t1, wi_, sb_, ALU.mult, rd=[SEL]); stt(al[k], wr, sa, t1, ALU.mult, ALU.add, rd=[SEL])
        ts(t1, wi_, sa, ALU.mult, rd=[SEL]); stt(be[k], wr, sb_, t1, ALU.mult, ALU.subtract, rd=[SEL])
    nlr, nli = [None] * 9, [None] * 9
    for k in range(1, 9):
        nlr[k] = ts(T(), lr[k], -1.0, ALU.mult)
        nli[k] = ts(T(), li[k], -1.0, ALU.mult)
    cst = p.sb("cst", [128, 64, 16], BF16)
    ctmp = p.sb("ctmp", [128, 64, 16], F32)
    ts(ctmp, CI, sb_, ALU.mult, rd=[SEL])
    stt(cst, CR, sa, ctmp, ALU.mult, ALU.subtract, rd=[SEL])
    Mr, Mi_ = [None] * 10, [None] * 10
    Mr[0] = ck8
    Mi_[0] = ts(T(), sk8, -1.0, ALU.mult)
    for k in range(1, 10):
        Mr[k], Mi_[k] = T(), T()
        tt(t0, Mi_[k - 1], Mi_[k - 1], ALU.mult)
        tt(Mr[k], Mr[k - 1], Mr[k - 1], ALU.mult)
        tt(Mr[k], Mr[k], t0, ALU.subtract)
        tt(Mi_[k], Mr[k - 1], Mi_[k - 1], ALU.mult)
        ts(Mi_[k], Mi_[k], 2.0, ALU.mult)

    NG = 8
    zpp = p.sb("zpp", [128, NG, 15, 16], BF16)
    p.op("pool", lambda e: e.memset(zpp[:], 0.0), [], [zpp])
    tz = [p.sb("tz%d" % i, [128, NG, 16], F32) for i in range(4)]
    Md = p.sb("Md", [128, NG, 128], BF16)
    Mi = p.sb("Mi", [128, NG, 128], BF16)
    MoR = p.sb("MoR", [128, NG, 128], BF16)
    MoI = p.sb("MoI", [128, NG, 128], BF16)
    G = p.sb("G", [64, 2, NG, NCH], F32)
    Hb = p.sb("Hb", [64, 2, NG, 512], BF16)
    Er = p.sb("Er", [64, NG, NCH], F32)
    Ei = p.sb("Ei", [64, NG, NCH], F32)
    tE = [p.sb("tE%d" % i, [64, NG, 256], F32) for i in range(2)]
    gm = [p.sb("gm%d" % i, [64, 2, NCH], F32) for i in range(2)]
    gsn = [p.sb("gsn%d" % i, [64, 2, NCH], F32) for i in range(2)]
    ta = [p.sb("ta%d" % i, [64, NCH], F32) for i in range(4)]
    uf = [p.sb("uf%d" % i, [128, NCH], F32) for i in range(1)]
    ub = [p.sb("ub%d" % i, [128, NCH], BF16) for i in range(NG)]
    yo = [p.sb("yo%d" % i, [128, 512], F32) for i in range(2)]
    ptp = p.ps("ptp", [128, 128], BF16)
    psI = p.ps("psI", [128, 128], F32)
    pg = [p.ps("pg%d" % i, [128, 512], F32) for i in range(4)]
    pgi = [0]
    zi = [0]

    for ps_ in range(64 // NG):
        g0 = ps_ * NG
        gs_ = slice(g0, g0 + NG)

        def bc(t_):
            return t_[:, gs_].unsqueeze(2).broadcast_to([128, NG, 16])

        for m in range(8):
            k = 7 - m
            p.op("dve", lambda e: e.tensor_tensor(out=tz[0][:], in0=BR[:, gs_, :], in1=bc(al[k]), op=ALU.mult), [BR, al[k]], [tz[0]])
            p.op("pool", lambda e: e.tensor_tensor(out=tz[1][:], in0=BI[:, gs_, :], in1=bc(be[k]), op=ALU.mult), [BI, be[k]], [tz[1]])
            p.op("dve", lambda e: e.tensor_tensor(out=zpp[:, :, m, :], in0=tz[0][:], in1=tz[1][:], op=ALU.add), [tz[0], tz[1]], [zpp])
        for t in range(8):
            k = t + 1
            mo_r = MoR[:, :, t * 16:(t + 1) * 16]
            mo_i = MoI[:, :, t * 16:(t + 1) * 16]
            p.op("dve", lambda e: e.tensor_tensor(out=tz[0][:], in0=CR[:, gs_, :], in1=bc(lr[k]), op=ALU.mult), [CR, lr[k]], [tz[0]])
            p.op("pool", lambda e: e.tensor_tensor(out=tz[1][:], in0=CI[:, gs_, :], in1=bc(nli[k]), op=ALU.mult), [CI, nli[k]], [tz[1]])
            p.op("dve", lambda e: e.tensor_tensor(out=mo_r, in0=tz[0][:], in1=tz[1][:], op=ALU.add), [tz[0], tz[1]], [MoR])
            p.op("pool", lambda e: e.tensor_tensor(out=tz[2][:], in0=CR[:, gs_, :], in1=bc(nli[k]), op=ALU.mult), [CR, nli[k]], [tz[2]])
            p.op("dve", lambda e: e.tensor_tensor(out=tz[3][:], in0=CI[:, gs_, :], in1=bc(nlr[k]), op=ALU.mult), [CI, nlr[k]], [tz[3]])
            p.op("pool", lambda e: e.tensor_tensor(out=mo_i, in0=tz[2][:], in1=tz[3][:], op=ALU.add), [tz[2], tz[3]], [MoI])
        for gl in range(NG):
            g = g0 + gl
            z = zpp
            zf = zpp[:, gl].rearrange("p m j -> p (m j)")
            p.op("pe", lambda e: e.transpose(out=ptp[:], in_=zf[:, 0:128], identity=IDB[:]), [z, IDB], [ptp])
            p.op("act", lambda e: e.copy(out=Md[:, gl, :], in_=ptp[:]), [ptp], [Md])
            for t in range(8):
                p.op("pe", lambda e, t=t: e.matmul(psI[:, t * 16:(t + 1) * 16], lhsT=zf[:, (7 - t) * 16:(15 - t) * 16], rhs=cst[:, g, :],
                                                  start=True, stop=True), [z, cst], [psI])
            p.op("dve", lambda e: e.scalar_tensor_tensor(out=Mi[:, gl, :], in0=IDF[:], scalar=DQ[:, g:g + 1], in1=psI[:],
                                                         op0=ALU.mult, op1=ALU.add), [IDF, DQ, psI], [Mi])
            u_f, u_b = uf[0], ub[gl]
            p.dma("sp", u_f[:], U[g], [U], [u_f])
            p.op("pool", lambda e: e.tensor_copy(out=u_b[:], in_=u_f[:]), [u_f], [u_b])
            for c in range(2):
                for (n0, nn) in ((0, 512), (512, 32)):
                    ps = pg[pgi[0] % 4]
                    pgi[0] += 1
                    p.op("pe", lambda e: e.matmul(ps[0:64, 0:nn], lhsT=Md[:, gl, c * 64:(c + 1) * 64], rhs=u_b[:, n0:n0 + nn],
                                                  start=True, stop=True), [Md, u_b], [ps])
                    p.op("act", lambda e: e.copy(out=G[:, c, gl, n0:n0 + nn], in_=ps[0:64, 0:nn]), [ps], [G])
        p.op("pool", lambda e: e.memset(Er[:, :, 0:1], 1.0), [], [Er])
        p.op("pool", lambda e: e.memset(Ei[:, :, 0:1], 0.0), [], [Ei])
        for k in range(10):
            ln = 1 << k
            cn = min(ln, NCH - ln)
            if cn <= 0:
                break
            mr = Mr[k][0:64, g0:g0 + NG].unsqueeze(2).broadcast_to([64, NG, cn])
            mi = Mi_[k][0:64, g0:g0 + NG].unsqueeze(2).broadcast_to([64, NG, cn])
            p.op("dve", lambda e: e.tensor_tensor(out=tE[0][:, :, 0:cn], in0=Ei[:, :, 0:cn], in1=mi, op=ALU.mult), [Ei, Mi_[k]], [tE[0]])
            p.op("pool", lambda e: e.tensor_tensor(out=tE[1][:, :, 0:cn], in0=Er[:, :, 0:cn], in1=mi, op=ALU.mult), [Er, Mi_[k]], [tE[1]])
            p.op("dve", lambda e: e.tensor_tensor(out=Er[:, :, ln:ln + cn], in0=Er[:, :, 0:cn], in1=mr, op=ALU.mult), [Er, Mr[k]], [Er])
            p.op("pool", lambda e: e.tensor_tensor(out=Ei[:, :, ln:ln + cn], in0=Ei[:, :, 0:cn], in1=mr, op=ALU.mult), [Ei, Mr[k]], [Ei])
            p.op("dve", lambda e: e.tensor_tensor(out=Er[:, :, ln:ln + cn], in0=Er[:, :, ln:ln + cn], in1=tE[0][:, :, 0:cn], op=ALU.subtract), [Er, tE[0]], [Er])
            p.op("pool", lambda e: e.tensor_tensor(out=Ei[:, :, ln:ln + cn], in0=Ei[:, :, ln:ln + cn], in1=tE[1][:, :, 0:cn], op=ALU.add), [Ei, tE[1]], [Ei])
        for gl in range(NG):
            g = g0 + gl
            gm_, gs_ = gm[gl % 2], gsn[gl % 2]
            er, ei = Er[:, gl, :], Ei[:, gl, :]
            gre, gim = G[:, 0, gl, :], G[:, 1, gl, :]
            p.op("dve", lambda e: e.tensor_tensor(out=ta[0][:], in0=er, in1=gre, op=ALU.mult), [Er, G], [ta[0]])
            p.op("dve", lambda e: e.tensor_tensor(out=ta[1][:], in0=ei, in1=gim, op=ALU.mult), [Ei, G], [ta[1]])
            p.op("dve", lambda e: e.tensor_tensor(out=gm_[:, 0, :], in0=ta[0][:], in1=ta[1][:], op=ALU.subtract), [ta[0], ta[1]], [gm_])
            p.op("pool", lambda e: e.tensor_tensor(out=ta[2][:], in0=er, in1=gim, op=ALU.mult), [Er, G], [ta[2]])
            p.op("pool", lambda e: e.tensor_tensor(out=ta[3][:], in0=ei, in1=gre, op=ALU.mult), [Ei, G], [ta[3]])
            p.op("pool", lambda e: e.tensor_tensor(out=gm_[:, 1, :], in0=ta[2][:], in1=ta[3][:], op=ALU.add), [ta[2], ta[3]], [gm_])
            rb = ek8[0:64, g:g + 1].broadcast_to([64, NCH])
            for c in range(2):
                p.op("dve", lambda e, c=c: e.tensor_tensor_scan(out=gs_[:, c, :], data0=rb, data1=gm_[:, c, :], initial=0.0,
                                                              op0=ALU.mult, op1=ALU.add), [gm_, ek8], [gs_])
            sl = slice(31, 543)
            p.op("dve", lambda e: e.tensor_tensor(out=ta[0][:, sl], in0=er[:, sl], in1=gs_[:, 0, sl], op=ALU.mult), [Er, gs_], [ta[0]])
            p.op("dve", lambda e: e.tensor_tensor(out=ta[1][:, sl], in0=ei[:, sl], in1=gs_[:, 1, sl], op=ALU.mult), [Ei, gs_], [ta[1]])
            p.op("dve", lambda e: e.tensor_tensor(out=Hb[:, 0, gl, :], in0=ta[0][:, sl], in1=ta[1][:, sl], op=ALU.add), [ta[0], ta[1]], [Hb])
            p.op("pool", lambda e: e.tensor_tensor(out=ta[2][:, sl], in0=er[:, sl], in1=gs_[:, 1, sl], op=ALU.mult), [Er, gs_], [ta[2]])
            p.op("pool", lambda e: e.tensor_tensor(out=ta[3][:, sl], in0=ei[:, sl], in1=gs_[:, 0, sl], op=ALU.mult), [Ei, gs_], [ta[3]])
            p.op("pool", lambda e: e.tensor_tensor(out=Hb[:, 1, gl, :], in0=ta[2][:, sl], in1=ta[3][:, sl], op=ALU.subtract), [ta[2], ta[3]], [Hb])
        for gl in range(NG):
            g = g0 + gl
            ps = pg[pgi[0] % 4]
            pgi[0] += 1
            p.op("pe", lambda e: e.matmul(ps[:, :], lhsT=Mi[:, gl, :], rhs=ub[gl][:, 32:NCH], start=True, stop=False), [Mi, ub[gl]], [ps])
            p.op("pe", lambda e: e.matmul(ps[:, :], lhsT=MoR[0:64, gl, :], rhs=Hb[:, 0, gl, :], start=False, stop=False), [MoR, Hb], [ps])
            p.op("pe", lambda e: e.matmul(ps[:, :], lhsT=MoI[0:64, gl, :], rhs=Hb[:, 1, gl, :], start=False, stop=True), [MoI, Hb], [ps])
            y_ = yo[gl % 2]
            p.op("act", lambda e: e.copy(out=y_[:], in_=ps[:, :]), [ps], [y_])
            p.dma("pool", Y[g], y_[:], [y_], [Y])
    return p.finish()


def l6_inputs(useq, d, od_a_re, od_a_im, od_log_step, od_b_re, od_b_im, od_c_re, od_c_im, od_d, with_skip):
    U = np.ascontiguousarray(f32(useq).reshape(NCH, 8, 64, 16).transpose(2, 1, 3, 0).reshape(64, 128, NCH))
    dup = lambda a: np.concatenate([a, a], axis=0)
    ar = dup(f32(od_a_re)[d].T)
    ai = dup(f32(od_a_im)[d].T)
    ls = np.broadcast_to(f32(od_log_step)[d][None, :], (128, 64))
    prm = np.ascontiguousarray(np.stack([ar, ai, ls]))
    br = dup(f32(od_b_re)[d].transpose(1, 0, 2))
    bi = dup(f32(od_b_im)[d].transpose(1, 0, 2))
    cr = dup(f32(od_c_re)[d].transpose(2, 0, 1))
    ci = dup(f32(od_c_im)[d].transpose(2, 0, 1))
    sel = np.zeros((128, 2), np.float32)
    sel[:64, 0] = 1.0
    sel[64:, 1] = 1.0
    dq = np.zeros((128, 64), np.float32)
    if with_skip:
        dq[:] = np.tile(f32(od_d).reshape(64, 16).T, (8, 1))
    return {"U": U, "prm": prm, "bri": np.ascontiguousarray(np.stack([br, bi])), "cri": np.ascontiguousarray(np.stack([cr, ci])),
            "sel": sel, "dq": dq, "identf": np.eye(128, dtype=np.float32), "ident": np.eye(128, dtype=np.float32).astype(NPBF)}


def l6_unpack(Y):
    return np.ascontiguousarray(np.asarray(Y).reshape(64, 8, 16, 512).transpose(3, 1, 0, 2).reshape(4096, 1024))


T7 = 2048


def build_l7():
    p = Prog()
    nc = p.nc
    x = p.dram("x", [T7, D], F32, "ExternalInput")
    yf = p.dram("yf", [T7, D], F32, "ExternalInput")
    yr = p.dram("yr", [T7, D], F32, "ExternalInput")
    wglu = p.dram("wglu", [D, 2048], F32, "ExternalInput")
    g1_d = p.dram("g1bc", [128, D], F32, "ExternalInput")
    g2_d = p.dram("g2bc", [128, D], F32, "ExternalInput")
    fg_d = p.dram("fgbc", [128, D], F32, "ExternalInput")
    gsT_d = p.dram("gsT", [128, 8], F32, "ExternalInput")
    shT_d = p.dram("shT", [128, 8], F32, "ExternalInput")
    wr_d = p.dram("wr", [128, 8, 8], F32, "ExternalInput")
    wge = p.dram("wge", [8, 28, 128, 8, 128], F32, "ExternalInput")
    wue = p.dram("wue", [8, 28, 128, 8, 128], F32, "ExternalInput")
    wde = p.dram("wde", [8, 3584, D], F32, "ExternalInput")
    ident_d = p.dram("ident", [128, 128], BF16, "ExternalInput")
    identf_d = p.dram("identf", [128, 128], F32, "ExternalInput")
    xm = p.dram("xm", [T7, D], F32, "Internal")
    out = p.dram("out", [T7, D], F32, "ExternalOutput")
    xmb = [Buf("xm%d" % i) for i in range(4)]
    pt = [p.ps("pt%d" % i, [128, 1024], BF16) for i in range(2)]
    ptf = p.ps("ptf", [128, 1024], F32)
    pp = [p.ps("pp%d" % i, [128, 512], F32) for i in range(4)]
    ppi = [0]

    def banks():
        ppi[0] += 1
        return pp[ppi[0] % 4]

    outer = p.es
    p.es = ExitStack()
    ident = p.sb("ident", [128, 128], BF16)
    p.dma("sp", ident[:], ident_d[:], [ident_d], [ident])
    g1 = p.sb("g1", [128, D], F32)
    p.dma("sp", g1[:], g1_d[:], [g1_d], [g1])
    stg = Stage(p, 2048)
    WG = load_w(p, stg, "WGLU", wglu, 8, 2048)
    ya = [p.sb("ya%d" % i, [128, D], F32) for i in range(2)]
    yb = [p.sb("yb%d" % i, [128, D], F32) for i in range(2)]
    y2 = p.sb("y2", [128, D], F32)
    gl = p.sb("gl", [128, D], BF16)
    glT = p.sb("glT", [128, 8, 128], BF16)
    xs = p.sb("xsA", [128, D], F32)
    sgm = p.sb("sgm", [128, 512], F32)
    ot = p.sb("ot", [128, 512], F32)
    for ti in range(T7 // 128):
        a, b_ = ya[ti % 2], yb[ti % 2]
        rs = slice(ti * 128, (ti + 1) * 128)
        p.dma("sp", a[:], yf[rs, :], [yf], [a])
        p.dma("sp", b_[:], yr[rs, :], [yr], [b_])
        p.dma("sp", xs[:], x[rs, :], [x], [xs])
        p.op("pool", lambda e: e.tensor_tensor(out=a[:], in0=a[:], in1=b_[:], op=ALU.add), [a, b_], [a])
        p.op("pool", lambda e: e.tensor_tensor(out=y2[:], in0=a[:], in1=a[:], op=ALU.mult), [a], [y2])
        p.op("dve", lambda e: e.tensor_scalar(out=y2[:], in0=y2[:], scalar1=0.044715, scalar2=1.0, op0=ALU.mult, op1=ALU.add), [y2], [y2])
        p.op("dve", lambda e: e.tensor_tensor(out=y2[:], in0=y2[:], in1=a[:], op=ALU.mult), [y2, a], [y2])
        p.op("act", lambda e: e.activation(out=y2[:], in_=y2[:], func=AF.Tanh, scale=0.7978845608028654), [y2], [y2])
        p.op("dve", lambda e: e.scalar_tensor_tensor(out=gl[:], in0=y2[:], scalar=1.0, in1=a[:], op0=ALU.add, op1=ALU.mult), [y2, a], [gl])
        ptt = pt[ti % 2]
        for c in range(8):
            p.op("pe", lambda e, c=c: e.transpose(out=ptt[:, c * 128:(c + 1) * 128], in_=gl[:, c * 128:(c + 1) * 128], identity=ident[:]), [gl, ident], [ptt])
        p.op("act", lambda e: e.copy(out=glT[:, 0:4, :], in_=ptt[:, 0:512].rearrange("p (c t) -> p c t", c=4)), [ptt], [glT])
        p.op("dve", lambda e: e.tensor_copy(out=glT[:, 4:8, :], in_=ptt[:, 512:1024].rearrange("p (c t) -> p c t", c=4)), [ptt], [glT])
        for cb in range(2):
            pa, pb = banks(), banks()
            for (ps, off) in ((pa, cb * 512), (pb, 1024 + cb * 512)):
                for c in range(8):
                    p.op("pe", lambda e, c=c, ps=ps, off=off: e.matmul(ps[:], lhsT=glT[:, c, :], rhs=WG[:, c, off:off + 512],
                                                                      start=(c == 0), stop=(c == 7)), [glT, WG], [ps])
            p.op("act", lambda e: e.activation(out=sgm[:], in_=pb[:], func=AF.Sigmoid, scale=0.5), [pb], [sgm])
            p.op("dve", lambda e: e.scalar_tensor_tensor(out=ot[:], in0=pa[:], scalar=0.5, in1=sgm[:], op0=ALU.mult, op1=ALU.mult), [pa, sgm], [ot])
            p.op("pool", lambda e: e.tensor_tensor(out=ot[:], in0=ot[:], in1=g1[:, cb * 512:(cb + 1) * 512], op=ALU.mult), [ot, g1], [ot])
            p.op("pool", lambda e: e.tensor_tensor(out=xs[:, cb * 512:(cb + 1) * 512], in0=xs[:, cb * 512:(cb + 1) * 512], in1=ot[:], op=ALU.add), [xs, ot], [xs])
        p.dma("pool", xm[rs, :], xs[:], [xs], [xmb[ti // 4]])
    p.barrier()
    p.es.close()
    p.es = ExitStack()
    identf = p.sb("identf", [128, 128], F32)
    p.dma("sp", identf[:], identf_d[:], [identf_d], [identf])
    g2 = p.sb("g2", [128, D], F32)
    fg = p.sb("fg", [128, D], F32)
    gsT = p.sb("gsT", [128, 8], F32)
    shT = p.sb("shT", [128, 8], F32)
    wr = p.sb("wr", [128, 8, 8], F32)
    for a, b_ in ((g2, g2_d), (fg, fg_d), (gsT, gsT_d), (shT, shT_d), (wr, wr_d)):
        p.dma("sp", a[:], b_[:], [b_], [a])
    eps = mk_eps(p)
    stg = Stage(p, 512, n=3, name="stgB")
    pan = Panels(p)
    NTB = 8
    actT = p.sb("actT", [128, 14, 128 * NTB], BF16)
    WDh = [p.sb("WDh%d" % i, [128, 14, 512], BF16) for i in range(2)]
    hT = p.sb("hTB", [128, 8, 128 * NTB], BF16)
    hT32 = p.sb("hT32", [128, 8, 128], F32)
    xs = p.sb("xsB", [128, NTB, D], F32)
    xn = p.sb("xnB", [128, D], F32)
    junk = p.sb("junkB", [128, D], BF16)
    ss = p.sb("ssB", [128, 4], F32)
    lg = p.sb("lg", [128, 8], F32)
    mx = p.sb("mx", [128, 8], F32)
    e1 = p.sb("e1", [128, 8], F32)
    e2 = p.sb("e2", [128, 8], F32)
    w12 = p.sb("w12", [128, 4], F32)
    comb = p.sb("comb", [128, NTB, 8], F32)
    sg = [p.sb("sgB%d" % i, [128, 512], F32) for i in range(2)]
    tm = [p.sb("tmB%d" % i, [128, 512], F32) for i in range(2)]
    ob = [p.sb("ob%d" % i, [128, D], F32) for i in range(1)]
    tmi = [0]
    wdi = [0]
    for blk in range(T7 // (128 * NTB)):
        c0 = blk * 128 * NTB
        for q4 in range(NTB // 4):
            p.dma("sp", xs[:, q4 * 4:(q4 + 1) * 4, :], xm[c0 + q4 * 512:c0 + (q4 + 1) * 512, :].rearrange("(t p) f -> p t f", p=128),
                  [xmb[(c0 + q4 * 512) // 512]], [xs])
        for ti in range(NTB):
            xt = xs[:, ti, :]
            p.op("act", lambda e: e.activation(out=junk[:], in_=xt, func=AF.Square, accum_out=ss[:, 0:1]), [xs], [junk, ss])
            p.op("act", lambda e: e.activation(out=ss[:, 1:2], in_=ss[:, 0:1], func=AF.Sqrt, scale=1.0 / D, bias=eps[:, 0:1]), [ss, eps], [ss])
            p.op("dve", lambda e: e.reciprocal(out=ss[:, 2:3], in_=ss[:, 1:2]), [ss], [ss])
            p.op("dve", lambda e: e.tensor_scalar(out=xn[:], in0=xt, scalar1=ss[:, 2:3], scalar2=None, op0=ALU.mult), [xs, ss], [xn])
            for c in range(8):
                p.op("pe", lambda e, c=c: e.transpose(out=ptf[:, c * 128:(c + 1) * 128], in_=xn[:, c * 128:(c + 1) * 128], identity=identf[:]), [xn, identf], [ptf])
            for c in range(8):
                p.op("dve", lambda e, c=c: e.tensor_scalar(out=hT32[:, c, :], in0=ptf[:, c * 128:(c + 1) * 128], scalar1=gsT[:, c:c + 1],
                                                           scalar2=shT[:, c:c + 1], op0=ALU.mult, op1=ALU.add), [ptf, gsT, shT], [hT32])
            p.op("pool", lambda e: e.tensor_copy(out=hT[:, :, ti * 128:(ti + 1) * 128], in_=hT32[:]), [hT32], [hT])
            ps = banks()
            for c in range(8):
                p.op("pe", lambda e, c=c: e.matmul(ps[:, 0:8], lhsT=hT32[:, c, :], rhs=wr[:, c, :], start=(c == 0), stop=(c == 7)), [hT32, wr], [ps])
            p.op("dve", lambda e: e.tensor_copy(out=lg[:], in_=ps[:, 0:8]), [ps], [lg])
            p.op("dve", lambda e: e.max(out=mx[:], in_=lg[:]), [lg], [mx])
            p.op("dve", lambda e: e.tensor_tensor(out=w12[:, 0:1], in0=mx[:, 0:1], in1=mx[:, 1:2], op=ALU.subtract), [mx], [w12])
            p.op("act", lambda e: e.activation(out=w12[:, 1:2], in_=w12[:, 0:1], func=AF.Sigmoid), [w12], [w12])
            p.op("dve", lambda e: e.tensor_scalar(out=w12[:, 2:3], in0=w12[:, 1:2], scalar1=-1.0, scalar2=1.0, op0=ALU.mult, op1=ALU.add), [w12], [w12])
            p.op("dve", lambda e: e.tensor_scalar(out=e1[:], in0=lg[:], scalar1=mx[:, 0:1], scalar2=w12[:, 1:2], op0=ALU.is_equal, op1=ALU.mult), [lg, mx, w12], [e1])
            p.op("dve", lambda e: e.tensor_scalar(out=e2[:], in0=lg[:], scalar1=mx[:, 1:2], scalar2=w12[:, 2:3], op0=ALU.is_equal, op1=ALU.mult), [lg, mx, w12], [e2])
            p.op("dve", lambda e: e.tensor_tensor(out=comb[:, ti, :], in0=e1[:], in1=e2[:], op=ALU.add), [e1, e2], [comb])
        for ex in range(8):
            for fh in range(2):
                wd0, wd1 = WDh[0], WDh[1]

                def ldwd(f, wd_, cb):
                    r0 = (fh * 14 + f) * 128
                    stg.load(wd_[:, f, :], wd_.b, wde[ex, r0:r0 + 128, cb * 512:(cb + 1) * 512], wde.b, 128, 512)

                ffn_phase1(p, pan, banks, hT, 128 * NTB, Tile(wge[ex], wge.b), Tile(wue[ex], wue.b), 14, actT, sg, f0=fh * 14,
                           between=lambda f: ldwd(f, wd0, 0))
                for cb in range(2):
                    wd_ = WDh[cb]
                    if cb == 1:
                        for f in range(14):
                            ldwd(f, wd1, 1)
                    for ti in range(NTB):
                        ps = banks()
                        for f in range(14):
                            p.op("pe", lambda e, f=f, ps=ps: e.matmul(ps[:], lhsT=actT[:, f, ti * 128:(ti + 1) * 128], rhs=wd_[:, f, :],
                                                                     start=(f == 0), stop=(f == 13)), [actT, wd_], [ps])
                        t = tm[tmi[0] % 2]
                        tmi[0] += 1
                        p.op("dve", lambda e: e.scalar_tensor_tensor(out=t[:], in0=ps[:], scalar=comb[:, ti, ex:ex + 1], in1=g2[:, cb * 512:(cb + 1) * 512],
                                                                     op0=ALU.mult, op1=ALU.mult), [ps, comb, g2], [t])
                        p.op("pool", lambda e: e.tensor_tensor(out=xs[:, ti, cb * 512:(cb + 1) * 512], in0=xs[:, ti, cb * 512:(cb + 1) * 512],
                                                               in1=t[:], op=ALU.add), [xs, t], [xs])
        for ti in range(NTB):
            xt = xs[:, ti, :]
            o_ = ob[ti % len(ob)]
            p.op("act", lambda e: e.activation(out=junk[:], in_=xt, func=AF.Square, accum_out=ss[:, 0:1]), [xs], [junk, ss])
            p.op("act", lambda e: e.activation(out=ss[:, 1:2], in_=ss[:, 0:1], func=AF.Sqrt, scale=1.0 / D, bias=eps[:, 0:1]), [ss, eps], [ss])
            p.op("dve", lambda e: e.reciprocal(out=ss[:, 2:3], in_=ss[:, 1:2]), [ss], [ss])
            p.op("dve", lambda e: e.scalar_tensor_tensor(out=o_[:], in0=xt, scalar=ss[:, 2:3], in1=fg[:], op0=ALU.mult, op1=ALU.mult), [xs, ss, fg], [o_])
            p.dma("pool", out[c0 + ti * 128:c0 + (ti + 1) * 128, :], o_[:], [o_], [out])
    p.es.close()
    p.es = outer
    return p.finish()


def kernel(x, c, ctx, c_ctx, mod_w, mod_b, norm1_g, norm2_g,
           ev_w_in, ev_q_norm_g, ev_w_qb, ev_kv_norm_g, ev_w_kvb, ev_na_rpb, ev_w_out,
           ev_ffn_w_gate, ev_ffn_w_up, ev_ffn_w_down,
           od_w_in, od_a_re, od_a_im, od_log_step, od_b_re, od_b_im, od_c_re, od_c_im, od_d, od_w_glu,
           moe_w_router, moe_w_gate, moe_w_up, moe_w_down, final_g):
    x = f32(x)
    ctx = f32(ctx)
    m, gs = run_adaln(c, c_ctx, mod_w, mod_b, norm1_g, norm2_g)
    identb = np.eye(128, dtype=np.float32).astype(NPBF)
    identf = np.eye(128, dtype=np.float32)
    cores = [(i // 2, i % 2) for i in range(NCORES)]

    def mods(layer, b, lo_g, lo_s):
        return (np.ascontiguousarray(np.stack([fm(gs[layer, b, lo_g:lo_g + D], 8), fm(gs[layer, 4, lo_g:lo_g + D], 8)], axis=2)),
                np.ascontiguousarray(np.stack([fm(m[layer, b, lo_s:lo_s + D], 8), fm(m[layer, 4, lo_s:lo_s + D], 8)], axis=2)))

    in2 = []
    for (b, hf) in cores:
        xtok = np.concatenate([x[b, hf * 2048:(hf + 1) * 2048], ctx[b, hf * 128:(hf + 1) * 128]], 0)
        pos = np.concatenate([np.arange(hf * 2048, (hf + 1) * 2048), -np.ones(128, np.int64)])
        in2.append(l2_inputs(xtok, pos, gs[0, b, 1024:2048], m[0, b, 0:1024], gs[0, 4, 1024:2048], m[0, 4, 0:1024],
                             ev_w_in[0], ev_q_norm_g[0], ev_w_qb[0], ev_kv_norm_g[0], ev_w_kvb[0]))
    res2 = run_prog(build_l2(), in2)
    res2 = [{k: np.asarray(v) for k, v in r.items()} for r in res2]
    in3 = [l3_inputs(b, hf, res2, ev_na_rpb[0]) for (b, hf) in cores]
    res3 = run_prog(build_l3(), in3)
    in4 = []
    for i, (b, hf) in enumerate(cores):
        gsT, shT = mods(0, b, 4096, 3072)
        in4.append({"x": in2[i]["x"], "aT": np.ascontiguousarray(np.asarray(res3[i]["AO"]).T), "wo": f32(ev_w_out[0]),
                    "wg": pretile(ev_ffn_w_gate[0]), "wu": pretile(ev_ffn_w_up[0]), "wd": f32(ev_ffn_w_down[0]),
                    "g1bc": np.stack([bc128(m[0, b, 2048:3072]), bc128(m[0, 4, 2048:3072])]),
                    "g2bc": np.stack([bc128(m[0, b, 5120:6144]), bc128(m[0, 4, 5120:6144])]),
                    "gsT": gsT, "shT": shT, "ident": identb})
    res4 = run_prog(build_l4(), in4)
    xo = [np.asarray(r["xo"]) for r in res4]
    in5 = []
    for i, (b, hf) in enumerate(cores):
        gsT, shT = mods(1, b, 1024, 0)
        in5.append({"x": xo[i], "wi": f32(od_w_in[0]), "gsT": gsT, "shT": shT, "ident": identb})
    res5 = run_prog(build_l5(), in5)
    u = [np.asarray(r["u"]) for r in res5]
    in6 = []
    for i in range(NCORES):
        b, dr = i // 2, i % 2
        u0, u1 = u[2 * b], u[2 * b + 1]
        lat = np.concatenate([u0[:2048], u1[:2048]], 0)
        cx = np.concatenate([u0[2048:], u1[2048:]], 0)
        seq = np.concatenate([cx, lat], 0) if dr == 0 else np.concatenate([cx[::-1], lat[::-1]], 0)
        in6.append(l6_inputs(seq, dr, od_a_re[0], od_a_im[0], od_log_step[0], od_b_re[0], od_b_im[0],
                             od_c_re[0], od_c_im[0], od_d[0], dr == 0))
    res6 = run_prog(build_l6(), in6)
    Y = [l6_unpack(r["Y"]) for r in res6]
    in7 = []
    wr = np.ascontiguousarray(f32(moe_w_router[0]).reshape(8, 128, 8).transpose(1, 0, 2))
    wge_t = np.stack([pretile(moe_w_gate[0][e]) for e in range(8)])
    wue_t = np.stack([pretile(moe_w_up[0][e]) for e in range(8)])
    for i, (b, hf) in enumerate(cores):
        yf = Y[2 * b][hf * 2048:(hf + 1) * 2048]
        yr = Y[2 * b + 1][::-1][hf * 2048:(hf + 1) * 2048]
        in7.append({"x": np.ascontiguousarray(xo[i][:2048]), "yf": np.ascontiguousarray(yf), "yr": np.ascontiguousarray(yr),
                    "wglu": f32(od_w_glu[0]), "g1bc": bc128(m[1, b, 2048:3072]), "g2bc": bc128(m[1, b, 5120:6144]),
                    "fgbc": bc128(final_g), "gsT": fm(gs[1, b, 4096:5120], 8), "shT": fm(m[1, b, 3072:4096], 8),
                    "wr": wr, "wge": wge_t, "wue": wue_t, "wde": f32(moe_w_down[0]),
                    "ident": identb, "identf": identf})
    res7 = run_prog(build_l7(), in7)
    out = np.zeros((B, L, D), np.float32)
    for i, (b, hf) in enumerate(cores):
        out[b, hf * 2048:(hf + 1) * 2048] = np.asarray(res7[i]["out"])
    return out
```

```python
import numpy as np
import concourse.bass as bass
import concourse.mybir as mybir
from concourse.bass_utils import run_bass_kernel_spmd
from contextlib import ExitStack
import ml_dtypes

F32 = mybir.dt.float32
BF16 = mybir.dt.bfloat16
I32 = mybir.dt.int32
U32 = mybir.dt.uint32
AF = mybir.ActivationFunctionType
ALU = mybir.AluOpType
AX = mybir.AxisListType
NPBF = ml_dtypes.bfloat16

NCORES = 8
D = 1024
B = 4
L = 4096
LC = 256
EPS = 1e-6


class Buf:
    __slots__ = ("w", "rs", "name")

    def __init__(self, name=""):
        self.w = None
        self.rs = {}
        self.name = name


class Tile:
    __slots__ = ("t", "b")

    def __init__(self, t, b):
        self.t = t
        self.b = b

    def __getitem__(self, idx):
        return self.t[idx]


COMPUTE = ("pe", "act", "dve", "pool")


class View:
    def __init__(self, ap, b):
        self.ap = ap
        self.b = b

    def __getitem__(self, idx):
        return self.ap


class Prog:
    def __init__(self, ndma=8):
        self.nc = bass.Bass("TRN2", target_bir_lowering=False)
        nc = self.nc
        self.es = ExitStack()
        self.eng = {"pe": nc.tensor, "act": nc.scalar, "dve": nc.vector,
                    "pool": nc.gpsimd, "sp": nc.sync}
        self.sems = {}
        self.cnt = {}
        for e in COMPUTE:
            self.sems[e] = self.es.enter_context(nc.semaphore("c_" + e))
            self.cnt[e] = 0
        self.waited = {e: {} for e in self.eng}
        self.dpool = {}
        self.didx = {}
        self.dval = {}
        for q in ("sp", "act", "pool"):
            n = ndma if q == "sp" else 4
            self.dpool[q] = []
            for i in range(n):
                k = ("d", q, i)
                self.sems[k] = self.es.enter_context(nc.semaphore("d_%s_%d" % (q, i)))
                self.dval[k] = 0
                self.dpool[q].append(k)
            self.didx[q] = 0
        self.ndram = 0

    def sb(self, name, shape, dt):
        t = self.es.enter_context(self.nc.sbuf_tensor("s_" + name, list(shape), dt))
        return Tile(t, Buf(name))

    def ps(self, name, shape, dt):
        t = self.es.enter_context(self.nc.psum_tensor("p_" + name, list(shape), dt))
        return Tile(t, Buf(name))

    def dram(self, name, shape, dt, kind):
        t = self.nc.dram_tensor(name, list(shape), dt, kind=kind)
        return Tile(t.ap(), Buf(name))

    def _wait(self, e, reads, writes):
        need = {}
        for b in reads:
            if b.w is not None:
                k, v = b.w
                if not (k == e and e == "pe"):
                    need[k] = max(need.get(k, 0), v)
        for b in writes:
            if b.w is not None:
                k, v = b.w
                if not (k == e and e == "pe"):
                    need[k] = max(need.get(k, 0), v)
            for k, v in b.rs.items():
                if not (k == e and e == "pe"):
                    need[k] = max(need.get(k, 0), v)
        w = self.waited[e]
        for k, v in need.items():
            if w.get(k, 0) < v:
                self.eng[e].wait_ge(self.sems[k], v)
                w[k] = v

    def _mark(self, tok, reads, writes):
        k, v = tok
        for b in reads:
            if b.rs.get(k, 0) < v:
                b.rs[k] = v
        for b in writes:
            b.w = tok
            b.rs = {}

    def op(self, e, fn, reads=(), writes=()):
        reads = [r.b if isinstance(r, (Tile, View)) else r for r in reads]
        writes = [r.b if isinstance(r, (Tile, View)) else r for r in writes]
        self._wait(e, reads, writes)
        inst = fn(self.eng[e])
        self.cnt[e] += 1
        inst.then_inc(self.sems[e], 1)
        self._mark((e, self.cnt[e]), reads, writes)

    def dma(self, q, out, in_, reads=(), writes=(), **kw):
        reads = [r.b if isinstance(r, (Tile, View)) else r for r in reads]
        writes = [r.b if isinstance(r, (Tile, View)) else r for r in writes]
        pool = self.dpool[q]
        k = pool[self.didx[q] % len(pool)]
        self.didx[q] += 1
        pv = self.dval[k]
        w = self.waited[q]
        if pv and w.get(k, 0) < pv:
            self.eng[q].wait_ge(self.sems[k], pv)
            w[k] = pv
        self._wait(q, reads, writes)
        self.eng[q].dma_start(out=out, in_=in_, **kw).then_inc(self.sems[k], 16)
        self.dval[k] = pv + 16
        self._mark((k, pv + 16), reads, writes)

    def barrier(self):
        for e in self.eng:
            w = self.waited[e]
            for k in COMPUTE:
                v = self.cnt[k]
                if v and k != e and w.get(k, 0) < v:
                    self.eng[e].wait_ge(self.sems[k], v)
                    w[k] = v
            for k, v in self.dval.items():
                if v and w.get(k, 0) < v:
                    self.eng[e].wait_ge(self.sems[k], v)
                    w[k] = v

    def finish(self):
        for k, v in self.dval.items():
            if v and self.waited["sp"].get(k, 0) < v:
                self.nc.sync.wait_ge(self.sems[k], v)
        self.es.close()
        return self.nc


TIMES = []


def run_prog(nc, in_maps, trace=False):
    res = run_bass_kernel_spmd(nc, in_maps, core_ids=list(range(len(in_maps))), trace=trace)
    if trace:
        TIMES.append(res.exec_time_ns)
    return res.results


def f32(a):
    return np.ascontiguousarray(a, dtype=np.float32)


def build_adaln():
    p = Prog()
    cond = p.dram("cond", [128, 8, 5], F32, "ExternalInput")
    w = p.dram("w", [2, 1024, 768], F32, "ExternalInput")
    bias5 = p.dram("bias5", [2, 5, 768], F32, "ExternalInput")
    gain5 = p.dram("gain5", [2, 5, 768], F32, "ExternalInput")
    m_out = p.dram("m", [2, 5, 768], F32, "ExternalOutput")
    gs_out = p.dram("gs", [2, 5, 768], F32, "ExternalOutput")
    ct = p.sb("ct", [128, 8, 5], F32)
    st = p.sb("st", [128, 8, 5], F32)
    p.dma("sp", ct[:], cond[:], [cond], [ct])
    p.op("act", lambda e: e.activation(out=st[:], in_=ct[:], func=AF.Silu), [ct], [st])
    pss = [p.ps("ps%d" % i, [128, 512], F32) for i in range(2)]
    for l in range(2):
        wt = p.sb("wt%d" % l, [128, 8, 768], F32)
        bt = p.sb("bt%d" % l, [5, 768], F32)
        gt = p.sb("gt%d" % l, [5, 768], F32)
        mt = p.sb("mt%d" % l, [5, 768], F32)
        gst = p.sb("gst%d" % l, [5, 768], F32)
        p.dma("sp", wt[:], w[l].rearrange("(c p) n -> p c n", p=128), [w], [wt])
        p.dma("sp", bt[:], bias5[l], [bias5], [bt])
        p.dma("sp", gt[:], gain5[l], [gain5], [gt])
        for nb in range(2):
            ps = pss[nb]
            cs = slice(nb * 384, (nb + 1) * 384)
            for c in range(8):
                p.op("pe", lambda e, c=c, cs=cs, ps=ps: e.matmul(
                    ps[0:5, 0:384], lhsT=st[:, c, :], rhs=wt[:, c, cs],
                    start=(c == 0), stop=(c == 7)), [st, wt], [ps])
            p.op("dve", lambda e, cs=cs, ps=ps: e.tensor_tensor(
                out=mt[:, cs], in0=ps[0:5, 0:384], in1=bt[:, cs], op=ALU.add), [ps, bt], [mt])
        p.op("dve", lambda e: e.scalar_tensor_tensor(
            out=gst[:], in0=mt[:], scalar=1.0, in1=gt[:], op0=ALU.add, op1=ALU.mult), [mt, gt], [gst])
        p.dma("sp", m_out[l], mt[:], [mt], [m_out])
        p.dma("sp", gs_out[l], gst[:], [gst], [gs_out])
    return p.finish()


def run_adaln(c, c_ctx, mod_w, mod_b, norm1_g, norm2_g):
    cond_all = np.concatenate([f32(c), f32(c_ctx)[None]], 0)
    condT = np.ascontiguousarray(cond_all.T.reshape(8, 128, 5).transpose(1, 0, 2))
    gain = np.zeros((2, 6144), np.float32)
    gain[:, 1024:2048] = f32(norm1_g)
    gain[:, 4096:5120] = f32(norm2_g)
    in_maps = []
    for j in range(NCORES):
        cs = slice(768 * j, 768 * j + 768)
        in_maps.append({
            "cond": condT,
            "w": np.ascontiguousarray(f32(mod_w)[:, :, cs]),
            "bias5": np.ascontiguousarray(np.broadcast_to(f32(mod_b)[:, None, cs], (2, 5, 768))),
            "gain5": np.ascontiguousarray(np.broadcast_to(gain[:, None, cs], (2, 5, 768))),
        })
    res = run_prog(build_adaln(), in_maps)
    m = np.concatenate([r["m"] for r in res], axis=2)
    gs = np.concatenate([r["gs"] for r in res], axis=2)
    return m, gs


class Stage:
    def __init__(self, p, width, n=2, name="stg"):
        self.p = p
        self.slots = [p.sb("%s%d" % (name, i), [128, width], F32) for i in range(n)]
        self.i = 0
        self.ce = 0

    def load(self, dst_ap, dst_buf, src_ap, src_buf, rows, n, eng=None, scale=None):
        p = self.p
        s = self.slots[self.i % len(self.slots)]
        self.i += 1
        p.dma("sp", s[0:rows, 0:n], src_ap, [src_buf], [s])
        if scale is not None:
            sc_ap, sc_t = scale
            p.op("dve", lambda e: e.tensor_scalar(out=dst_ap, in0=s[0:rows, 0:n], scalar1=sc_ap, scalar2=None,
                                                  op0=ALU.mult), [s, sc_t], [dst_buf])
            return
        if eng is None:
            eng = ("pool", "dve")[self.ce % 2]
            self.ce += 1
        p.op(eng, lambda e: e.tensor_copy(out=dst_ap, in_=s[0:rows, 0:n]), [s], [dst_buf])


def load_w(p, stg, name, src, kc, n, eng=None, rowscale=None):
    dst = p.sb(name, [128, kc, n], BF16)
    for c in range(kc):
        sc = None if rowscale is None else (rowscale[:, c:c + 1], rowscale)
        stg.load(dst[:, c, :], dst.b, src[c * 128:(c + 1) * 128, :], src.b, 128, n, eng, sc)
    return dst


class NormT:
    def __init__(self, p, ident, nslots=2):
        self.p = p
        self.ident = ident
        self.xt = [p.sb("nx%d" % i, [128, 1024], F32) for i in range(nslots)]
        self.xn = [p.sb("nn%d" % i, [128, 1024], BF16) for i in range(nslots)]
        self.junk = p.sb("njunk", [128, 1024], BF16)
        self.ss = [p.sb("nss%d" % i, [128, 2], F32) for i in range(nslots)]
        self.pt = [p.ps("npt%d" % i, [128, 1024], BF16) for i in range(2)]
        self.i = 0
        self.eps = mk_eps(p)

    def run(self, x_ap, x_buf, gsT, shT, cond, hT, col0, x_loaded=None):
        p = self.p
        k = self.i % len(self.xt)
        self.i += 1
        xt, xn, ss, pt = self.xt[k], self.xn[k], self.ss[k], self.pt[k % 2]
        if x_loaded is None:
            p.dma("sp", xt[:], x_ap, [x_buf], [xt])
        else:
            xt = x_loaded
        p.op("act", lambda e: e.activation(out=self.junk[:], in_=xt[:], func=AF.Square,
                                           accum_out=ss[:, 0:1]), [xt], [self.junk, ss])
        p.op("act", lambda e: e.activation(out=ss[:, 1:2], in_=ss[:, 0:1], func=AF.Sqrt,
                                           scale=1.0 / D, bias=self.eps[:, 0:1]), [ss, self.eps], [ss])
        p.op("dve", lambda e: e.reciprocal(out=ss[:, 0:1], in_=ss[:, 1:2]), [ss], [ss])
        p.op("dve", lambda e: e.tensor_scalar(out=xn[:], in0=xt[:], scalar1=ss[:, 0:1], scalar2=None,
                                              op0=ALU.mult), [xt, ss], [xn])
        for c in range(8):
            p.op("pe", lambda e, c=c: e.transpose(out=pt[:, c * 128:(c + 1) * 128],
                                                   in_=xn[:, c * 128:(c + 1) * 128],
                                                   identity=self.ident[:]), [xn, self.ident], [pt])
        for c in range(8):
            p.op("dve" if c % 2 == 0 else "act",
                 (lambda e, c=c: e.tensor_scalar(out=hT[:, c, col0:col0 + 128], in0=pt[:, c * 128:(c + 1) * 128],
                                                 scalar1=gsT[:, c, cond:cond + 1], scalar2=shT[:, c, cond:cond + 1],
                                                 op0=ALU.mult, op1=ALU.add)) if c % 2 == 0 else
                 (lambda e, c=c: e.activation(out=hT[:, c, col0:col0 + 128], in_=pt[:, c * 128:(c + 1) * 128],
                                              func=AF.Identity, scale=gsT[:, c, cond:cond + 1],
                                              bias=shT[:, c, cond:cond + 1])),
                 [pt, gsT, shT], [hT])


def mk_eps(p, val=EPS):
    t = p.sb("epsc", [128, 1], F32)
    p.op("pool", lambda e: e.memset(t[:], val), [], [t])
    return t


T2 = 2176
BLK2 = [(0, 512), (512, 512), (1024, 512), (1536, 512), (2048, 128)]
NA_SCALE = 64 ** -0.5
MLA_SCALE = 96 ** -0.5


def build_l2(stop=None):
    p = Prog()
    x = p.dram("x", [T2, D], F32, "ExternalInput")
    w1 = p.dram("w1", [D, 2368], F32, "ExternalInput")
    wq = p.dram("wq", [384, 1536], F32, "ExternalInput")
    wkv = p.dram("wkv", [256, 1024], F32, "ExternalInput")
    cos_d = p.dram("cos96", [96, T2], F32, "ExternalInput")
    sin_d = p.dram("sin96", [96, T2], F32, "ExternalInput")
    gsT_d = p.dram("gsT", [128, 8, 2], F32, "ExternalInput")
    shT_d = p.dram("shT", [128, 8, 2], F32, "ExternalInput")
    gq_d = p.dram("gq", [128, 3], F32, "ExternalInput")
    gkv_d = p.dram("gkv", [128, 2], F32, "ExternalInput")
    ident_d = p.dram("ident", [128, 128], BF16, "ExternalInput")
    QT = p.dram("QT", [96, 8, T2], BF16, "ExternalOutput")
    KT = p.dram("KT", [96, 8, T2], BF16, "ExternalOutput")
    V = p.dram("V", [T2, 512], BF16, "ExternalOutput")
    NQT = p.dram("NQT", [64, 8, T2], BF16, "ExternalOutput")
    NKT = p.dram("NKT", [64, 8, T2], BF16, "ExternalOutput")
    NV = p.dram("NV", [T2, 512], BF16, "ExternalOutput")

    ident = p.sb("ident", [128, 128], BF16)
    p.dma("sp", ident[:], ident_d[:], [ident_d], [ident])
    ones = p.sb("ones", [128, 128], BF16)
    p.op("pool", lambda e: e.memset(ones[:], 1.0), [], [ones])
    cos = p.sb("cos", [96, T2], F32)
    sin = p.sb("sin", [96, T2], F32)
    p.dma("sp", cos[:], cos_d[:], [cos_d], [cos])
    p.dma("sp", sin[:], sin_d[:], [sin_d], [sin])
    gsT = p.sb("gsT", [128, 8, 2], F32)
    shT = p.sb("shT", [128, 8, 2], F32)
    gq = p.sb("gq", [128, 3], F32)
    gkv = p.sb("gkv", [128, 2], F32)
    for a, b_ in ((gsT, gsT_d), (shT, shT_d), (gq, gq_d), (gkv, gkv_d)):
        p.dma("sp", a[:], b_[:], [b_], [a])
    stg = Stage(p, 2368)
    W1 = load_w(p, stg, "W1", w1, 8, 2368)
    WQ = load_w(p, stg, "WQ", wq, 3, 1536, rowscale=gq)
    WKV = load_w(p, stg, "WKV", wkv, 2, 1024, rowscale=gkv)
    O_CQ, O_CKV, O_KR, O_KRR, O_NQ, O_NK, O_NV = 0, 384, 640, 736, 832, 1344, 1856
    nt = NormT(p, ident)
    eps = nt.eps
    hT = p.sb("hT", [128, 8, 512], BF16)
    cqg = p.sb("cqg", [128, 3, 512], BF16)
    sqq = p.sb("sqq", [128, 3, 512], BF16)
    ckvg = p.sb("ckvg", [128, 2, 512], BF16)
    sqkv = p.sb("sqkv", [128, 2, 512], BF16)
    rq = p.sb("rq", [128, 512], F32)
    rkv = p.sb("rkv", [128, 512], F32)
    rtok = p.sb("rtok", [128, 8], F32)
    tmpa = [p.sb("tmpa%d" % i, [128, 512], F32) for i in range(2)]
    tmpb = [p.sb("tmpb%d" % i, [128, 512], F32) for i in range(2)]
    krt = p.sb("krt", [96, 512], BF16)
    Qb = p.sb("Qb", [96, 8, 512], BF16)
    Kb = p.sb("Kb", [96, 8, 512], BF16)
    NQb = p.sb("NQb", [64, 8, 512], BF16)
    NKb = p.sb("NKb", [64, 8, 512], BF16)
    Vb = p.sb("Vb", [128, 4, 512], BF16)
    NVb = p.sb("NVb", [128, 4, 512], BF16)
    pp = [p.ps("pp%d" % i, [128, 512], F32) for i in range(6)]
    ppi = [0]

    def bank():
        ppi[0] += 1
        return pp[ppi[0] % 6]

    def mm(ps_ap, ps_t, pairs, rd):
        n = len(pairs)
        for i, (l, r) in enumerate(pairs):
            p.op("pe", lambda e, l=l, r=r, i=i: e.matmul(ps_ap, lhsT=l, rhs=r, start=(i == 0), stop=(i == n - 1)),
                 rd, [ps_t])

    for (c0, n) in BLK2:
        ntile = n // 128
        for ti in range(ntile):
            t0 = c0 + ti * 128
            cond = 0 if t0 < 2048 else 1
            nt.run(x[t0:t0 + 128, :], x.b, gsT, shT, cond, hT, ti * 128)
        if stop == 'norm':
            break
        for (dst, sq, g, off, nch, rbc, dim) in ((cqg, sqq, gq, O_CQ, 3, rq, 384), (ckvg, sqkv, gkv, O_CKV, 2, rkv, 256)):
            for c3 in range(nch):
                ps = bank()
                mm(ps[:, 0:n], ps, [(W1[:, c, off + c3 * 128: off + (c3 + 1) * 128], hT[:, c, 0:n]) for c in range(8)], [W1, hT])
                if stop == 'cq_mm':
                    continue
                p.op("dve", lambda e, ps=ps, c3=c3, dst=dst: e.tensor_copy(out=dst[:, c3, 0:n], in_=ps[:, 0:n]), [ps], [dst])
                p.op("act", lambda e, c3=c3, sq=sq, dst=dst: e.activation(out=sq[:, c3, 0:n], in_=dst[:, c3, 0:n], func=AF.Square), [dst], [sq])
            if stop in ('cq_mm', 'cq_dve', 'cq_act', 'cq_act2'):
                continue
            ps = bank()
            mm(ps[:, 0:n], ps, [(ones[:], sq[:, c3, 0:n]) for c3 in range(nch)], [ones, sq])
            if stop == 'cq_ones':
                continue
            p.op("act", lambda e, ps=ps, rbc=rbc, dim=dim: e.activation(out=rbc[:, 0:n], in_=ps[:, 0:n], func=AF.Sqrt,
                                                                      scale=1.0 / dim, bias=eps[:, 0:1]), [ps, eps], [rbc])
            p.op("dve", lambda e, rbc=rbc: e.reciprocal(out=rbc[:, 0:n], in_=rbc[:, 0:n]), [rbc], [rbc])
        if stop in ('cq', 'cq_mm', 'cq_dve', 'cq_act', 'cq_ones', 'cq_act2'):
            break
        ps = bank()
        for ti in range(ntile):
            mm(ps[:, ti:ti + 1], ps, [(sqkv[:, c2, ti * 128:(ti + 1) * 128], ones[:, 0:1]) for c2 in range(2)], [sqkv, ones])
        p.op("act", lambda e, ps=ps: e.activation(out=rtok[:, 0:ntile], in_=ps[:, 0:ntile], func=AF.Sqrt,
                                                  scale=1.0 / 256, bias=eps[:, 0:1]), [ps, eps], [rtok])
        p.op("dve", lambda e: e.reciprocal(out=rtok[:, 0:ntile], in_=rtok[:, 0:ntile]), [rtok], [rtok])
        if stop == 'rtok':
            break
        def rope(pa, pb, out_ap, out_t, scale_ap=None, scale_t=None, k=0):
            ta, tb = tmpa[k % 2], tmpb[k % 2]
            p.op("dve", lambda e: e.tensor_tensor(out=ta[0:96, 0:n], in0=pa[0:96, 0:n], in1=cos[:, c0:c0 + n], op=ALU.mult), [pa, cos], [ta])
            p.op("dve", lambda e: e.tensor_tensor(out=tb[0:96, 0:n], in0=pb[0:96, 0:n], in1=sin[:, c0:c0 + n], op=ALU.mult), [pb, sin], [tb])
            if scale_ap is None:
                p.op("pool", lambda e: e.tensor_tensor(out=out_ap, in0=ta[0:96, 0:n], in1=tb[0:96, 0:n], op=ALU.add), [ta, tb], [out_t])
            else:
                p.op("pool", lambda e: e.tensor_tensor(out=ta[0:96, 0:n], in0=ta[0:96, 0:n], in1=tb[0:96, 0:n], op=ALU.add), [ta, tb], [ta])
                p.op("pool", lambda e: e.tensor_tensor(out=out_ap, in0=ta[0:96, 0:n], in1=scale_ap, op=ALU.mult), [ta, scale_t], [out_t])
        for h in range(8):
            pa, pb = bank(), bank()
            mm(pa[0:96, 0:n], pa, [(WQ[:, c3, h * 96:(h + 1) * 96], cqg[:, c3, 0:n]) for c3 in range(3)], [WQ, cqg])
            mm(pb[0:96, 0:n], pb, [(WQ[:, c3, 768 + h * 96: 768 + (h + 1) * 96], cqg[:, c3, 0:n]) for c3 in range(3)], [WQ, cqg])
            rope(pa, pb, Qb[:, h, 0:n], Qb, rq[0:96, 0:n], rq, k=h)
        if stop == 'q':
            break
        pa, pb = bank(), bank()
        mm(pa[0:96, 0:n], pa, [(W1[:, c, O_KR:O_KR + 96], hT[:, c, 0:n]) for c in range(8)], [W1, hT])
        mm(pb[0:96, 0:n], pb, [(W1[:, c, O_KRR:O_KRR + 96], hT[:, c, 0:n]) for c in range(8)], [W1, hT])
        rope(pa, pb, krt[:, 0:n], krt)
        if stop == 'kr':
            break
        for h in range(8):
            ps = bank()
            mm(ps[0:64, 0:n], ps, [(WKV[:, c2, h * 64:(h + 1) * 64], ckvg[:, c2, 0:n]) for c2 in range(2)], [WKV, ckvg])
            p.op("dve", lambda e, ps=ps, h=h: e.tensor_tensor(out=Kb[0:64, h, 0:n], in0=ps[0:64, 0:n], in1=rkv[0:64, 0:n], op=ALU.mult), [ps, rkv], [Kb])
            p.op("pool", lambda e, h=h: e.tensor_copy(out=Kb[64:96, h, 0:n], in_=krt[64:96, 0:n]), [krt], [Kb])
        for ti in range(ntile):
            ps = bank()
            mm(ps[:, :], ps, [(ckvg[:, c2, ti * 128:(ti + 1) * 128], WKV[:, c2, 512:1024]) for c2 in range(2)], [WKV, ckvg])
            p.op("dve", lambda e, ps=ps, ti=ti: e.tensor_scalar(out=Vb[:, ti, :], in0=ps[:, :], scalar1=rtok[:, ti:ti + 1], scalar2=None, op0=ALU.mult), [ps, rtok], [Vb])
        if stop == 'kv':
            break
        for h in range(8):
            ps = bank()
            mm(ps[0:64, 0:n], ps, [(W1[:, c, O_NQ + h * 64:O_NQ + (h + 1) * 64], hT[:, c, 0:n]) for c in range(8)], [W1, hT])
            p.op("act", lambda e, ps=ps, h=h: e.mul(out=NQb[:, h, 0:n], in_=ps[0:64, 0:n], mul=NA_SCALE), [ps], [NQb])
            ps = bank()
            mm(ps[0:64, 0:n], ps, [(W1[:, c, O_NK + h * 64:O_NK + (h + 1) * 64], hT[:, c, 0:n]) for c in range(8)], [W1, hT])
            p.op("dve", lambda e, ps=ps, h=h: e.tensor_copy(out=NKb[:, h, 0:n], in_=ps[0:64, 0:n]), [ps], [NKb])
        for ti in range(ntile):
            ps = bank()
            mm(ps[:, :], ps, [(hT[:, c, ti * 128:(ti + 1) * 128], W1[:, c, O_NV:O_NV + 512]) for c in range(8)], [W1, hT])
            p.op("act", lambda e, ps=ps, ti=ti: e.copy(out=NVb[:, ti, :], in_=ps[:, :]), [ps], [NVb])
        if stop == 'na':
            break
        p.dma("pool", QT[:, :, c0:c0 + n], Qb[:, :, 0:n], [Qb], [QT])
        p.dma("pool", KT[:, :, c0:c0 + n], Kb[:, :, 0:n], [Kb], [KT])
        p.dma("pool", NQT[:, :, c0:c0 + n], NQb[:, :, 0:n], [NQb], [NQT])
        p.dma("pool", NKT[:, :, c0:c0 + n], NKb[:, :, 0:n], [NKb], [NKT])
        p.dma("pool", V[c0:c0 + n, :].rearrange("(t p) f -> p t f", p=128), Vb[:, 0:ntile, :], [Vb], [V])
        p.dma("pool", NV[c0:c0 + n, :].rearrange("(t p) f -> p t f", p=128), NVb[:, 0:ntile, :], [NVb], [NV])
    return p.finish()


def rope_tables(pos):
    T = len(pos)
    cos = np.ones((96, T), np.float64)
    sin = np.zeros((96, T), np.float64)
    invf = 10000.0 ** (-np.arange(8) / 8.0)
    valid = pos >= 0
    row = (pos // 64).astype(np.float64)
    col = (pos % 64).astype(np.float64)
    for j in range(32):
        pp_ = row if j < 16 else col
        ang = (pp_.astype(np.float32) * invf[j % 8].astype(np.float32)).astype(np.float64)
        cj = np.where(valid, np.cos(ang), 1.0)
        sj = np.where(valid, np.sin(ang), 0.0)
        cos[64 + j] = cj
        sin[64 + j] = -sj if (j % 16) < 8 else sj
    return cos.astype(np.float32), sin.astype(np.float32)


ROPE_PERM = np.array([j + 8 if (j % 16) < 8 else j - 8 for j in range(32)])


def fm(vec, nch):
    return np.ascontiguousarray(f32(vec).reshape(nch, 128).T)


def l2_inputs(xtok, pos, gs_lat, sh_lat, gs_ctx, sh_ctx, ev_w_in, q_norm_g, w_qb, kv_norm_g, w_kvb):
    w_in = f32(ev_w_in)
    kr = w_in[:, 640:672]
    z64 = np.zeros((D, 64), np.float32)
    w1 = np.concatenate([w_in[:, 0:640], z64, kr, z64, kr[:, ROPE_PERM], w_in[:, 672:2208]], axis=1)
    wqb = f32(w_qb).reshape(384, 8, 96)
    wq_rot = np.concatenate([np.zeros((384, 8, 64), np.float32), wqb[:, :, 64:][:, :, ROPE_PERM]], axis=2)
    wq = np.concatenate([wqb.reshape(384, 768), wq_rot.reshape(384, 768)], axis=1)
    wkvb = f32(w_kvb).reshape(256, 8, 128)
    wkv = np.concatenate([wkvb[:, :, :64].reshape(256, 512), wkvb[:, :, 64:].reshape(256, 512)], axis=1)
    cos96, sin96 = rope_tables(pos)
    ident = np.eye(128, dtype=np.float32).astype(NPBF)
    return {
        "x": f32(xtok), "w1": np.ascontiguousarray(w1), "wq": np.ascontiguousarray(wq), "wkv": np.ascontiguousarray(wkv),
        "cos96": cos96, "sin96": sin96,
        "gsT": np.ascontiguousarray(np.stack([fm(gs_lat, 8), fm(gs_ctx, 8)], axis=2)),
        "shT": np.ascontiguousarray(np.stack([fm(sh_lat, 8), fm(sh_ctx, 8)], axis=2)),
        "gq": fm(q_norm_g, 3), "gkv": fm(kv_norm_g, 2), "ident": ident,
    }


NKEY = 4352
NAK = 40 * 64 + 256


def build_l3():
    p = Prog()
    QT = p.dram("QT", [96, 8, T2], BF16, "ExternalInput")
    KT = p.dram("KT", [96, 8, NKEY], BF16, "ExternalInput")
    VA = p.dram("VA", [NKEY, 8, 65], BF16, "ExternalInput")
    NQT = p.dram("NQT", [64, 8, T2], BF16, "ExternalInput")
    NKT = p.dram("NKT", [64, 8, NAK], BF16, "ExternalInput")
    NVA = p.dram("NVA", [NAK, 8, 65], BF16, "ExternalInput")
    NB = p.dram("NB", [8, 128, 18, 256], F32, "ExternalInput")
    AO = p.dram("AO", [T2, D], BF16, "ExternalOutput")
    attn = p.sb("attn", [128, 17, D], BF16)
    S = [p.ps("S%d" % i, [128, 512], F32) for i in range(2)]
    O = [p.ps("O%d" % i, [128, 512], F32) for i in range(4)]
    PT = [p.sb("PT%d" % i, [128, 512], BF16) for i in range(3)]
    rden = [p.sb("rden%d" % i, [128, 1], F32) for i in range(4)]
    tmp = [p.sb("tmpf%d" % i, [128, 256], F32) for i in range(2)]
    cnt = {"s": 0, "pt": 0, "tm": 0}

    def attend(kt, ktoff, q, q0, nq, keytiles, v, scale, bias=None, tile0=0, col0=0):
        nqs = nq // 128
        nk = len(keytiles)

        def score(i):
            kb, bj = keytiles[i]
            ps = S[cnt["s"] % 2]
            cnt["s"] += 1
            pt = PT[cnt["pt"] % 3]
            cnt["pt"] += 1
            p.op("pe", lambda e: e.matmul(ps[:, 0:nq], lhsT=kt[:, kb * 128:(kb + 1) * 128], rhs=q[:, q0:q0 + nq],
                                          start=True, stop=True), [kt, q], [ps])
            if bj is None:
                p.op("act", lambda e: e.activation(out=pt[:, 0:nq], in_=ps[:, 0:nq], func=AF.Exp, scale=scale), [ps], [pt])
            else:
                tm = tmp[cnt["tm"] % 2]
                cnt["tm"] += 1
                p.op("dve", lambda e: e.tensor_tensor(out=tm[:, 0:nq], in0=ps[:, 0:nq], in1=bias[:, bj, 0:nq], op=ALU.add), [ps, bias], [tm])
                p.op("act", lambda e: e.activation(out=pt[:, 0:nq], in_=tm[:, 0:nq], func=AF.Exp, scale=scale), [tm], [pt])
            return pt

        pts = [score(0)]
        for i, (kb, bj) in enumerate(keytiles):
            if i + 1 < nk:
                pts.append(score(i + 1))
            pt = pts[i]
            for qs in range(nqs):
                p.op("pe", lambda e, qs=qs: e.matmul(O[qs][:, 0:65], lhsT=pt[:, qs * 128:(qs + 1) * 128], rhs=v[:, kb, :],
                                                     start=(i == 0), stop=(i == nk - 1)), [pt, v], [O[qs]])
        for qs in range(nqs):
            p.op("dve", lambda e, qs=qs: e.reciprocal(out=rden[qs][:], in_=O[qs][:, 64:65]), [O[qs]], [rden[qs]])
            p.op("dve", lambda e, qs=qs: e.tensor_scalar(out=attn[:, tile0 + qs, col0:col0 + 64], in0=O[qs][:, 0:64],
                                                         scalar1=rden[qs][:, 0:1], scalar2=None, op0=ALU.mult),
                 [O[qs], rden[qs]], [attn])

    kth = [p.sb("kth%d" % i, [96, NKEY], BF16) for i in range(2)]
    vh = [p.sb("vh%d" % i, [128, 34, 65], BF16) for i in range(2)]
    qh = [p.sb("qh%d" % i, [96, T2], BF16) for i in range(2)]
    for h in range(8):
        k_, v_, q_ = kth[h % 2], vh[h % 2], qh[h % 2]
        p.dma("sp", k_[:], KT[:, h, :], [KT], [k_])
        p.dma("sp", v_[:], VA[:, h, :].rearrange("(t p) f -> p t f", p=128), [VA], [v_])
        p.dma("sp", q_[:], QT[:, h, :], [QT], [q_])
        for qb in range(4):
            attend(k_, 0, q_, qb * 512, 512, [(kb, None) for kb in range(34)], v_, MLA_SCALE, tile0=qb * 4, col0=h * 64)
        attend(k_, 0, q_, 2048, 128, [(32, None), (33, None)], v_, MLA_SCALE, tile0=16, col0=h * 64)
    nkh = [p.sb("nkh%d" % i, [64, NAK], BF16) for i in range(2)]
    nvh = [p.sb("nvh%d" % i, [128, 22, 65], BF16) for i in range(2)]
    nqh = [p.sb("nqh%d" % i, [64, T2], BF16) for i in range(2)]
    nbh = [p.sb("nbh%d" % i, [128, 18, 256], F32) for i in range(2)]
    for h in range(8):
        k_, v_, q_, b_ = nkh[h % 2], nvh[h % 2], nqh[h % 2], nbh[h % 2]
        p.dma("sp", k_[:], NKT[:, h, :], [NKT], [k_])
        p.dma("sp", v_[:], NVA[:, h, :].rearrange("(t p) f -> p t f", p=128), [NVA], [v_])
        p.dma("sp", q_[:], NQT[:, h, :], [NQT], [q_])
        p.dma("sp", b_[:], NB[h], [NB], [b_])
        for qt in range(8):
            slot = 0 if qt == 0 else (2 if qt == 7 else 1)
            kts = [(2 * qt + j, slot * 6 + j) for j in range(6)] + [(20, None), (21, None)]
            attend(k_, 0, q_, qt * 256, 256, kts, v_, 1.0, bias=b_, tile0=qt * 2, col0=512 + h * 64)
        attend(k_, 0, q_, 2048, 128, [(20, None), (21, None)], v_, 1.0, tile0=16, col0=512 + h * 64)
    p.dma("sp", AO.t.rearrange("(t p) f -> p t f", p=128), attn[:], [attn], [AO])
    return p.finish()


def na_bias(rpb, hf, qt):
    rpb = f32(rpb)
    j = np.arange(6)[:, None, None, None, None]
    krl = np.arange(2)[None, :, None, None, None]
    kc = np.arange(64)[None, None, :, None, None]
    qrl = np.arange(4)[None, None, None, :, None]
    qc = np.arange(64)[None, None, None, None, :]
    kr = 32 * hf + 4 * qt - 4 + 2 * j + krl
    r = 32 * hf + 4 * qt + qrl
    rs = np.clip(r - 4, 0, 56)
    cs = np.clip(qc - 8, 0, 48)
    ok = (kr >= 0) & (kr < 64) & (kr >= rs) & (kr < rs + 8) & (kc >= cs) & (kc < cs + 16)
    ro = np.clip(kr - r + 7, 0, 14) + 0 * kc + 0 * qc
    co = np.clip(kc - qc + 15, 0, 30) + 0 * kr + 0 * r
    ok = np.broadcast_to(ok, ro.shape)
    out = np.where(ok[None], rpb[:, ro, co], np.float32(-30000.0))
    return out.reshape(8, 6, 128, 256).astype(np.float32)


def l3_inputs(b, hf, l2res, rpb):
    r0, r1 = l2res[2 * b], l2res[2 * b + 1]
    own = l2res[2 * b + hf]
    KT = np.concatenate([r0["KT"][:, :, :2048], r1["KT"][:, :, :2048], r0["KT"][:, :, 2048:], r1["KT"][:, :, 2048:]], axis=2)
    Vall = np.concatenate([r0["V"][:2048], r1["V"][:2048], r0["V"][2048:], r1["V"][2048:]], axis=0).reshape(NKEY, 8, 64)
    VA = np.concatenate([Vall, np.ones((NKEY, 8, 1), NPBF)], axis=2)
    nk_lat = np.concatenate([r0["NKT"][:, :, :2048], r1["NKT"][:, :, :2048]], axis=2)
    nk_ctx = np.concatenate([r0["NKT"][:, :, 2048:], r1["NKT"][:, :, 2048:]], axis=2)
    nv_lat = np.concatenate([r0["NV"][:2048], r1["NV"][:2048]], axis=0).reshape(4096, 8, 64)
    nv_ctx = np.concatenate([r0["NV"][2048:], r1["NV"][2048:]], axis=0).reshape(256, 8, 64)
    NK = np.zeros((64, 8, NAK), NPBF)
    NVv = np.zeros((NAK, 8, 64), NPBF)
    for i in range(40):
        gr = 32 * hf - 4 + i
        if 0 <= gr < 64:
            NK[:, :, i * 64:(i + 1) * 64] = nk_lat[:, :, gr * 64:(gr + 1) * 64]
            NVv[i * 64:(i + 1) * 64] = nv_lat[gr * 64:(gr + 1) * 64]
    NK[:, :, 2560:] = nk_ctx
    NVv[2560:] = nv_ctx
    NVA = np.concatenate([NVv, np.ones((NAK, 8, 1), NPBF)], axis=2)
    nb = np.stack([na_bias(rpb, hf, qt) for qt in (0, 3, 7)], axis=1)
    NB = np.ascontiguousarray(nb.reshape(8, 18, 128, 256).transpose(0, 2, 1, 3))
    return {"QT": own["QT"], "KT": np.ascontiguousarray(KT), "VA": np.ascontiguousarray(VA), "NQT": own["NQT"],
            "NKT": NK, "NVA": np.ascontiguousarray(NVA), "NB": NB}


class View:
    def __init__(self, ap, b):
        self.ap = ap
        self.b = b

    def __getitem__(self, idx):
        return self.ap


class Panels:
    def __init__(self, p, nslots=4):
        self.p = p
        self.st = [p.sb("pst%d" % i, [128, 8, 128], F32) for i in range(nslots)]
        self.bf = [p.sb("pbf%d" % i, [128, 8, 128], BF16) for i in range(nslots)]
        self.i = 0

    def get(self, w, col0):
        p = self.p
        k = self.i % len(self.st)
        self.i += 1
        st, bf = self.st[k], self.bf[k]
        if len(w.t.shape) == 4:
            p.dma("sp", st[:], w[col0 // 128], [w], [st])
        else:
            p.dma("sp", st[:], w[:, col0:col0 + 128].rearrange("(c p) n -> p c n", p=128), [w], [st])
        p.op("pool" if k % 2 == 0 else "dve", lambda e: e.tensor_copy(out=bf[:], in_=st[:]), [st], [bf])
        return bf


def ffn_phase1(p, pan, banks, hT, n, wg, wu, nf, actT, sg, f0=0, between=None):
    for f in range(f0, f0 + nf):
        g_, u_ = pan.get(wg, f * 128), pan.get(wu, f * 128)
        if between is not None:
            between(f - f0)
        for n0 in range(0, n, 512):
            nn = min(512, n - n0)
            pg, pu = banks(), banks()
            for (ps, w_) in ((pg, g_), (pu, u_)):
                for c in range(8):
                    p.op("pe", lambda e, ps=ps, w_=w_, c=c: e.matmul(ps[:, 0:nn], lhsT=w_[:, c, :], rhs=hT[:, c, n0:n0 + nn],
                                                                   start=(c == 0), stop=(c == 7)), [w_, hT], [ps])
            s = sg[(f + n0 // 512) % 2]
            p.op("act", lambda e, s=s, pg=pg: e.activation(out=s[:, 0:nn], in_=pg[:, 0:nn], func=AF.Silu), [pg], [s])
            p.op("dve", lambda e, s=s, pu=pu, f=f: e.tensor_tensor(out=actT[:, f - f0, n0:n0 + nn], in0=s[:, 0:nn], in1=pu[:, 0:nn],
                                                                 op=ALU.mult), [s, pu], [actT])


def build_l4():
    p = Prog()
    x = p.dram("x", [T2, D], F32, "ExternalInput")
    aT = p.dram("aT", [D, T2], BF16, "ExternalInput")
    wo = p.dram("wo", [D, D], F32, "ExternalInput")
    wg = p.dram("wg", [22, 128, 8, 128], F32, "ExternalInput")
    wu = p.dram("wu", [22, 128, 8, 128], F32, "ExternalInput")
    wd = p.dram("wd", [2816, D], F32, "ExternalInput")
    g1_d = p.dram("g1bc", [2, 128, D], F32, "ExternalInput")
    g2_d = p.dram("g2bc", [2, 128, D], F32, "ExternalInput")
    gsT_d = p.dram("gsT", [128, 8, 2], F32, "ExternalInput")
    shT_d = p.dram("shT", [128, 8, 2], F32, "ExternalInput")
    ident_d = p.dram("ident", [128, 128], BF16, "ExternalInput")
    xo = p.dram("xo", [T2, D], F32, "ExternalOutput")
    ident = p.sb("ident", [128, 128], BF16)
    p.dma("sp", ident[:], ident_d[:], [ident_d], [ident])
    gsT = p.sb("gsT", [128, 8, 2], F32)
    shT = p.sb("shT", [128, 8, 2], F32)
    p.dma("sp", gsT[:], gsT_d[:], [gsT_d], [gsT])
    p.dma("sp", shT[:], shT_d[:], [shT_d], [shT])
    stg = Stage(p, 1024)
    WO = load_w(p, stg, "WO", wo, 8, 1024)
    WD = load_w(p, stg, "WD", wd, 22, 1024)
    nt = NormT(p, ident, nslots=1)
    pan = Panels(p)
    hT = p.sb("hT", [128, 8, 512], BF16)
    at = p.sb("at", [128, 8, 512], BF16)
    actT = p.sb("actT", [128, 22, 512], BF16)
    xs = p.sb("xs", [128, 4, D], F32)
    g1 = p.sb("g1", [128, D], F32)
    g2 = p.sb("g2", [128, D], F32)
    sg = [p.sb("sg%d" % i, [128, 512], F32) for i in range(2)]
    tm = [p.sb("tm%d" % i, [128, 512], F32) for i in range(2)]
    pp = [p.ps("pp%d" % i, [128, 512], F32) for i in range(6)]
    ppi = [0]

    def banks():
        ppi[0] += 1
        return pp[ppi[0] % 6]

    tmi = [0]

    def resid(ps, gt, ti, cb):
        t = tm[tmi[0] % 2]
        tmi[0] += 1
        p.op("dve", lambda e: e.tensor_tensor(out=t[:], in0=ps[:], in1=gt[:, cb * 512:(cb + 1) * 512], op=ALU.mult), [ps, gt], [t])
        p.op("pool", lambda e: e.tensor_tensor(out=xs[:, ti, cb * 512:(cb + 1) * 512], in0=xs[:, ti, cb * 512:(cb + 1) * 512],
                                               in1=t[:], op=ALU.add), [xs, t], [xs])

    for (c0, n) in BLK2:
        ntile = n // 128
        cond = 0 if c0 < 2048 else 1
        p.dma("sp", g1[:], g1_d[cond], [g1_d], [g1])
        p.dma("sp", g2[:], g2_d[cond], [g2_d], [g2])
        p.dma("sp", xs[:, 0:ntile, :], x[c0:c0 + n, :].rearrange("(t p) f -> p t f", p=128), [x], [xs])
        p.dma("sp", at[:, :, 0:n], aT[:, c0:c0 + n].rearrange("(c p) t -> p c t", p=128), [aT], [at])
        for ti in range(ntile):
            for cb in range(2):
                ps = banks()
                for c in range(8):
                    p.op("pe", lambda e, c=c, ps=ps: e.matmul(ps[:], lhsT=at[:, c, ti * 128:(ti + 1) * 128],
                                                             rhs=WO[:, c, cb * 512:(cb + 1) * 512], start=(c == 0), stop=(c == 7)),
                         [at, WO], [ps])
                resid(ps, g1, ti, cb)
            nt.run(None, None, gsT, shT, cond, hT, ti * 128, x_loaded=View(xs[:, ti, :], xs.b))
        ffn_phase1(p, pan, banks, hT, n, wg, wu, 22, actT, sg)
        for ti in range(ntile):
            for cb in range(2):
                ps = banks()
                for f in range(22):
                    p.op("pe", lambda e, f=f, ps=ps: e.matmul(ps[:], lhsT=actT[:, f, ti * 128:(ti + 1) * 128],
                                                             rhs=WD[:, f, cb * 512:(cb + 1) * 512], start=(f == 0), stop=(f == 21)),
                         [actT, WD], [ps])
                resid(ps, g2, ti, cb)
        p.dma("pool", xo[c0:c0 + n, :].rearrange("(t p) f -> p t f", p=128), xs[:, 0:ntile, :], [xs], [xo])
    return p.finish()


def pretile(w):
    w = f32(w)
    F = w.shape[1]
    return np.ascontiguousarray(w.reshape(8, 128, F // 128, 128).transpose(2, 1, 0, 3))


def bc128(v):
    return np.ascontiguousarray(np.broadcast_to(f32(v)[None, :], (128, len(v))))


def build_l5():
    p = Prog()
    x = p.dram("x", [T2, D], F32, "ExternalInput")
    wi = p.dram("wi", [D, D], F32, "ExternalInput")
    gsT_d = p.dram("gsT", [128, 8, 2], F32, "ExternalInput")
    shT_d = p.dram("shT", [128, 8, 2], F32, "ExternalInput")
    ident_d = p.dram("ident", [128, 128], BF16, "ExternalInput")
    uo = p.dram("u", [T2, D], F32, "ExternalOutput")
    ident = p.sb("ident", [128, 128], BF16)
    p.dma("sp", ident[:], ident_d[:], [ident_d], [ident])
    gsT = p.sb("gsT", [128, 8, 2], F32)
    shT = p.sb("shT", [128, 8, 2], F32)
    p.dma("sp", gsT[:], gsT_d[:], [gsT_d], [gsT])
    p.dma("sp", shT[:], shT_d[:], [shT_d], [shT])
    stg = Stage(p, 1024)
    WI = load_w(p, stg, "WI", wi, 8, 1024)
    nt = NormT(p, ident)
    hT = [p.sb("hT%d" % i, [128, 8, 128], BF16) for i in range(2)]
    us = [p.sb("us%d" % i, [128, D], F32) for i in range(2)]
    pp = [p.ps("pp%d" % i, [128, 512], F32) for i in range(4)]
    k = 0
    for ti in range(T2 // 128):
        cond = 0 if ti < 16 else 1
        h_, u_ = hT[ti % 2], us[ti % 2]
        nt.run(x[ti * 128:(ti + 1) * 128, :], x.b, gsT, shT, cond, h_, 0)
        for cb in range(2):
            ps = pp[k % 4]
            k += 1
            for c in range(8):
                p.op("pe", lambda e, c=c, ps=ps: e.matmul(ps[:], lhsT=h_[:, c, :], rhs=WI[:, c, cb * 512:(cb + 1) * 512],
                                                         start=(c == 0), stop=(c == 7)), [h_, WI], [ps])
            p.op("act" if cb else "dve", (lambda e, ps=ps: e.copy(out=u_[:, cb * 512:(cb + 1) * 512], in_=ps[:])) if cb else
                 (lambda e, ps=ps: e.tensor_copy(out=u_[:, cb * 512:(cb + 1) * 512], in_=ps[:])), [ps], [u_])
        p.dma("pool", uo[ti * 128:(ti + 1) * 128, :], u_[:], [u_], [uo])
    return p.finish()


NCH = 544
TWO_PI = 2.0 * np.pi


def build_l6():
    p = Prog()
    U = p.dram("U", [64, 128, NCH], F32, "ExternalInput")
    prm = p.dram("prm", [3, 128, 64], F32, "ExternalInput")
    bri = p.dram("bri", [2, 128, 64, 16], F32, "ExternalInput")
    cri = p.dram("cri", [2, 128, 64, 16], F32, "ExternalInput")
    sel = p.dram("sel", [128, 2], F32, "ExternalInput")
    dq_d = p.dram("dq", [128, 64], F32, "ExternalInput")
    mk_d = p.dram("mk", [128, 64], F32, "ExternalInput")
    identf_d = p.dram("identf", [128, 128], F32, "ExternalInput")
    ident_d = p.dram("ident", [128, 128], BF16, "ExternalInput")
    Y = p.dram("Y", [64, 128, 512], F32, "ExternalOutput")

    def ld(name, shape, src, dt=F32):
        t = p.sb(name, shape, dt)
        p.dma("sp", t[:], src, [src] if isinstance(src, Tile) else [], [t])
        return t

    AR = ld("AR", [128, 64], prm[0]); AI = ld("AI", [128, 64], prm[1]); LS = ld("LS", [128, 64], prm[2])
    BR = ld("BR", [128, 64, 16], bri[0]); BI = ld("BI", [128, 64, 16], bri[1])
    CR = ld("CR", [128, 64, 16], cri[0]); CI = ld("CI", [128, 64, 16], cri[1])
    SEL = ld("SEL", [128, 2], sel[:]); DQ = ld("DQ", [128, 64], dq_d[:]); MK = ld("MK", [128, 64], mk_d[:])
    IDF = ld("IDF", [128, 128], identf_d[:]); IDB = ld("IDB", [128, 128], ident_d[:], BF16)
    sa, sb_ = SEL[:, 0:1], SEL[:, 1:2]
    n_ = [0]

    def T(shape=(128, 64), dt=F32):
        n_[0] += 1
        return p.sb("g%d" % n_[0], list(shape), dt)

    def tt(out, a, b, op, eng="dve"):
        p.op(eng, lambda e: e.tensor_tensor(out=out[:], in0=a[:], in1=b[:], op=op), [a, b], [out])
        return out

    def ts(out, a, s1, op0, s2=None, op1=None, rd=()):
        if op1 is None:
            p.op("dve", lambda e: e.tensor_scalar(out=out[:], in0=a[:], scalar1=s1, scalar2=None, op0=op0), [a] + list(rd), [out])
        else:
            p.op("dve", lambda e: e.tensor_scalar(out=out[:], in0=a[:], scalar1=s1, scalar2=s2, op0=op0, op1=op1), [a] + list(rd), [out])
        return out

    def stt(out, a, s, b, op0, op1, rd=()):
        p.op("dve", lambda e: e.scalar_tensor_tensor(out=out[:], in0=a[:], scalar=s, in1=b[:], op0=op0, op1=op1), [a, b] + list(rd), [out])
        return out

    def act(out, a, func, scale=1.0):
        p.op("act", lambda e: e.activation(out=out[:], in_=a[:], func=func, scale=scale), [a], [out])
        return out

    dt_ = act(T(), LS, AF.Exp)
    xd = tt(T(), AR, dt_, ALU.mult)
    th = tt(T(), AI, dt_, ALU.mult)
    ki = p.sb("ki", [128, 64], I32)
    kf, m1 = T(), T()

    def reduce_(r):
        ts(kf, r, 1.0 / TWO_PI, ALU.mult)
        p.op("dve", lambda e: e.tensor_copy(out=ki[:], in_=kf[:]), [kf], [ki])
        p.op("dve", lambda e: e.tensor_copy(out=kf[:], in_=ki[:]), [ki], [kf])
        stt(r, kf, -TWO_PI, r, ALU.mult, ALU.add)
        wrap(r)

    def wrap(r):
        ts(m1, r, float(np.pi), ALU.is_gt)
        stt(r, m1, -TWO_PI, r, ALU.mult, ALU.add)
        ts(m1, r, -float(np.pi), ALU.is_lt)
        stt(r, m1, TWO_PI, r, ALU.mult, ALU.add)

    lr, li = [None] * 9, [None] * 9
    for k in range(9):
        lr[k], li[k] = T(), T()
        if k == 0:
            p.op("pool", lambda e: e.memset(lr[0][:], 1.0), [], [lr[0]])
            p.op("pool", lambda e: e.memset(li[0][:], 0.0), [], [li[0]])
            continue
        ek = act(T(), xd, AF.Exp, scale=float(k))
        ph = ts(T(), th, float(k), ALU.mult)
        reduce_(ph)
        sk = act(T(), ph, AF.Sin)
        ts(ph, ph, float(np.pi / 2), ALU.add)
        wrap(ph)
        ck = act(T(), ph, AF.Sin)
        tt(lr[k], ek, ck, ALU.mult)
        tt(li[k], ek, sk, ALU.mult)
        if k == 8:
            ek8, ck8, sk8 = ek, ck, sk
    den = tt(T(), AR, AR, ALU.mult)
    t0 = tt(T(), AI, AI, ALU.mult)
    tt(den, den, t0, ALU.add)
    p.op("dve", lambda e: e.reciprocal(out=den[:], in_=den[:]), [den], [den])
    lm1 = ts(T(), lr[1], -1.0, ALU.add)
    fr = tt(T(), lm1, AR, ALU.mult); tt(t0, li[1], AI, ALU.mult); tt(fr, fr, t0, ALU.add); tt(fr, fr, den, ALU.mult)
    fi = tt(T(), li[1], AR, ALU.mult); tt(t0, lm1, AI, ALU.mult); tt(fi, fi, t0, ALU.subtract); tt(fi, fi, den, ALU.mult)
    al, be = [None] * 8, [None] * 8
    wr, wi_, t1 = T(), T(), T()
    for k in range(8):
        tt(wr, lr[k], fr, ALU.mult); tt(t0, li[k], fi, ALU.mult); tt(wr, wr, t0, ALU.subtract)
        tt(wi_, lr[k], fi, ALU.mult); tt(t0, li[k], fr, ALU.mult); tt(wi_, wi_, t0, ALU.add)
        al[k], be[k] = T(), T()
        ts(t1, wi_, sb_, ALU.mult, rd=[SEL]); stt(al[k], wr, sa, t1, ALU.mult, ALU.add, rd=[SEL])
        ts(t1, wi_, sa, ALU.mult, rd=[SEL]); stt(be[k], wr, sb_, t1, ALU.mult, ALU.subtract, rd=[SEL])
    nlr, nli = [None] * 9, [None] * 9
    for k in range(1, 9):
        nlr[k] = ts(T(), lr[k], -1.0, ALU.mult)
        nli[k] = ts(T(), li[k], -1.0, ALU.mult)
    cst = p.sb("cst", [128, 64, 16], BF16)
    ctmp = p.sb("ctmp", [128, 64, 16], F32)
    ts(ctmp, CI, sb_, ALU.mult, rd=[SEL])
    stt(cst, CR, sa, ctmp, ALU.mult, ALU.subtract, rd=[SEL])
    Mr, Mi_ = [None] * 10, [None] * 10
    Mr[0] = ck8
    Mi_[0] = ts(T(), sk8, -1.0, ALU.mult)
    for k in range(1, 10):
        Mr[k], Mi_[k] = T(), T()
        tt(t0, Mi_[k - 1], Mi_[k - 1], ALU.mult)
        tt(Mr[k], Mr[k - 1], Mr[k - 1], ALU.mult)
        tt(Mr[k], Mr[k], t0, ALU.subtract)
        tt(Mi_[k], Mr[k - 1], Mi_[k - 1], ALU.mult)
        ts(Mi_[k], Mi_[k], 2.0, ALU.mult)

    NG = 8
    NP = NG // 2
    def pairpack(X):
        Xp = T((128, 32))
        Xv = X[:].rearrange("p (g two) -> p g two", two=2)
        p.op("dve", lambda e: e.tensor_copy(out=Xp[0:64, :], in_=Xv[0:64, :, 0]), [X], [Xp])
        p.op("dve", lambda e: e.tensor_copy(out=Xp[64:128, :], in_=Xv[64:128, :, 1]), [X], [Xp])
        return Xp

    ek8p = pairpack(ek8)
    Mrp = [pairpack(Mr[k]) for k in range(10)]
    Mip = [pairpack(Mi_[k]) for k in range(10)]
    zpp = p.sb("zpp", [128, NG, 15, 16], BF16)
    p.op("pool", lambda e: e.memset(zpp[:], 0.0), [], [zpp])
    tz = [p.sb("tz%d" % i, [128, NG, 16], F32) for i in range(4)]
    Md = p.sb("Md", [128, NG, 256], BF16)
    p.op("pool", lambda e: e.memset(Md[:], 0.0), [], [Md])
    Mi = p.sb("Mi", [128, NG, 128], BF16)
    MoR = p.sb("MoR", [128, NG, 128], BF16)
    MoI = p.sb("MoI", [128, NG, 128], BF16)
    G = p.sb("G", [128, 2, NP, NCH], F32)
    Hb = p.sb("Hb", [128, 2, NP, 512], BF16)
    Er = p.sb("Er", [128, NP, NCH], F32)
    Ei = p.sb("Ei", [128, NP, NCH], F32)
    tE = [p.sb("tE%d" % i, [128, NP, 256], F32) for i in range(2)]
    gm = [p.sb("gm%d" % i, [128, 2, NCH], F32) for i in range(2)]
    gsn = [p.sb("gsn%d" % i, [128, 2, NCH], F32) for i in range(2)]
    ta = [p.sb("ta%d" % i, [128, NCH], F32) for i in range(4)]
    uf = [p.sb("uf%d" % i, [128, NCH], F32) for i in range(2)]
    ub = [p.sb("ub%d" % i, [128, NCH], BF16) for i in range(NG)]
    yo = [p.sb("yo%d" % i, [128, 512], F32) for i in range(2)]
    ptp = p.ps("ptp", [128, 128], BF16)
    psI = p.ps("psI", [128, 128], F32)
    pg = [p.ps("pg%d" % i, [128, 512], F32) for i in range(4)]
    pgi = [0]

    for ps_ in range(64 // NG):
        g0 = ps_ * NG
        pp0 = g0 // 2
        gs_ = slice(g0, g0 + NG)

        def bc(t_):
            return t_[:, gs_].unsqueeze(2).broadcast_to([128, NG, 16])

        for m in range(8):
            k = 7 - m
            p.op("dve", lambda e: e.tensor_tensor(out=tz[0][:], in0=BR[:, gs_, :], in1=bc(al[k]), op=ALU.mult), [BR, al[k]], [tz[0]])
            p.op("pool", lambda e: e.tensor_tensor(out=tz[1][:], in0=BI[:, gs_, :], in1=bc(be[k]), op=ALU.mult), [BI, be[k]], [tz[1]])
            p.op("dve", lambda e: e.tensor_tensor(out=zpp[:, :, m, :], in0=tz[0][:], in1=tz[1][:], op=ALU.add), [tz[0], tz[1]], [zpp])
        for t in range(8):
            k = t + 1
            mo_r = MoR[:, :, t * 16:(t + 1) * 16]
            mo_i = MoI[:, :, t * 16:(t + 1) * 16]
            p.op("dve", lambda e: e.tensor_tensor(out=tz[0][:], in0=CR[:, gs_, :], in1=bc(lr[k]), op=ALU.mult), [CR, lr[k]], [tz[0]])
            p.op("pool", lambda e: e.tensor_tensor(out=tz[1][:], in0=CI[:, gs_, :], in1=bc(nli[k]), op=ALU.mult), [CI, nli[k]], [tz[1]])
            p.op("dve", lambda e: e.tensor_tensor(out=tz[0][:], in0=tz[0][:], in1=tz[1][:], op=ALU.add), [tz[0], tz[1]], [tz[0]])
            p.op("dve", lambda e: e.tensor_tensor(out=mo_r, in0=tz[0][:], in1=bc(MK), op=ALU.mult), [tz[0], MK], [MoR])
            p.op("pool", lambda e: e.tensor_tensor(out=tz[2][:], in0=CR[:, gs_, :], in1=bc(nli[k]), op=ALU.mult), [CR, nli[k]], [tz[2]])
            p.op("dve", lambda e: e.tensor_tensor(out=tz[3][:], in0=CI[:, gs_, :], in1=bc(nlr[k]), op=ALU.mult), [CI, nlr[k]], [tz[3]])
            p.op("pool", lambda e: e.tensor_tensor(out=tz[2][:], in0=tz[2][:], in1=tz[3][:], op=ALU.add), [tz[2], tz[3]], [tz[2]])
            p.op("pool", lambda e: e.tensor_tensor(out=mo_i, in0=tz[2][:], in1=bc(MK), op=ALU.mult), [tz[2], MK], [MoI])
        for gl in range(NG):
            g = g0 + gl
            gp, h = gl // 2, gl % 2
            z = zpp
            zf = zpp[:, gl].rearrange("p m j -> p (m j)")
            p.op("pe", lambda e: e.transpose(out=ptp[:], in_=zf[:, 0:128], identity=IDB[:]), [z, IDB], [ptp])
            p.op("act", lambda e: e.copy(out=Md[:, gl, 64:128], in_=ptp[:, 0:64]), [ptp], [Md])
            p.op("act", lambda e: e.copy(out=Md[:, gl, 192:256], in_=ptp[:, 64:128]), [ptp], [Md])
            for t in range(8):
                p.op("pe", lambda e, t=t: e.matmul(psI[:, t * 16:(t + 1) * 16], lhsT=zf[:, (7 - t) * 16:(15 - t) * 16], rhs=cst[:, g, :],
                                                  start=True, stop=True), [z, cst], [psI])
            p.op("dve", lambda e: e.scalar_tensor_tensor(out=Mi[:, gl, :], in0=IDF[:], scalar=DQ[:, g:g + 1], in1=psI[:],
                                                         op0=ALU.mult, op1=ALU.add), [IDF, DQ, psI], [Mi])
            u_f, u_b = uf[gl % 2], ub[gl]
            p.dma("sp", u_f[:], U[g], [U], [u_f])
            p.op("act", lambda e: e.copy(out=u_b[:], in_=u_f[:]), [u_f], [u_b])
            for c in range(2):
                for (n0, nn) in ((0, 512), (512, 32)):
                    ps = pg[pgi[0] % 4]
                    pgi[0] += 1
                    if h == 0:
                        p.op("pe", lambda e: e.matmul(ps[0:64, 0:nn], lhsT=Md[:, gl, 64 + 128 * c:128 + 128 * c], rhs=u_b[:, n0:n0 + nn],
                                                      start=True, stop=True), [Md, u_b], [ps])
                        p.op("act", lambda e: e.copy(out=G[0:64, c, gp, n0:n0 + nn], in_=ps[0:64, 0:nn]), [ps], [G])
                    else:
                        p.op("pe", lambda e: e.matmul(ps[:, 0:nn], lhsT=Md[:, gl, 128 * c:128 * c + 128], rhs=u_b[:, n0:n0 + nn],
                                                      start=True, stop=True), [Md, u_b], [ps])
                        p.op("act", lambda e: e.copy(out=G[64:128, c, gp, n0:n0 + nn], in_=ps[64:128, 0:nn]), [ps], [G])
        p.op("pool", lambda e: e.memset(Er[:, :, 0:1], 1.0), [], [Er])
        p.op("pool", lambda e: e.memset(Ei[:, :, 0:1], 0.0), [], [Ei])
        for k in range(10):
            ln = 1 << k
            cn = min(ln, NCH - ln)
            if cn <= 0:
                break
            mr = Mrp[k][:, pp0:pp0 + NP].unsqueeze(2).broadcast_to([128, NP, cn])
            mi = Mip[k][:, pp0:pp0 + NP].unsqueeze(2).broadcast_to([128, NP, cn])
            p.op("dve", lambda e: e.tensor_tensor(out=tE[0][:, :, 0:cn], in0=Ei[:, :, 0:cn], in1=mi, op=ALU.mult), [Ei, Mip[k]], [tE[0]])
            p.op("pool", lambda e: e.tensor_tensor(out=tE[1][:, :, 0:cn], in0=Er[:, :, 0:cn], in1=mi, op=ALU.mult), [Er, Mip[k]], [tE[1]])
            p.op("dve", lambda e: e.tensor_tensor(out=Er[:, :, ln:ln + cn], in0=Er[:, :, 0:cn], in1=mr, op=ALU.mult), [Er, Mrp[k]], [Er])
            p.op("pool", lambda e: e.tensor_tensor(out=Ei[:, :, ln:ln + cn], in0=Ei[:, :, 0:cn], in1=mr, op=ALU.mult), [Ei, Mrp[k]], [Ei])
            p.op("dve", lambda e: e.tensor_tensor(out=Er[:, :, ln:ln + cn], in0=Er[:, :, ln:ln + cn], in1=tE[0][:, :, 0:cn], op=ALU.subtract), [Er, tE[0]], [Er])
            p.op("pool", lambda e: e.tensor_tensor(out=Ei[:, :, ln:ln + cn], in0=Ei[:, :, ln:ln + cn], in1=tE[1][:, :, 0:cn], op=ALU.add), [Ei, tE[1]], [Ei])
        for gp in range(NP):
            gm_, gsc = gm[gp % 2], gsn[gp % 2]
            er, ei = Er[:, gp, :], Ei[:, gp, :]
            gre, gim = G[:, 0, gp, :], G[:, 1, gp, :]
            p.op("dve", lambda e: e.tensor_tensor(out=ta[0][:], in0=er, in1=gre, op=ALU.mult), [Er, G], [ta[0]])
            p.op("dve", lambda e: e.tensor_tensor(out=ta[1][:], in0=ei, in1=gim, op=ALU.mult), [Ei, G], [ta[1]])
            p.op("dve", lambda e: e.tensor_tensor(out=gm_[:, 0, :], in0=ta[0][:], in1=ta[1][:], op=ALU.subtract), [ta[0], ta[1]], [gm_])
            p.op("pool", lambda e: e.tensor_tensor(out=ta[2][:], in0=er, in1=gim, op=ALU.mult), [Er, G], [ta[2]])
            p.op("pool", lambda e: e.tensor_tensor(out=ta[3][:], in0=ei, in1=gre, op=ALU.mult), [Ei, G], [ta[3]])
            p.op("pool", lambda e: e.tensor_tensor(out=gm_[:, 1, :], in0=ta[2][:], in1=ta[3][:], op=ALU.add), [ta[2], ta[3]], [gm_])
            rb = ek8p[:, pp0 + gp:pp0 + gp + 1].broadcast_to([128, NCH])
            for c in range(2):
                p.op("dve", lambda e, c=c: e.tensor_tensor_scan(out=gsc[:, c, :], data0=rb, data1=gm_[:, c, :], initial=0.0,
                                                              op0=ALU.mult, op1=ALU.add), [gm_, ek8p], [gsc])
            sl = slice(31, 543)
            p.op("dve", lambda e: e.tensor_tensor(out=ta[0][:, sl], in0=er[:, sl], in1=gsc[:, 0, sl], op=ALU.mult), [Er, gsc], [ta[0]])
            p.op("dve", lambda e: e.tensor_tensor(out=ta[1][:, sl], in0=ei[:, sl], in1=gsc[:, 1, sl], op=ALU.mult), [Ei, gsc], [ta[1]])
            p.op("dve", lambda e: e.tensor_tensor(out=Hb[:, 0, gp, :], in0=ta[0][:, sl], in1=ta[1][:, sl], op=ALU.add), [ta[0], ta[1]], [Hb])
            p.op("pool", lambda e: e.tensor_tensor(out=ta[2][:, sl], in0=er[:, sl], in1=gsc[:, 1, sl], op=ALU.mult), [Er, gsc], [ta[2]])
            p.op("pool", lambda e: e.tensor_tensor(out=ta[3][:, sl], in0=ei[:, sl], in1=gsc[:, 0, sl], op=ALU.mult), [Ei, gsc], [ta[3]])
            p.op("pool", lambda e: e.tensor_tensor(out=Hb[:, 1, gp, :], in0=ta[2][:, sl], in1=ta[3][:, sl], op=ALU.subtract), [ta[2], ta[3]], [Hb])
        for gl in range(NG):
            g = g0 + gl
            gp = gl // 2
            ps = pg[pgi[0] % 4]
            pgi[0] += 1
            p.op("pe", lambda e: e.matmul(ps[:, :], lhsT=Mi[:, gl, :], rhs=ub[gl][:, 32:NCH], start=True, stop=False), [Mi, ub[gl]], [ps])
            p.op("pe", lambda e: e.matmul(ps[:, :], lhsT=MoR[:, gl, :], rhs=Hb[:, 0, gp, :], start=False, stop=False), [MoR, Hb], [ps])
            p.op("pe", lambda e: e.matmul(ps[:, :], lhsT=MoI[:, gl, :], rhs=Hb[:, 1, gp, :], start=False, stop=True), [MoI, Hb], [ps])
            y_ = yo[gl % 2]
            p.op("act", lambda e: e.copy(out=y_[:], in_=ps[:, :]), [ps], [y_])
            p.dma("pool", Y[g], y_[:], [y_], [Y])
    return p.finish()


def l6_inputs(useq, d, od_a_re, od_a_im, od_log_step, od_b_re, od_b_im, od_c_re, od_c_im, od_d, with_skip):
    U = np.ascontiguousarray(f32(useq).reshape(NCH, 8, 64, 16).transpose(2, 1, 3, 0).reshape(64, 128, NCH))
    dup = lambda a: np.concatenate([a, a], axis=0)
    ar = dup(f32(od_a_re)[d].T)
    ai = dup(f32(od_a_im)[d].T)
    ls = np.broadcast_to(f32(od_log_step)[d][None, :], (128, 64))
    prm = np.ascontiguousarray(np.stack([ar, ai, ls]))
    br = dup(f32(od_b_re)[d].transpose(1, 0, 2))
    bi = dup(f32(od_b_im)[d].transpose(1, 0, 2))
    cr = dup(f32(od_c_re)[d].transpose(2, 0, 1))
    ci = dup(f32(od_c_im)[d].transpose(2, 0, 1))
    sel = np.zeros((128, 2), np.float32)
    sel[:64, 0] = 1.0
    sel[64:, 1] = 1.0
    dq = np.zeros((128, 64), np.float32)
    if with_skip:
        dq[:] = np.tile(f32(od_d).reshape(64, 16).T, (8, 1))
    mk = np.zeros((128, 64), np.float32)
    mk[:64, 0::2] = 1.0
    mk[64:, 1::2] = 1.0
    return {"mk": mk, "U": U, "prm": prm, "bri": np.ascontiguousarray(np.stack([br, bi])), "cri": np.ascontiguousarray(np.stack([cr, ci])),
            "sel": sel, "dq": dq, "identf": np.eye(128, dtype=np.float32), "ident": np.eye(128, dtype=np.float32).astype(NPBF)}


def l6_unpack(Y):
    return np.ascontiguousarray(np.asarray(Y).reshape(64, 8, 16, 512).transpose(3, 1, 0, 2).reshape(4096, 1024))


T7 = 2048


def build_l7():
    p = Prog()
    nc = p.nc
    x = p.dram("x", [T7, D], F32, "ExternalInput")
    yf = p.dram("yf", [T7, D], F32, "ExternalInput")
    yr = p.dram("yr", [T7, D], F32, "ExternalInput")
    wglu = p.dram("wglu", [D, 2048], F32, "ExternalInput")
    g1_d = p.dram("g1bc", [128, D], F32, "ExternalInput")
    g2_d = p.dram("g2bc", [128, D], F32, "ExternalInput")
    fg_d = p.dram("fgbc", [128, D], F32, "ExternalInput")
    gsT_d = p.dram("gsT", [128, 8], F32, "ExternalInput")
    shT_d = p.dram("shT", [128, 8], F32, "ExternalInput")
    wr_d = p.dram("wr", [128, 8, 8], F32, "ExternalInput")
    wge = p.dram("wge", [8, 28, 128, 8, 128], F32, "ExternalInput")
    wue = p.dram("wue", [8, 28, 128, 8, 128], F32, "ExternalInput")
    wde = p.dram("wde", [8, 3584, D], F32, "ExternalInput")
    ident_d = p.dram("ident", [128, 128], BF16, "ExternalInput")
    identf_d = p.dram("identf", [128, 128], F32, "ExternalInput")
    xm = p.dram("xm", [T7, D], F32, "Internal")
    out = p.dram("out", [T7, D], F32, "ExternalOutput")
    xmb = [Buf("xm%d" % i) for i in range(4)]
    pt = [p.ps("pt%d" % i, [128, 1024], BF16) for i in range(2)]
    ptf = p.ps("ptf", [128, 1024], F32)
    pp = [p.ps("pp%d" % i, [128, 512], F32) for i in range(4)]
    ppi = [0]

    def banks():
        ppi[0] += 1
        return pp[ppi[0] % 4]

    outer = p.es
    p.es = ExitStack()
    ident = p.sb("ident", [128, 128], BF16)
    p.dma("sp", ident[:], ident_d[:], [ident_d], [ident])
    g1 = p.sb("g1", [128, D], F32)
    p.dma("sp", g1[:], g1_d[:], [g1_d], [g1])
    stg = Stage(p, 2048)
    WG = load_w(p, stg, "WGLU", wglu, 8, 2048)
    ya = [p.sb("ya%d" % i, [128, D], F32) for i in range(2)]
    yb = [p.sb("yb%d" % i, [128, D], F32) for i in range(2)]
    y2 = p.sb("y2", [128, D], F32)
    gl = p.sb("gl", [128, D], BF16)
    glT = p.sb("glT", [128, 8, 128], BF16)
    xs = p.sb("xsA", [128, D], F32)
    sgm = p.sb("sgm", [128, 512], F32)
    ot = p.sb("ot", [128, 512], F32)
    for ti in range(T7 // 128):
        a, b_ = ya[ti % 2], yb[ti % 2]
        rs = slice(ti * 128, (ti + 1) * 128)
        p.dma("sp", a[:], yf[rs, :], [yf], [a])
        p.dma("sp", b_[:], yr[rs, :], [yr], [b_])
        p.dma("sp", xs[:], x[rs, :], [x], [xs])
        p.op("pool", lambda e: e.tensor_tensor(out=a[:], in0=a[:], in1=b_[:], op=ALU.add), [a, b_], [a])
        p.op("pool", lambda e: e.tensor_tensor(out=y2[:], in0=a[:], in1=a[:], op=ALU.mult), [a], [y2])
        p.op("dve", lambda e: e.tensor_scalar(out=y2[:], in0=y2[:], scalar1=0.044715, scalar2=1.0, op0=ALU.mult, op1=ALU.add), [y2], [y2])
        p.op("dve", lambda e: e.tensor_tensor(out=y2[:], in0=y2[:], in1=a[:], op=ALU.mult), [y2, a], [y2])
        p.op("act", lambda e: e.activation(out=y2[:], in_=y2[:], func=AF.Tanh, scale=0.7978845608028654), [y2], [y2])
        p.op("dve", lambda e: e.scalar_tensor_tensor(out=gl[:], in0=y2[:], scalar=1.0, in1=a[:], op0=ALU.add, op1=ALU.mult), [y2, a], [gl])
        ptt = pt[ti % 2]
        for c in range(8):
            p.op("pe", lambda e, c=c: e.transpose(out=ptt[:, c * 128:(c + 1) * 128], in_=gl[:, c * 128:(c + 1) * 128], identity=ident[:]), [gl, ident], [ptt])
        p.op("act", lambda e: e.copy(out=glT[:, 0:4, :], in_=ptt[:, 0:512].rearrange("p (c t) -> p c t", c=4)), [ptt], [glT])
        p.op("dve", lambda e: e.tensor_copy(out=glT[:, 4:8, :], in_=ptt[:, 512:1024].rearrange("p (c t) -> p c t", c=4)), [ptt], [glT])
        for cb in range(2):
            pa, pb = banks(), banks()
            for (ps, off) in ((pa, cb * 512), (pb, 1024 + cb * 512)):
                for c in range(8):
                    p.op("pe", lambda e, c=c, ps=ps, off=off: e.matmul(ps[:], lhsT=glT[:, c, :], rhs=WG[:, c, off:off + 512],
                                                                      start=(c == 0), stop=(c == 7)), [glT, WG], [ps])
            p.op("act", lambda e: e.activation(out=sgm[:], in_=pb[:], func=AF.Sigmoid, scale=0.5), [pb], [sgm])
            p.op("dve", lambda e: e.scalar_tensor_tensor(out=ot[:], in0=pa[:], scalar=0.5, in1=sgm[:], op0=ALU.mult, op1=ALU.mult), [pa, sgm], [ot])
            p.op("pool", lambda e: e.tensor_tensor(out=ot[:], in0=ot[:], in1=g1[:, cb * 512:(cb + 1) * 512], op=ALU.mult), [ot, g1], [ot])
            p.op("pool", lambda e: e.tensor_tensor(out=xs[:, cb * 512:(cb + 1) * 512], in0=xs[:, cb * 512:(cb + 1) * 512], in1=ot[:], op=ALU.add), [xs, ot], [xs])
        p.dma("pool", xm[rs, :], xs[:], [xs], [xmb[ti // 4]])
    p.barrier()
    p.es.close()
    p.es = ExitStack()
    identf = p.sb("identf", [128, 128], F32)
    p.dma("sp", identf[:], identf_d[:], [identf_d], [identf])
    g2 = p.sb("g2", [128, D], F32)
    fg = p.sb("fg", [128, D], F32)
    gsT = p.sb("gsT", [128, 8], F32)
    shT = p.sb("shT", [128, 8], F32)
    wr = p.sb("wr", [128, 8, 8], F32)
    for a, b_ in ((g2, g2_d), (fg, fg_d), (gsT, gsT_d), (shT, shT_d), (wr, wr_d)):
        p.dma("sp", a[:], b_[:], [b_], [a])
    eps = mk_eps(p)
    stg = Stage(p, 512, n=3, name="stgB")
    pan = Panels(p)
    NTB = 8
    actT = p.sb("actT", [128, 14, 128 * NTB], BF16)
    WDh = [p.sb("WDh%d" % i, [128, 14, 512], BF16) for i in range(2)]
    hT = p.sb("hTB", [128, 8, 128 * NTB], BF16)
    hT32 = p.sb("hT32", [128, 8, 128], F32)
    xs = p.sb("xsB", [128, NTB, D], F32)
    xn = p.sb("xnB", [128, D], F32)
    junk = p.sb("junkB", [128, D], BF16)
    ss = p.sb("ssB", [128, 4], F32)
    lg = p.sb("lg", [128, 8], F32)
    mx = p.sb("mx", [128, 8], F32)
    e1 = p.sb("e1", [128, 8], F32)
    e2 = p.sb("e2", [128, 8], F32)
    w12 = p.sb("w12", [128, 4], F32)
    comb = p.sb("comb", [128, NTB, 8], F32)
    sg = [p.sb("sgB%d" % i, [128, 512], F32) for i in range(2)]
    tm = [p.sb("tmB%d" % i, [128, 512], F32) for i in range(2)]
    ob = [p.sb("ob%d" % i, [128, D], F32) for i in range(1)]
    tmi = [0]
    wdi = [0]
    for blk in range(T7 // (128 * NTB)):
        c0 = blk * 128 * NTB
        for q4 in range(NTB // 4):
            p.dma("sp", xs[:, q4 * 4:(q4 + 1) * 4, :], xm[c0 + q4 * 512:c0 + (q4 + 1) * 512, :].rearrange("(t p) f -> p t f", p=128),
                  [xmb[(c0 + q4 * 512) // 512]], [xs])
        for ti in range(NTB):
            xt = xs[:, ti, :]
            p.op("act", lambda e: e.activation(out=junk[:], in_=xt, func=AF.Square, accum_out=ss[:, 0:1]), [xs], [junk, ss])
            p.op("act", lambda e: e.activation(out=ss[:, 1:2], in_=ss[:, 0:1], func=AF.Sqrt, scale=1.0 / D, bias=eps[:, 0:1]), [ss, eps], [ss])
            p.op("dve", lambda e: e.reciprocal(out=ss[:, 2:3], in_=ss[:, 1:2]), [ss], [ss])
            p.op("dve", lambda e: e.tensor_scalar(out=xn[:], in0=xt, scalar1=ss[:, 2:3], scalar2=None, op0=ALU.mult), [xs, ss], [xn])
            for c in range(8):
                p.op("pe", lambda e, c=c: e.transpose(out=ptf[:, c * 128:(c + 1) * 128], in_=xn[:, c * 128:(c + 1) * 128], identity=identf[:]), [xn, identf], [ptf])
            for c in range(8):
                p.op("dve", lambda e, c=c: e.tensor_scalar(out=hT32[:, c, :], in0=ptf[:, c * 128:(c + 1) * 128], scalar1=gsT[:, c:c + 1],
                                                           scalar2=shT[:, c:c + 1], op0=ALU.mult, op1=ALU.add), [ptf, gsT, shT], [hT32])
            p.op("pool", lambda e: e.tensor_copy(out=hT[:, :, ti * 128:(ti + 1) * 128], in_=hT32[:]), [hT32], [hT])
            ps = banks()
            for c in range(8):
                p.op("pe", lambda e, c=c: e.matmul(ps[:, 0:8], lhsT=hT32[:, c, :], rhs=wr[:, c, :], start=(c == 0), stop=(c == 7)), [hT32, wr], [ps])
            p.op("dve", lambda e: e.tensor_copy(out=lg[:], in_=ps[:, 0:8]), [ps], [lg])
            p.op("dve", lambda e: e.max(out=mx[:], in_=lg[:]), [lg], [mx])
            p.op("dve", lambda e: e.tensor_tensor(out=w12[:, 0:1], in0=mx[:, 0:1], in1=mx[:, 1:2], op=ALU.subtract), [mx], [w12])
            p.op("act", lambda e: e.activation(out=w12[:, 1:2], in_=w12[:, 0:1], func=AF.Sigmoid), [w12], [w12])
            p.op("dve", lambda e: e.tensor_scalar(out=w12[:, 2:3], in0=w12[:, 1:2], scalar1=-1.0, scalar2=1.0, op0=ALU.mult, op1=ALU.add), [w12], [w12])
            p.op("dve", lambda e: e.tensor_scalar(out=e1[:], in0=lg[:], scalar1=mx[:, 0:1], scalar2=w12[:, 1:2], op0=ALU.is_equal, op1=ALU.mult), [lg, mx, w12], [e1])
            p.op("dve", lambda e: e.tensor_scalar(out=e2[:], in0=lg[:], scalar1=mx[:, 1:2], scalar2=w12[:, 2:3], op0=ALU.is_equal, op1=ALU.mult), [lg, mx, w12], [e2])
            p.op("dve", lambda e: e.tensor_tensor(out=comb[:, ti, :], in0=e1[:], in1=e2[:], op=ALU.add), [e1, e2], [comb])
        for ex in range(8):
            for fh in range(2):
                wd0, wd1 = WDh[0], WDh[1]

                def ldwd(f, wd_, cb):
                    r0 = (fh * 14 + f) * 128
                    stg.load(wd_[:, f, :], wd_.b, wde[ex, r0:r0 + 128, cb * 512:(cb + 1) * 512], wde.b, 128, 512)

                ffn_phase1(p, pan, banks, hT, 128 * NTB, Tile(wge[ex], wge.b), Tile(wue[ex], wue.b), 14, actT, sg, f0=fh * 14,
                           between=lambda f: ldwd(f, wd0, 0))
                for cb in range(2):
                    wd_ = WDh[cb]
                    if cb == 1:
                        for f in range(14):
                            ldwd(f, wd1, 1)
                    for ti in range(NTB):
                        ps = banks()
                        for f in range(14):
                            p.op("pe", lambda e, f=f, ps=ps: e.matmul(ps[:], lhsT=actT[:, f, ti * 128:(ti + 1) * 128], rhs=wd_[:, f, :],
                                                                     start=(f == 0), stop=(f == 13)), [actT, wd_], [ps])
                        t = tm[tmi[0] % 2]
                        tmi[0] += 1
                        p.op("dve", lambda e: e.scalar_tensor_tensor(out=t[:], in0=ps[:], scalar=comb[:, ti, ex:ex + 1], in1=g2[:, cb * 512:(cb + 1) * 512],
                                                                     op0=ALU.mult, op1=ALU.mult), [ps, comb, g2], [t])
                        p.op("pool", lambda e: e.tensor_tensor(out=xs[:, ti, cb * 512:(cb + 1) * 512], in0=xs[:, ti, cb * 512:(cb + 1) * 512],
                                                               in1=t[:], op=ALU.add), [xs, t], [xs])
        for ti in range(NTB):
            xt = xs[:, ti, :]
            o_ = ob[ti % len(ob)]
            p.op("act", lambda e: e.activation(out=junk[:], in_=xt, func=AF.Square, accum_out=ss[:, 0:1]), [xs], [junk, ss])
            p.op("act", lambda e: e.activation(out=ss[:, 1:2], in_=ss[:, 0:1], func=AF.Sqrt, scale=1.0 / D, bias=eps[:, 0:1]), [ss, eps], [ss])
            p.op("dve", lambda e: e.reciprocal(out=ss[:, 2:3], in_=ss[:, 1:2]), [ss], [ss])
            p.op("dve", lambda e: e.scalar_tensor_tensor(out=o_[:], in0=xt, scalar=ss[:, 2:3], in1=fg[:], op0=ALU.mult, op1=ALU.mult), [xs, ss, fg], [o_])
            p.dma("pool", out[c0 + ti * 128:c0 + (ti + 1) * 128, :], o_[:], [o_], [out])
    p.es.close()
    p.es = outer
    return p.finish()


def kernel(x, c, ctx, c_ctx, mod_w, mod_b, norm1_g, norm2_g,
           ev_w_in, ev_q_norm_g, ev_w_qb, ev_kv_norm_g, ev_w_kvb, ev_na_rpb, ev_w_out,
           ev_ffn_w_gate, ev_ffn_w_up, ev_ffn_w_down,
           od_w_in, od_a_re, od_a_im, od_log_step, od_b_re, od_b_im, od_c_re, od_c_im, od_d, od_w_glu,
           moe_w_router, moe_w_gate, moe_w_up, moe_w_down, final_g):
    x = f32(x)
    ctx = f32(ctx)
    m, gs = run_adaln(c, c_ctx, mod_w, mod_b, norm1_g, norm2_g)
    identb = np.eye(128, dtype=np.float32).astype(NPBF)
    identf = np.eye(128, dtype=np.float32)
    cores = [(i // 2, i % 2) for i in range(NCORES)]

    def mods(layer, b, lo_g, lo_s):
        return (np.ascontiguousarray(np.stack([fm(gs[layer, b, lo_g:lo_g + D], 8), fm(gs[layer, 4, lo_g:lo_g + D], 8)], axis=2)),
                np.ascontiguousarray(np.stack([fm(m[layer, b, lo_s:lo_s + D], 8), fm(m[layer, 4, lo_s:lo_s + D], 8)], axis=2)))

    in2 = []
    for (b, hf) in cores:
        xtok = np.concatenate([x[b, hf * 2048:(hf + 1) * 2048], ctx[b, hf * 128:(hf + 1) * 128]], 0)
        pos = np.concatenate([np.arange(hf * 2048, (hf + 1) * 2048), -np.ones(128, np.int64)])
        in2.append(l2_inputs(xtok, pos, gs[0, b, 1024:2048], m[0, b, 0:1024], gs[0, 4, 1024:2048], m[0, 4, 0:1024],
                             ev_w_in[0], ev_q_norm_g[0], ev_w_qb[0], ev_kv_norm_g[0], ev_w_kvb[0]))
    res2 = run_prog(build_l2(), in2)
    res2 = [{k: np.asarray(v) for k, v in r.items()} for r in res2]
    in3 = [l3_inputs(b, hf, res2, ev_na_rpb[0]) for (b, hf) in cores]
    res3 = run_prog(build_l3(), in3)
    in4 = []
    for i, (b, hf) in enumerate(cores):
        gsT, shT = mods(0, b, 4096, 3072)
        in4.append({"x": in2[i]["x"], "aT": np.ascontiguousarray(np.asarray(res3[i]["AO"]).T), "wo": f32(ev_w_out[0]),
                    "wg": pretile(ev_ffn_w_gate[0]), "wu": pretile(ev_ffn_w_up[0]), "wd": f32(ev_ffn_w_down[0]),
                    "g1bc": np.stack([bc128(m[0, b, 2048:3072]), bc128(m[0, 4, 2048:3072])]),
                    "g2bc": np.stack([bc128(m[0, b, 5120:6144]), bc128(m[0, 4, 5120:6144])]),
                    "gsT": gsT, "shT": shT, "ident": identb})
    res4 = run_prog(build_l4(), in4)
    xo = [np.asarray(r["xo"]) for r in res4]
    in5 = []
    for i, (b, hf) in enumerate(cores):
        gsT, shT = mods(1, b, 1024, 0)
        in5.append({"x": xo[i], "wi": f32(od_w_in[0]), "gsT": gsT, "shT": shT, "ident": identb})
    res5 = run_prog(build_l5(), in5)
    u = [np.asarray(r["u"]) for r in res5]
    in6 = []
    for i in range(NCORES):
        b, dr = i // 2, i % 2
        u0, u1 = u[2 * b], u[2 * b + 1]
        lat = np.concatenate([u0[:2048], u1[:2048]], 0)
        cx = np.concatenate([u0[2048:], u1[2048:]], 0)
        seq = np.concatenate([cx, lat], 0) if dr == 0 else np.concatenate([cx[::-1], lat[::-1]], 0)
        in6.append(l6_inputs(seq, dr, od_a_re[0], od_a_im[0], od_log_step[0], od_b_re[0], od_b_im[0],
                             od_c_re[0], od_c_im[0], od_d[0], dr == 0))
    res6 = run_prog(build_l6(), in6)
    Y = [l6_unpack(r["Y"]) for r in res6]
    in7 = []
    wr = np.ascontiguousarray(f32(moe_w_router[0]).reshape(8, 128, 8).transpose(1, 0, 2))
    wge_t = np.stack([pretile(moe_w_gate[0][e]) for e in range(8)])
    wue_t = np.stack([pretile(moe_w_up[0][e]) for e in range(8)])
    for i, (b, hf) in enumerate(cores):
        yf = Y[2 * b][hf * 2048:(hf + 1) * 2048]
        yr = Y[2 * b + 1][::-1][hf * 2048:(hf + 1) * 2048]
        in7.append({"x": np.ascontiguousarray(xo[i][:2048]), "yf": np.ascontiguousarray(yf), "yr": np.ascontiguousarray(yr),
                    "wglu": f32(od_w_glu[0]), "g1bc": bc128(m[1, b, 2048:3072]), "g2bc": bc128(m[1, b, 5120:6144]),
                    "fgbc": bc128(final_g), "gsT": fm(gs[1, b, 4096:5120], 8), "shT": fm(m[1, b, 3072:4096], 8),
                    "wr": wr, "wge": wge_t, "wue": wue_t, "wde": f32(moe_w_down[0]),
                    "ident": identb, "identf": identf})
    res7 = run_prog(build_l7(), in7)
    out = np.zeros((B, L, D), np.float32)
    for i, (b, hf) in enumerate(cores):
        out[b, hf * 2048:(hf + 1) * 2048] = np.asarray(res7[i]["out"])
    return out
```

```python
import numpy as np
import concourse.bass as bass
import concourse.mybir as mybir
from concourse.bass_utils import run_bass_kernel_spmd
from contextlib import ExitStack
import ml_dtypes

F32 = mybir.dt.float32
BF16 = mybir.dt.bfloat16
I32 = mybir.dt.int32
U32 = mybir.dt.uint32
AF = mybir.ActivationFunctionType
ALU = mybir.AluOpType
AX = mybir.AxisListType
NPBF = ml_dtypes.bfloat16

NCORES = 8
D = 1024
B = 4
L = 4096
LC = 256
EPS = 1e-6


class Buf:
    __slots__ = ("w", "rs", "name")

    def __init__(self, name=""):
        self.w = None
        self.rs = {}
        self.name = name


class Tile:
    __slots__ = ("t", "b")

    def __init__(self, t, b):
        self.t = t
        self.b = b

    def __getitem__(self, idx):
        return self.t[idx]


COMPUTE = ("pe", "act", "dve", "pool")


class View:
    def __init__(self, ap, b):
        self.ap = ap
        self.b = b

    def __getitem__(self, idx):
        return self.ap


class Prog:
    def __init__(self, ndma=8):
        self.nc = bass.Bass("TRN2", target_bir_lowering=False)
        nc = self.nc
        self.es = ExitStack()
        self.eng = {"pe": nc.tensor, "act": nc.scalar, "dve": nc.vector,
                    "pool": nc.gpsimd, "sp": nc.sync}
        self.sems = {}
        self.cnt = {}
        for e in COMPUTE:
            self.sems[e] = self.es.enter_context(nc.semaphore("c_" + e))
            self.cnt[e] = 0
        self.waited = {e: {} for e in self.eng}
        self.dpool = {}
        self.didx = {}
        self.dval = {}
        for q in ("sp", "act", "pool"):
            n = ndma if q == "sp" else 4
            self.dpool[q] = []
            for i in range(n):
                k = ("d", q, i)
                self.sems[k] = self.es.enter_context(nc.semaphore("d_%s_%d" % (q, i)))
                self.dval[k] = 0
                self.dpool[q].append(k)
            self.didx[q] = 0
        self.ndram = 0

    def sb(self, name, shape, dt):
        t = self.es.enter_context(self.nc.sbuf_tensor("s_" + name, list(shape), dt))
        return Tile(t, Buf(name))

    def ps(self, name, shape, dt):
        t = self.es.enter_context(self.nc.psum_tensor("p_" + name, list(shape), dt))
        return Tile(t, Buf(name))

    def dram(self, name, shape, dt, kind):
        t = self.nc.dram_tensor(name, list(shape), dt, kind=kind)
        return Tile(t.ap(), Buf(name))

    def _wait(self, e, reads, writes):
        need = {}
        for b in reads:
            if b.w is not None:
                k, v = b.w
                if not (k == e and e == "pe"):
                    need[k] = max(need.get(k, 0), v)
        for b in writes:
            if b.w is not None:
                k, v = b.w
                if not (k == e and e == "pe"):
                    need[k] = max(need.get(k, 0), v)
            for k, v in b.rs.items():
                if not (k == e and e == "pe"):
                    need[k] = max(need.get(k, 0), v)
        w = self.waited[e]
        for k, v in need.items():
            if w.get(k, 0) < v:
                self.eng[e].wait_ge(self.sems[k], v)
                w[k] = v

    def _mark(self, tok, reads, writes):
        k, v = tok
        for b in reads:
            if b.rs.get(k, 0) < v:
                b.rs[k] = v
        for b in writes:
            b.w = tok
            b.rs = {}

    def op(self, e, fn, reads=(), writes=()):
        reads = [r.b if isinstance(r, (Tile, View)) else r for r in reads]
        writes = [r.b if isinstance(r, (Tile, View)) else r for r in writes]
        self._wait(e, reads, writes)
        inst = fn(self.eng[e])
        self.cnt[e] += 1
        inst.then_inc(self.sems[e], 1)
        self._mark((e, self.cnt[e]), reads, writes)

    def dma(self, q, out, in_, reads=(), writes=(), **kw):
        reads = [r.b if isinstance(r, (Tile, View)) else r for r in reads]
        writes = [r.b if isinstance(r, (Tile, View)) else r for r in writes]
        pool = self.dpool[q]
        k = pool[self.didx[q] % len(pool)]
        self.didx[q] += 1
        pv = self.dval[k]
        w = self.waited[q]
        if pv and w.get(k, 0) < pv:
            self.eng[q].wait_ge(self.sems[k], pv)
            w[k] = pv
        self._wait(q, reads, writes)
        self.eng[q].dma_start(out=out, in_=in_, **kw).then_inc(self.sems[k], 16)
        self.dval[k] = pv + 16
        self._mark((k, pv + 16), reads, writes)

    def barrier(self):
        for e in self.eng:
            w = self.waited[e]
            for k in COMPUTE:
                v = self.cnt[k]
                if v and k != e and w.get(k, 0) < v:
                    self.eng[e].wait_ge(self.sems[k], v)
                    w[k] = v
            for k, v in self.dval.items():
                if v and w.get(k, 0) < v:
                    self.eng[e].wait_ge(self.sems[k], v)
                    w[k] = v

    def finish(self):
        for k, v in self.dval.items():
            if v and self.waited["sp"].get(k, 0) < v:
                self.nc.sync.wait_ge(self.sems[k], v)
        self.es.close()
        return self.nc


TIMES = []


def run_prog(nc, in_maps, trace=False):
    res = run_bass_kernel_spmd(nc, in_maps, core_ids=list(range(len(in_maps))), trace=trace)
    if trace:
        TIMES.append(res.exec_time_ns)
    return res.results


def f32(a):
    return np.ascontiguousarray(a, dtype=np.float32)


def build_adaln():
    p = Prog()
    cond = p.dram("cond", [128, 8, 5], F32, "ExternalInput")
    w = p.dram("w", [2, 1024, 768], F32, "ExternalInput")
    bias5 = p.dram("bias5", [2, 5, 768], F32, "ExternalInput")
    gain5 = p.dram("gain5", [2, 5, 768], F32, "ExternalInput")
    m_out = p.dram("m", [2, 5, 768], F32, "ExternalOutput")
    gs_out = p.dram("gs", [2, 5, 768], F32, "ExternalOutput")
    ct = p.sb("ct", [128, 8, 5], F32)
    st = p.sb("st", [128, 8, 5], F32)
    p.dma("sp", ct[:], cond[:], [cond], [ct])
    p.op("act", lambda e: e.activation(out=st[:], in_=ct[:], func=AF.Silu), [ct], [st])
    pss = [p.ps("ps%d" % i, [128, 512], F32) for i in range(2)]
    for l in range(2):
        wt = p.sb("wt%d" % l, [128, 8, 768], F32)
        bt = p.sb("bt%d" % l, [5, 768], F32)
        gt = p.sb("gt%d" % l, [5, 768], F32)
        mt = p.sb("mt%d" % l, [5, 768], F32)
        gst = p.sb("gst%d" % l, [5, 768], F32)
        p.dma("sp", wt[:], w[l].rearrange("(c p) n -> p c n", p=128), [w], [wt])
        p.dma("sp", bt[:], bias5[l], [bias5], [bt])
        p.dma("sp", gt[:], gain5[l], [gain5], [gt])
        for nb in range(2):
            ps = pss[nb]
            cs = slice(nb * 384, (nb + 1) * 384)
            for c in range(8):
                p.op("pe", lambda e, c=c, cs=cs, ps=ps: e.matmul(
                    ps[0:5, 0:384], lhsT=st[:, c, :], rhs=wt[:, c, cs],
                    start=(c == 0), stop=(c == 7)), [st, wt], [ps])
            p.op("dve", lambda e, cs=cs, ps=ps: e.tensor_tensor(
                out=mt[:, cs], in0=ps[0:5, 0:384], in1=bt[:, cs], op=ALU.add), [ps, bt], [mt])
        p.op("dve", lambda e: e.scalar_tensor_tensor(
            out=gst[:], in0=mt[:], scalar=1.0, in1=gt[:], op0=ALU.add, op1=ALU.mult), [mt, gt], [gst])
        p.dma("sp", m_out[l], mt[:], [mt], [m_out])
        p.dma("sp", gs_out[l], gst[:], [gst], [gs_out])
    return p.finish()


def run_adaln(c, c_ctx, mod_w, mod_b, norm1_g, norm2_g):
    cond_all = np.concatenate([f32(c), f32(c_ctx)[None]], 0)
    condT = np.ascontiguousarray(cond_all.T.reshape(8, 128, 5).transpose(1, 0, 2))
    gain = np.zeros((2, 6144), np.float32)
    gain[:, 1024:2048] = f32(norm1_g)
    gain[:, 4096:5120] = f32(norm2_g)
    in_maps = []
    for j in range(NCORES):
        cs = slice(768 * j, 768 * j + 768)
        in_maps.append({
            "cond": condT,
            "w": np.ascontiguousarray(f32(mod_w)[:, :, cs]),
            "bias5": np.ascontiguousarray(np.broadcast_to(f32(mod_b)[:, None, cs], (2, 5, 768))),
            "gain5": np.ascontiguousarray(np.broadcast_to(gain[:, None, cs], (2, 5, 768))),
        })
    res = run_prog(build_adaln(), in_maps)
    m = np.concatenate([r["m"] for r in res], axis=2)
    gs = np.concatenate([r["gs"] for r in res], axis=2)
    return m, gs


class Stage:
    def __init__(self, p, width, n=2, name="stg"):
        self.p = p
        self.slots = [p.sb("%s%d" % (name, i), [128, width], F32) for i in range(n)]
        self.i = 0
        self.ce = 0

    def load(self, dst_ap, dst_buf, src_ap, src_buf, rows, n, eng=None, scale=None):
        p = self.p
        s = self.slots[self.i % len(self.slots)]
        self.i += 1
        p.dma("sp", s[0:rows, 0:n], src_ap, [src_buf], [s])
        if scale is not None:
            sc_ap, sc_t = scale
            p.op("dve", lambda e: e.tensor_scalar(out=dst_ap, in0=s[0:rows, 0:n], scalar1=sc_ap, scalar2=None,
                                                  op0=ALU.mult), [s, sc_t], [dst_buf])
            return
        if eng is None:
            eng = ("pool", "dve")[self.ce % 2]
            self.ce += 1
        p.op(eng, lambda e: e.tensor_copy(out=dst_ap, in_=s[0:rows, 0:n]), [s], [dst_buf])


def load_w(p, stg, name, src, kc, n, eng=None, rowscale=None):
    dst = p.sb(name, [128, kc, n], BF16)
    for c in range(kc):
        sc = None if rowscale is None else (rowscale[:, c:c + 1], rowscale)
        stg.load(dst[:, c, :], dst.b, src[c * 128:(c + 1) * 128, :], src.b, 128, n, eng, sc)
    return dst


class NormT:
    def __init__(self, p, ident, nslots=2):
        self.p = p
        self.ident = ident
        self.xt = [p.sb("nx%d" % i, [128, 1024], F32) for i in range(nslots)]
        self.xn = [p.sb("nn%d" % i, [128, 1024], BF16) for i in range(nslots)]
        self.junk = p.sb("njunk", [128, 1024], BF16)
        self.ss = [p.sb("nss%d" % i, [128, 2], F32) for i in range(nslots)]
        self.pt = [p.ps("npt%d" % i, [128, 1024], BF16) for i in range(2)]
        self.i = 0
        self.eps = mk_eps(p)

    def run(self, x_ap, x_buf, gsT, shT, cond, hT, col0, x_loaded=None):
        p = self.p
        k = self.i % len(self.xt)
        self.i += 1
        xt, xn, ss, pt = self.xt[k], self.xn[k], self.ss[k], self.pt[k % 2]
        if x_loaded is None:
            p.dma("sp", xt[:], x_ap, [x_buf], [xt])
        else:
            xt = x_loaded
        p.op("act", lambda e: e.activation(out=self.junk[:], in_=xt[:], func=AF.Square,
                                           accum_out=ss[:, 0:1]), [xt], [self.junk, ss])
        p.op("act", lambda e: e.activation(out=ss[:, 1:2], in_=ss[:, 0:1], func=AF.Sqrt,
                                           scale=1.0 / D, bias=self.eps[:, 0:1]), [ss, self.eps], [ss])
        p.op("dve", lambda e: e.reciprocal(out=ss[:, 0:1], in_=ss[:, 1:2]), [ss], [ss])
        p.op("dve", lambda e: e.tensor_scalar(out=xn[:], in0=xt[:], scalar1=ss[:, 0:1], scalar2=None,
                                              op0=ALU.mult), [xt, ss], [xn])
        for c in range(8):
            p.op("pe", lambda e, c=c: e.transpose(out=pt[:, c * 128:(c + 1) * 128],
                                                   in_=xn[:, c * 128:(c + 1) * 128],
                                                   identity=self.ident[:]), [xn, self.ident], [pt])
        for c in range(8):
            p.op("dve" if c % 2 == 0 else "act",
                 (lambda e, c=c: e.tensor_scalar(out=hT[:, c, col0:col0 + 128], in0=pt[:, c * 128:(c + 1) * 128],
                                                 scalar1=gsT[:, c, cond:cond + 1], scalar2=shT[:, c, cond:cond + 1],
                                                 op0=ALU.mult, op1=ALU.add)) if c % 2 == 0 else
                 (lambda e, c=c: e.activation(out=hT[:, c, col0:col0 + 128], in_=pt[:, c * 128:(c + 1) * 128],
                                              func=AF.Identity, scale=gsT[:, c, cond:cond + 1],
                                              bias=shT[:, c, cond:cond + 1])),
                 [pt, gsT, shT], [hT])


def mk_eps(p, val=EPS):
    t = p.sb("epsc", [128, 1], F32)
    p.op("pool", lambda e: e.memset(t[:], val), [], [t])
    return t


T2 = 2176
BLK2 = [(0, 512), (512, 512), (1024, 512), (1536, 512), (2048, 128)]
NA_SCALE = 64 ** -0.5
MLA_SCALE = 96 ** -0.5


def build_l2(stop=None):
    p = Prog()
    x = p.dram("x", [T2, D], F32, "ExternalInput")
    w1 = p.dram("w1", [D, 2368], F32, "ExternalInput")
    wq = p.dram("wq", [384, 1536], F32, "ExternalInput")
    wkv = p.dram("wkv", [256, 1024], F32, "ExternalInput")
    cos_d = p.dram("cos96", [96, T2], F32, "ExternalInput")
    sin_d = p.dram("sin96", [96, T2], F32, "ExternalInput")
    gsT_d = p.dram("gsT", [128, 8, 2], F32, "ExternalInput")
    shT_d = p.dram("shT", [128, 8, 2], F32, "ExternalInput")
    gq_d = p.dram("gq", [128, 3], F32, "ExternalInput")
    gkv_d = p.dram("gkv", [128, 2], F32, "ExternalInput")
    ident_d = p.dram("ident", [128, 128], BF16, "ExternalInput")
    QT = p.dram("QT", [96, 8, T2], BF16, "ExternalOutput")
    KT = p.dram("KT", [96, 8, T2], BF16, "ExternalOutput")
    V = p.dram("V", [T2, 512], BF16, "ExternalOutput")
    NQT = p.dram("NQT", [64, 8, T2], BF16, "ExternalOutput")
    NKT = p.dram("NKT", [64, 8, T2], BF16, "ExternalOutput")
    NV = p.dram("NV", [T2, 512], BF16, "ExternalOutput")

    ident = p.sb("ident", [128, 128], BF16)
    p.dma("sp", ident[:], ident_d[:], [ident_d], [ident])
    ones = p.sb("ones", [128, 128], BF16)
    p.op("pool", lambda e: e.memset(ones[:], 1.0), [], [ones])
    cos = p.sb("cos", [96, T2], F32)
    sin = p.sb("sin", [96, T2], F32)
    p.dma("sp", cos[:], cos_d[:], [cos_d], [cos])
    p.dma("sp", sin[:], sin_d[:], [sin_d], [sin])
    gsT = p.sb("gsT", [128, 8, 2], F32)
    shT = p.sb("shT", [128, 8, 2], F32)
    gq = p.sb("gq", [128, 3], F32)
    gkv = p.sb("gkv", [128, 2], F32)
    for a, b_ in ((gsT, gsT_d), (shT, shT_d), (gq, gq_d), (gkv, gkv_d)):
        p.dma("sp", a[:], b_[:], [b_], [a])
    stg = Stage(p, 2368)
    W1 = load_w(p, stg, "W1", w1, 8, 2368)
    WQ = load_w(p, stg, "WQ", wq, 3, 1536, rowscale=gq)
    WKV = load_w(p, stg, "WKV", wkv, 2, 1024, rowscale=gkv)
    O_CQ, O_CKV, O_KR, O_KRR, O_NQ, O_NK, O_NV = 0, 384, 640, 736, 832, 1344, 1856
    nt = NormT(p, ident)
    eps = nt.eps
    hT = p.sb("hT", [128, 8, 512], BF16)
    cqg = p.sb("cqg", [128, 3, 512], BF16)
    sqq = p.sb("sqq", [128, 3, 512], BF16)
    ckvg = p.sb("ckvg", [128, 2, 512], BF16)
    sqkv = p.sb("sqkv", [128, 2, 512], BF16)
    rq = p.sb("rq", [128, 512], F32)
    rkv = p.sb("rkv", [128, 512], F32)
    rtok = p.sb("rtok", [128, 8], F32)
    tmpa = [p.sb("tmpa%d" % i, [128, 512], F32) for i in range(2)]
    tmpb = [p.sb("tmpb%d" % i, [128, 512], F32) for i in range(2)]
    krt = p.sb("krt", [96, 512], BF16)
    Qb = p.sb("Qb", [96, 8, 512], BF16)
    Kb = p.sb("Kb", [96, 8, 512], BF16)
    NQb = p.sb("NQb", [64, 8, 512], BF16)
    NKb = p.sb("NKb", [64, 8, 512], BF16)
    Vb = p.sb("Vb", [128, 4, 512], BF16)
    NVb = p.sb("NVb", [128, 4, 512], BF16)
    pp = [p.ps("pp%d" % i, [128, 512], F32) for i in range(6)]
    ppi = [0]

    def bank():
        ppi[0] += 1
        return pp[ppi[0] % 6]

    def mm(ps_ap, ps_t, pairs, rd):
        n = len(pairs)
        for i, (l, r) in enumerate(pairs):
            p.op("pe", lambda e, l=l, r=r, i=i: e.matmul(ps_ap, lhsT=l, rhs=r, start=(i == 0), stop=(i == n - 1)),
                 rd, [ps_t])

    for (c0, n) in BLK2:
        ntile = n // 128
        for ti in range(ntile):
            t0 = c0 + ti * 128
            cond = 0 if t0 < 2048 else 1
            nt.run(x[t0:t0 + 128, :], x.b, gsT, shT, cond, hT, ti * 128)
        if stop == 'norm':
            break
        for (dst, sq, g, off, nch, rbc, dim) in ((cqg, sqq, gq, O_CQ, 3, rq, 384), (ckvg, sqkv, gkv, O_CKV, 2, rkv, 256)):
            for c3 in range(nch):
                ps = bank()
                mm(ps[:, 0:n], ps, [(W1[:, c, off + c3 * 128: off + (c3 + 1) * 128], hT[:, c, 0:n]) for c in range(8)], [W1, hT])
                if stop == 'cq_mm':
                    continue
                p.op("dve", lambda e, ps=ps, c3=c3, dst=dst: e.tensor_copy(out=dst[:, c3, 0:n], in_=ps[:, 0:n]), [ps], [dst])
                p.op("act", lambda e, c3=c3, sq=sq, dst=dst: e.activation(out=sq[:, c3, 0:n], in_=dst[:, c3, 0:n], func=AF.Square), [dst], [sq])
            if stop in ('cq_mm', 'cq_dve', 'cq_act', 'cq_act2'):
                continue
            ps = bank()
            mm(ps[:, 0:n], ps, [(ones[:], sq[:, c3, 0:n]) for c3 in range(nch)], [ones, sq])
            if stop == 'cq_ones':
                continue
            p.op("act", lambda e, ps=ps, rbc=rbc, dim=dim: e.activation(out=rbc[:, 0:n], in_=ps[:, 0:n], func=AF.Sqrt,
                                                                      scale=1.0 / dim, bias=eps[:, 0:1]), [ps, eps], [rbc])
            p.op("dve", lambda e, rbc=rbc: e.reciprocal(out=rbc[:, 0:n], in_=rbc[:, 0:n]), [rbc], [rbc])
        if stop in ('cq', 'cq_mm', 'cq_dve', 'cq_act', 'cq_ones', 'cq_act2'):
            break
        ps = bank()
        for ti in range(ntile):
            mm(ps[:, ti:ti + 1], ps, [(sqkv[:, c2, ti * 128:(ti + 1) * 128], ones[:, 0:1]) for c2 in range(2)], [sqkv, ones])
        p.op("act", lambda e, ps=ps: e.activation(out=rtok[:, 0:ntile], in_=ps[:, 0:ntile], func=AF.Sqrt,
                                                  scale=1.0 / 256, bias=eps[:, 0:1]), [ps, eps], [rtok])
        p.op("dve", lambda e: e.reciprocal(out=rtok[:, 0:ntile], in_=rtok[:, 0:ntile]), [rtok], [rtok])
        if stop == 'rtok':
            break
        def rope(pa, pb, out_ap, out_t, scale_ap=None, scale_t=None, k=0):
            ta, tb = tmpa[k % 2], tmpb[k % 2]
            p.op("dve", lambda e: e.tensor_tensor(out=ta[0:96, 0:n], in0=pa[0:96, 0:n], in1=cos[:, c0:c0 + n], op=ALU.mult), [pa, cos], [ta])
            p.op("dve", lambda e: e.tensor_tensor(out=tb[0:96, 0:n], in0=pb[0:96, 0:n], in1=sin[:, c0:c0 + n], op=ALU.mult), [pb, sin], [tb])
            if scale_ap is None:
                p.op("pool", lambda e: e.tensor_tensor(out=out_ap, in0=ta[0:96, 0:n], in1=tb[0:96, 0:n], op=ALU.add), [ta, tb], [out_t])
            else:
                p.op("pool", lambda e: e.tensor_tensor(out=ta[0:96, 0:n], in0=ta[0:96, 0:n], in1=tb[0:96, 0:n], op=ALU.add), [ta, tb], [ta])
                p.op("pool", lambda e: e.tensor_tensor(out=out_ap, in0=ta[0:96, 0:n], in1=scale_ap, op=ALU.mult), [ta, scale_t], [out_t])
        for h in range(8):
            pa, pb = bank(), bank()
            mm(pa[0:96, 0:n], pa, [(WQ[:, c3, h * 96:(h + 1) * 96], cqg[:, c3, 0:n]) for c3 in range(3)], [WQ, cqg])
            mm(pb[0:96, 0:n], pb, [(WQ[:, c3, 768 + h * 96: 768 + (h + 1) * 96], cqg[:, c3, 0:n]) for c3 in range(3)], [WQ, cqg])
            rope(pa, pb, Qb[:, h, 0:n], Qb, rq[0:96, 0:n], rq, k=h)
        if stop == 'q':
            break
        pa, pb = bank(), bank()
        mm(pa[0:96, 0:n], pa, [(W1[:, c, O_KR:O_KR + 96], hT[:, c, 0:n]) for c in range(8)], [W1, hT])
        mm(pb[0:96, 0:n], pb, [(W1[:, c, O_KRR:O_KRR + 96], hT[:, c, 0:n]) for c in range(8)], [W1, hT])
        rope(pa, pb, krt[:, 0:n], krt)
        if stop == 'kr':
            break
        for h in range(8):
            ps = bank()
            mm(ps[0:64, 0:n], ps, [(WKV[:, c2, h * 64:(h + 1) * 64], ckvg[:, c2, 0:n]) for c2 in range(2)], [WKV, ckvg])
            p.op("dve", lambda e, ps=ps, h=h: e.tensor_tensor(out=Kb[0:64, h, 0:n], in0=ps[0:64, 0:n], in1=rkv[0:64, 0:n], op=ALU.mult), [ps, rkv], [Kb])
            p.op("pool", lambda e, h=h: e.tensor_copy(out=Kb[64:96, h, 0:n], in_=krt[64:96, 0:n]), [krt], [Kb])
        for ti in range(ntile):
            ps = bank()
            mm(ps[:, :], ps, [(ckvg[:, c2, ti * 128:(ti + 1) * 128], WKV[:, c2, 512:1024]) for c2 in range(2)], [WKV, ckvg])
            p.op("dve", lambda e, ps=ps, ti=ti: e.tensor_scalar(out=Vb[:, ti, :], in0=ps[:, :], scalar1=rtok[:, ti:ti + 1], scalar2=None, op0=ALU.mult), [ps, rtok], [Vb])
        if stop == 'kv':
            break
        for h in range(8):
            ps = bank()
            mm(ps[0:64, 0:n], ps, [(W1[:, c, O_NQ + h * 64:O_NQ + (h + 1) * 64], hT[:, c, 0:n]) for c in range(8)], [W1, hT])
            p.op("act", lambda e, ps=ps, h=h: e.mul(out=NQb[:, h, 0:n], in_=ps[0:64, 0:n], mul=NA_SCALE), [ps], [NQb])
            ps = bank()
            mm(ps[0:64, 0:n], ps, [(W1[:, c, O_NK + h * 64:O_NK + (h + 1) * 64], hT[:, c, 0:n]) for c in range(8)], [W1, hT])
            p.op("dve", lambda e, ps=ps, h=h: e.tensor_copy(out=NKb[:, h, 0:n], in_=ps[0:64, 0:n]), [ps], [NKb])
        for ti in range(ntile):
            ps = bank()
            mm(ps[:, :], ps, [(hT[:, c, ti * 128:(ti + 1) * 128], W1[:, c, O_NV:O_NV + 512]) for c in range(8)], [W1, hT])
            p.op("act", lambda e, ps=ps, ti=ti: e.copy(out=NVb[:, ti, :], in_=ps[:, :]), [ps], [NVb])
        if stop == 'na':
            break
        p.dma("pool", QT[:, :, c0:c0 + n], Qb[:, :, 0:n], [Qb], [QT])
        p.dma("pool", KT[:, :, c0:c0 + n], Kb[:, :, 0:n], [Kb], [KT])
        p.dma("pool", NQT[:, :, c0:c0 + n], NQb[:, :, 0:n], [NQb], [NQT])
        p.dma("pool", NKT[:, :, c0:c0 + n], NKb[:, :, 0:n], [NKb], [NKT])
        p.dma("pool", V[c0:c0 + n, :].rearrange("(t p) f -> p t f", p=128), Vb[:, 0:ntile, :], [Vb], [V])
        p.dma("pool", NV[c0:c0 + n, :].rearrange("(t p) f -> p t f", p=128), NVb[:, 0:ntile, :], [NVb], [NV])
    return p.finish()


def rope_tables(pos):
    T = len(pos)
    cos = np.ones((96, T), np.float64)
    sin = np.zeros((96, T), np.float64)
    invf = 10000.0 ** (-np.arange(8) / 8.0)
    valid = pos >= 0
    row = (pos // 64).astype(np.float64)
    col = (pos % 64).astype(np.float64)
    for j in range(32):
        pp_ = row if j < 16 else col
        ang = (pp_.astype(np.float32) * invf[j % 8].astype(np.float32)).astype(np.float64)
        cj = np.where(valid, np.cos(ang), 1.0)
        sj = np.where(valid, np.sin(ang), 0.0)
        cos[64 + j] = cj
        sin[64 + j] = -sj if (j % 16) < 8 else sj
    return cos.astype(np.float32), sin.astype(np.float32)


ROPE_PERM = np.array([j + 8 if (j % 16) < 8 else j - 8 for j in range(32)])


def fm(vec, nch):
    return np.ascontiguousarray(f32(vec).reshape(nch, 128).T)


def l2_inputs(xtok, pos, gs_lat, sh_lat, gs_ctx, sh_ctx, ev_w_in, q_norm_g, w_qb, kv_norm_g, w_kvb):
    w_in = f32(ev_w_in)
    kr = w_in[:, 640:672]
    z64 = np.zeros((D, 64), np.float32)
    w1 = np.concatenate([w_in[:, 0:640], z64, kr, z64, kr[:, ROPE_PERM], w_in[:, 672:2208]], axis=1)
    wqb = f32(w_qb).reshape(384, 8, 96)
    wq_rot = np.concatenate([np.zeros((384, 8, 64), np.float32), wqb[:, :, 64:][:, :, ROPE_PERM]], axis=2)
    wq = np.concatenate([wqb.reshape(384, 768), wq_rot.reshape(384, 768)], axis=1)
    wkvb = f32(w_kvb).reshape(256, 8, 128)
    wkv = np.concatenate([wkvb[:, :, :64].reshape(256, 512), wkvb[:, :, 64:].reshape(256, 512)], axis=1)
    cos96, sin96 = rope_tables(pos)
    ident = np.eye(128, dtype=np.float32).astype(NPBF)
    return {
        "x": f32(xtok), "w1": np.ascontiguousarray(w1), "wq": np.ascontiguousarray(wq), "wkv": np.ascontiguousarray(wkv),
        "cos96": cos96, "sin96": sin96,
        "gsT": np.ascontiguousarray(np.stack([fm(gs_lat, 8), fm(gs_ctx, 8)], axis=2)),
        "shT": np.ascontiguousarray(np.stack([fm(sh_lat, 8), fm(sh_ctx, 8)], axis=2)),
        "gq": fm(q_norm_g, 3), "gkv": fm(kv_norm_g, 2), "ident": ident,
    }


NKEY = 4352
NAK = 40 * 64 + 256


def build_l3():
    p = Prog()
    QT = p.dram("QT", [96, 8, T2], BF16, "ExternalInput")
    KT = p.dram("KT", [96, 8, NKEY], BF16, "ExternalInput")
    VA = p.dram("VA", [NKEY, 8, 65], BF16, "ExternalInput")
    NQT = p.dram("NQT", [64, 8, T2], BF16, "ExternalInput")
    NKT = p.dram("NKT", [64, 8, NAK], BF16, "ExternalInput")
    NVA = p.dram("NVA", [NAK, 8, 65], BF16, "ExternalInput")
    NB = p.dram("NB", [8, 128, 18, 256], F32, "ExternalInput")
    AO = p.dram("AO", [T2, D], BF16, "ExternalOutput")
    attn = p.sb("attn", [128, 17, D], BF16)
    S = [p.ps("S%d" % i, [128, 512], F32) for i in range(2)]
    O = [p.ps("O%d" % i, [128, 512], F32) for i in range(4)]
    PT = [p.sb("PT%d" % i, [128, 512], BF16) for i in range(3)]
    rden = [p.sb("rden%d" % i, [128, 1], F32) for i in range(4)]
    tmp = [p.sb("tmpf%d" % i, [128, 256], F32) for i in range(2)]
    cnt = {"s": 0, "pt": 0, "tm": 0}

    def attend(kt, ktoff, q, q0, nq, keytiles, v, scale, bias=None, tile0=0, col0=0):
        nqs = nq // 128
        nk = len(keytiles)

        def score(i):
            kb, bj = keytiles[i]
            ps = S[cnt["s"] % 2]
            cnt["s"] += 1
            pt = PT[cnt["pt"] % 3]
            cnt["pt"] += 1
            p.op("pe", lambda e: e.matmul(ps[:, 0:nq], lhsT=kt[:, kb * 128:(kb + 1) * 128], rhs=q[:, q0:q0 + nq],
                                          start=True, stop=True), [kt, q], [ps])
            if bj is None:
                p.op("act", lambda e: e.activation(out=pt[:, 0:nq], in_=ps[:, 0:nq], func=AF.Exp, scale=scale), [ps], [pt])
            else:
                tm = tmp[cnt["tm"] % 2]
                cnt["tm"] += 1
                p.op("dve", lambda e: e.tensor_tensor(out=tm[:, 0:nq], in0=ps[:, 0:nq], in1=bias[:, bj, 0:nq], op=ALU.add), [ps, bias], [tm])
                p.op("act", lambda e: e.activation(out=pt[:, 0:nq], in_=tm[:, 0:nq], func=AF.Exp, scale=scale), [tm], [pt])
            return pt

        pts = [score(0)]
        for i, (kb, bj) in enumerate(keytiles):
            if i + 1 < nk:
                pts.append(score(i + 1))
            pt = pts[i]
            for qs in range(nqs):
                p.op("pe", lambda e, qs=qs: e.matmul(O[qs][:, 0:65], lhsT=pt[:, qs * 128:(qs + 1) * 128], rhs=v[:, kb, :],
                                                     start=(i == 0), stop=(i == nk - 1)), [pt, v], [O[qs]])
        for qs in range(nqs):
            p.op("dve", lambda e, qs=qs: e.reciprocal(out=rden[qs][:], in_=O[qs][:, 64:65]), [O[qs]], [rden[qs]])
            p.op("dve", lambda e, qs=qs: e.tensor_scalar(out=attn[:, tile0 + qs, col0:col0 + 64], in0=O[qs][:, 0:64],
                                                         scalar1=rden[qs][:, 0:1], scalar2=None, op0=ALU.mult),
                 [O[qs], rden[qs]], [attn])

    kth = [p.sb("kth%d" % i, [96, NKEY], BF16) for i in range(2)]
    vh = [p.sb("vh%d" % i, [128, 34, 65], BF16) for i in range(2)]
    qh = [p.sb("qh%d" % i, [96, T2], BF16) for i in range(2)]
    for h in range(8):
        k_, v_, q_ = kth[h % 2], vh[h % 2], qh[h % 2]
        p.dma("sp", k_[:], KT[:, h, :], [KT], [k_])
        p.dma("sp", v_[:], VA[:, h, :].rearrange("(t p) f -> p t f", p=128), [VA], [v_])
        p.dma("sp", q_[:], QT[:, h, :], [QT], [q_])
        for qb in range(4):
            attend(k_, 0, q_, qb * 512, 512, [(kb, None) for kb in range(34)], v_, MLA_SCALE, tile0=qb * 4, col0=h * 64)
        attend(k_, 0, q_, 2048, 128, [(32, None), (33, None)], v_, MLA_SCALE, tile0=16, col0=h * 64)
    nkh = [p.sb("nkh%d" % i, [64, NAK], BF16) for i in range(2)]
    nvh = [p.sb("nvh%d" % i, [128, 22, 65], BF16) for i in range(2)]
    nqh = [p.sb("nqh%d" % i, [64, T2], BF16) for i in range(2)]
    nbh = [p.sb("nbh%d" % i, [128, 18, 256], F32) for i in range(2)]
    for h in range(8):
        k_, v_, q_, b_ = nkh[h % 2], nvh[h % 2], nqh[h % 2], nbh[h % 2]
        p.dma("sp", k_[:], NKT[:, h, :], [NKT], [k_])
        p.dma("sp", v_[:], NVA[:, h, :].rearrange("(t p) f -> p t f", p=128), [NVA], [v_])
        p.dma("sp", q_[:], NQT[:, h, :], [NQT], [q_])
        p.dma("sp", b_[:], NB[h], [NB], [b_])
        for qt in range(8):
            slot = 0 if qt == 0 else (2 if qt == 7 else 1)
            kts = [(2 * qt + j, slot * 6 + j) for j in range(6)] + [(20, None), (21, None)]
            attend(k_, 0, q_, qt * 256, 256, kts, v_, 1.0, bias=b_, tile0=qt * 2, col0=512 + h * 64)
        attend(k_, 0, q_, 2048, 128, [(20, None), (21, None)], v_, 1.0, tile0=16, col0=512 + h * 64)
    p.dma("sp", AO.t.rearrange("(t p) f -> p t f", p=128), attn[:], [attn], [AO])
    return p.finish()


def na_bias(rpb, hf, qt):
    rpb = f32(rpb)
    j = np.arange(6)[:, None, None, None, None]
    krl = np.arange(2)[None, :, None, None, None]
    kc = np.arange(64)[None, None, :, None, None]
    qrl = np.arange(4)[None, None, None, :, None]
    qc = np.arange(64)[None, None, None, None, :]
    kr = 32 * hf + 4 * qt - 4 + 2 * j + krl
    r = 32 * hf + 4 * qt + qrl
    rs = np.clip(r - 4, 0, 56)
    cs = np.clip(qc - 8, 0, 48)
    ok = (kr >= 0) & (kr < 64) & (kr >= rs) & (kr < rs + 8) & (kc >= cs) & (kc < cs + 16)
    ro = np.clip(kr - r + 7, 0, 14) + 0 * kc + 0 * qc
    co = np.clip(kc - qc + 15, 0, 30) + 0 * kr + 0 * r
    ok = np.broadcast_to(ok, ro.shape)
    out = np.where(ok[None], rpb[:, ro, co], np.float32(-30000.0))
    return out.reshape(8, 6, 128, 256).astype(np.float32)


def l3_inputs(b, hf, l2res, rpb):
    r0, r1 = l2res[2 * b], l2res[2 * b + 1]
    own = l2res[2 * b + hf]
    KT = np.concatenate([r0["KT"][:, :, :2048], r1["KT"][:, :, :2048], r0["KT"][:, :, 2048:], r1["KT"][:, :, 2048:]], axis=2)
    Vall = np.concatenate([r0["V"][:2048], r1["V"][:2048], r0["V"][2048:], r1["V"][2048:]], axis=0).reshape(NKEY, 8, 64)
    VA = np.concatenate([Vall, np.ones((NKEY, 8, 1), NPBF)], axis=2)
    nk_lat = np.concatenate([r0["NKT"][:, :, :2048], r1["NKT"][:, :, :2048]], axis=2)
    nk_ctx = np.concatenate([r0["NKT"][:, :, 2048:], r1["NKT"][:, :, 2048:]], axis=2)
    nv_lat = np.concatenate([r0["NV"][:2048], r1["NV"][:2048]], axis=0).reshape(4096, 8, 64)
    nv_ctx = np.concatenate([r0["NV"][2048:], r1["NV"][2048:]], axis=0).reshape(256, 8, 64)
    NK = np.zeros((64, 8, NAK), NPBF)
    NVv = np.zeros((NAK, 8, 64), NPBF)
    for i in range(40):
        gr = 32 * hf - 4 + i
        if 0 <= gr < 64:
            NK[:, :, i * 64:(i + 1) * 64] = nk_lat[:, :, gr * 64:(gr + 1) * 64]
            NVv[i * 64:(i + 1) * 64] = nv_lat[gr * 64:(gr + 1) * 64]
    NK[:, :, 2560:] = nk_ctx
    NVv[2560:] = nv_ctx
    NVA = np.concatenate([NVv, np.ones((NAK, 8, 1), NPBF)], axis=2)
    nb = np.stack([na_bias(rpb, hf, qt) for qt in (0, 3, 7)], axis=1)
    NB = np.ascontiguousarray(nb.reshape(8, 18, 128, 256).transpose(0, 2, 1, 3))
    return {"QT": own["QT"], "KT": np.ascontiguousarray(KT), "VA": np.ascontiguousarray(VA), "NQT": own["NQT"],
            "NKT": NK, "NVA": np.ascontiguousarray(NVA), "NB": NB}


class View:
    def __init__(self, ap, b):
        self.ap = ap
        self.b = b

    def __getitem__(self, idx):
        return self.ap


class Panels:
    def __init__(self, p, nslots=4):
        self.p = p
        self.st = [p.sb("pst%d" % i, [128, 8, 128], F32) for i in range(nslots)]
        self.bf = [p.sb("pbf%d" % i, [128, 8, 128], BF16) for i in range(nslots)]
        self.i = 0

    def get(self, w, col0):
        p = self.p
        k = self.i % len(self.st)
        self.i += 1
        st, bf = self.st[k], self.bf[k]
        if len(w.t.shape) == 4:
            p.dma("sp", st[:], w[col0 // 128], [w], [st])
        else:
            p.dma("sp", st[:], w[:, col0:col0 + 128].rearrange("(c p) n -> p c n", p=128), [w], [st])
        p.op("pool" if k % 2 == 0 else "dve", lambda e: e.tensor_copy(out=bf[:], in_=st[:]), [st], [bf])
        return bf


def ffn_phase1(p, pan, banks, hT, n, wg, wu, nf, actT, sg, f0=0, between=None):
    for f in range(f0, f0 + nf):
        g_, u_ = pan.get(wg, f * 128), pan.get(wu, f * 128)
        if between is not None:
            between(f - f0)
        for n0 in range(0, n, 512):
            nn = min(512, n - n0)
            pg, pu = banks(), banks()
            for (ps, w_) in ((pg, g_), (pu, u_)):
                for c in range(8):
                    p.op("pe", lambda e, ps=ps, w_=w_, c=c: e.matmul(ps[:, 0:nn], lhsT=w_[:, c, :], rhs=hT[:, c, n0:n0 + nn],
                                                                   start=(c == 0), stop=(c == 7)), [w_, hT], [ps])
            s = sg[(f + n0 // 512) % 2]
            p.op("act", lambda e, s=s, pg=pg: e.activation(out=s[:, 0:nn], in_=pg[:, 0:nn], func=AF.Silu), [pg], [s])
            p.op("dve", lambda e, s=s, pu=pu, f=f: e.tensor_tensor(out=actT[:, f - f0, n0:n0 + nn], in0=s[:, 0:nn], in1=pu[:, 0:nn],
                                                                 op=ALU.mult), [s, pu], [actT])


def build_l4():
    p = Prog()
    x = p.dram("x", [T2, D], F32, "ExternalInput")
    aT = p.dram("aT", [D, T2], BF16, "ExternalInput")
    wo = p.dram("wo", [D, D], F32, "ExternalInput")
    wg = p.dram("wg", [22, 128, 8, 128], F32, "ExternalInput")
    wu = p.dram("wu", [22, 128, 8, 128], F32, "ExternalInput")
    wd = p.dram("wd", [2816, D], F32, "ExternalInput")
    g1_d = p.dram("g1bc", [2, 128, D], F32, "ExternalInput")
    g2_d = p.dram("g2bc", [2, 128, D], F32, "ExternalInput")
    gsT_d = p.dram("gsT", [128, 8, 2], F32, "ExternalInput")
    shT_d = p.dram("shT", [128, 8, 2], F32, "ExternalInput")
    ident_d = p.dram("ident", [128, 128], BF16, "ExternalInput")
    xo = p.dram("xo", [T2, D], F32, "ExternalOutput")
    ident = p.sb("ident", [128, 128], BF16)
    p.dma("sp", ident[:], ident_d[:], [ident_d], [ident])
    gsT = p.sb("gsT", [128, 8, 2], F32)
    shT = p.sb("shT", [128, 8, 2], F32)
    p.dma("sp", gsT[:], gsT_d[:], [gsT_d], [gsT])
    p.dma("sp", shT[:], shT_d[:], [shT_d], [shT])
    stg = Stage(p, 1024)
    WO = load_w(p, stg, "WO", wo, 8, 1024)
    WD = load_w(p, stg, "WD", wd, 22, 1024)
    nt = NormT(p, ident, nslots=1)
    pan = Panels(p)
    hT = p.sb("hT", [128, 8, 512], BF16)
    at = p.sb("at", [128, 8, 512], BF16)
    actT = p.sb("actT", [128, 22, 512], BF16)
    xs = p.sb("xs", [128, 4, D], F32)
    g1 = p.sb("g1", [128, D], F32)
    g2 = p.sb("g2", [128, D], F32)
    sg = [p.sb("sg%d" % i, [128, 512], F32) for i in range(2)]
    tm = [p.sb("tm%d" % i, [128, 512], F32) for i in range(2)]
    pp = [p.ps("pp%d" % i, [128, 512], F32) for i in range(6)]
    ppi = [0]

    def banks():
        ppi[0] += 1
        return pp[ppi[0] % 6]

    tmi = [0]

    def resid(ps, gt, ti, cb):
        t = tm[tmi[0] % 2]
        tmi[0] += 1
        p.op("dve", lambda e: e.tensor_tensor(out=t[:], in0=ps[:], in1=gt[:, cb * 512:(cb + 1) * 512], op=ALU.mult), [ps, gt], [t])
        p.op("pool", lambda e: e.tensor_tensor(out=xs[:, ti, cb * 512:(cb + 1) * 512], in0=xs[:, ti, cb * 512:(cb + 1) * 512],
                                               in1=t[:], op=ALU.add), [xs, t], [xs])

    for (c0, n) in BLK2:
        ntile = n // 128
        cond = 0 if c0 < 2048 else 1
        p.dma("sp", g1[:], g1_d[cond], [g1_d], [g1])
        p.dma("sp", g2[:], g2_d[cond], [g2_d], [g2])
        p.dma("sp", xs[:, 0:ntile, :], x[c0:c0 + n, :].rearrange("(t p) f -> p t f", p=128), [x], [xs])
        p.dma("sp", at[:, :, 0:n], aT[:, c0:c0 + n].rearrange("(c p) t -> p c t", p=128), [aT], [at])
        for ti in range(ntile):
            for cb in range(2):
                ps = banks()
                for c in range(8):
                    p.op("pe", lambda e, c=c, ps=ps: e.matmul(ps[:], lhsT=at[:, c, ti * 128:(ti + 1) * 128],
                                                             rhs=WO[:, c, cb * 512:(cb + 1) * 512], start=(c == 0), stop=(c == 7)),
                         [at, WO], [ps])
                resid(ps, g1, ti, cb)
            nt.run(None, None, gsT, shT, cond, hT, ti * 128, x_loaded=View(xs[:, ti, :], xs.b))
        ffn_phase1(p, pan, banks, hT, n, wg, wu, 22, actT, sg)
        for ti in range(ntile):
            for cb in range(2):
                ps = banks()
                for f in range(22):
                    p.op("pe", lambda e, f=f, ps=ps: e.matmul(ps[:], lhsT=actT[:, f, ti * 128:(ti + 1) * 128],
                                                             rhs=WD[:, f, cb * 512:(cb + 1) * 512], start=(f == 0), stop=(f == 21)),
                         [actT, WD], [ps])
                resid(ps, g2, ti, cb)
        p.dma("pool", xo[c0:c0 + n, :].rearrange("(t p) f -> p t f", p=128), xs[:, 0:ntile, :], [xs], [xo])
    return p.finish()


def pretile(w):
    w = f32(w)
    F = w.shape[1]
    return np.ascontiguousarray(w.reshape(8, 128, F // 128, 128).transpose(2, 1, 0, 3))


def bc128(v):
    return np.ascontiguousarray(np.broadcast_to(f32(v)[None, :], (128, len(v))))


def build_l5():
    p = Prog()
    x = p.dram("x", [T2, D], F32, "ExternalInput")
    wi = p.dram("wi", [D, D], F32, "ExternalInput")
    gsT_d = p.dram("gsT", [128, 8, 2], F32, "ExternalInput")
    shT_d = p.dram("shT", [128, 8, 2], F32, "ExternalInput")
    ident_d = p.dram("ident", [128, 128], BF16, "ExternalInput")
    uo = p.dram("u", [T2, D], F32, "ExternalOutput")
    ident = p.sb("ident", [128, 128], BF16)
    p.dma("sp", ident[:], ident_d[:], [ident_d], [ident])
    gsT = p.sb("gsT", [128, 8, 2], F32)
    shT = p.sb("shT", [128, 8, 2], F32)
    p.dma("sp", gsT[:], gsT_d[:], [gsT_d], [gsT])
    p.dma("sp", shT[:], shT_d[:], [shT_d], [shT])
    stg = Stage(p, 1024)
    WI = load_w(p, stg, "WI", wi, 8, 1024)
    nt = NormT(p, ident)
    hT = [p.sb("hT%d" % i, [128, 8, 128], BF16) for i in range(2)]
    us = [p.sb("us%d" % i, [128, D], F32) for i in range(2)]
    pp = [p.ps("pp%d" % i, [128, 512], F32) for i in range(4)]
    k = 0
    for ti in range(T2 // 128):
        cond = 0 if ti < 16 else 1
        h_, u_ = hT[ti % 2], us[ti % 2]
        nt.run(x[ti * 128:(ti + 1) * 128, :], x.b, gsT, shT, cond, h_, 0)
        for cb in range(2):
            ps = pp[k % 4]
            k += 1
            for c in range(8):
                p.op("pe", lambda e, c=c, ps=ps: e.matmul(ps[:], lhsT=h_[:, c, :], rhs=WI[:, c, cb * 512:(cb + 1) * 512],
                                                         start=(c == 0), stop=(c == 7)), [h_, WI], [ps])
            p.op("act" if cb else "dve", (lambda e, ps=ps: e.copy(out=u_[:, cb * 512:(cb + 1) * 512], in_=ps[:])) if cb else
                 (lambda e, ps=ps: e.tensor_copy(out=u_[:, cb * 512:(cb + 1) * 512], in_=ps[:])), [ps], [u_])
        p.dma("pool", uo[ti * 128:(ti + 1) * 128, :], u_[:], [u_], [uo])
    return p.finish()


NCH = 544
TWO_PI = 2.0 * np.pi


def build_l6():
    p = Prog()
    U = p.dram("U", [64, 128, NCH], F32, "ExternalInput")
    prm = p.dram("prm", [3, 128, 64], F32, "ExternalInput")
    bri = p.dram("bri", [2, 128, 64, 16], F32, "ExternalInput")
    cri = p.dram("cri", [2, 128, 64, 16], F32, "ExternalInput")
    sel = p.dram("sel", [128, 2], F32, "ExternalInput")
    dq_d = p.dram("dq", [128, 64], F32, "ExternalInput")
    mk_d = p.dram("mk", [128, 64], F32, "ExternalInput")
    identf_d = p.dram("identf", [128, 128], F32, "ExternalInput")
    ident_d = p.dram("ident", [128, 128], BF16, "ExternalInput")
    Y = p.dram("Y", [64, 128, 512], F32, "ExternalOutput")

    def ld(name, shape, src, dt=F32):
        t = p.sb(name, shape, dt)
        p.dma("sp", t[:], src, [src] if isinstance(src, Tile) else [], [t])
        return t

    AR = ld("AR", [128, 64], prm[0]); AI = ld("AI", [128, 64], prm[1]); LS = ld("LS", [128, 64], prm[2])
    BR = ld("BR", [128, 64, 16], bri[0]); BI = ld("BI", [128, 64, 16], bri[1])
    CR = ld("CR", [128, 64, 16], cri[0]); CI = ld("CI", [128, 64, 16], cri[1])
    SEL = ld("SEL", [128, 2], sel[:]); DQ = ld("DQ", [128, 64], dq_d[:]); MK = ld("MK", [128, 64], mk_d[:])
    IDF = ld("IDF", [128, 128], identf_d[:]); IDB = ld("IDB", [128, 128], ident_d[:], BF16)
    sa, sb_ = SEL[:, 0:1], SEL[:, 1:2]
    n_ = [0]

    def T(shape=(128, 64), dt=F32):
        n_[0] += 1
        return p.sb("g%d" % n_[0], list(shape), dt)

    def tt(out, a, b, op, eng="dve"):
        p.op(eng, lambda e: e.tensor_tensor(out=out[:], in0=a[:], in1=b[:], op=op), [a, b], [out])
        return out

    def ts(out, a, s1, op0, s2=None, op1=None, rd=()):
        if op1 is None:
            p.op("dve", lambda e: e.tensor_scalar(out=out[:], in0=a[:], scalar1=s1, scalar2=None, op0=op0), [a] + list(rd), [out])
        else:
            p.op("dve", lambda e: e.tensor_scalar(out=out[:], in0=a[:], scalar1=s1, scalar2=s2, op0=op0, op1=op1), [a] + list(rd), [out])
        return out

    def stt(out, a, s, b, op0, op1, rd=()):
        p.op("dve", lambda e: e.scalar_tensor_tensor(out=out[:], in0=a[:], scalar=s, in1=b[:], op0=op0, op1=op1), [a, b] + list(rd), [out])
        return out

    def act(out, a, func, scale=1.0):
        p.op("act", lambda e: e.activation(out=out[:], in_=a[:], func=func, scale=scale), [a], [out])
        return out

    dt_ = act(T(), LS, AF.Exp)
    xd = tt(T(), AR, dt_, ALU.mult)
    th = tt(T(), AI, dt_, ALU.mult)
    ki = p.sb("ki", [128, 64], I32)
    kf, m1 = T(), T()

    def reduce_(r):
        ts(kf, r, 1.0 / TWO_PI, ALU.mult)
        p.op("dve", lambda e: e.tensor_copy(out=ki[:], in_=kf[:]), [kf], [ki])
        p.op("dve", lambda e: e.tensor_copy(out=kf[:], in_=ki[:]), [ki], [kf])
        stt(r, kf, -TWO_PI, r, ALU.mult, ALU.add)
        wrap(r)

    def wrap(r):
        ts(m1, r, float(np.pi), ALU.is_gt)
        stt(r, m1, -TWO_PI, r, ALU.mult, ALU.add)
        ts(m1, r, -float(np.pi), ALU.is_lt)
        stt(r, m1, TWO_PI, r, ALU.mult, ALU.add)

    lr, li = [None] * 9, [None] * 9
    for k in range(9):
        lr[k], li[k] = T(), T()
        if k == 0:
            p.op("pool", lambda e: e.memset(lr[0][:], 1.0), [], [lr[0]])
            p.op("pool", lambda e: e.memset(li[0][:], 0.0), [], [li[0]])
            continue
        ek = act(T(), xd, AF.Exp, scale=float(k))
        ph = ts(T(), th, float(k), ALU.mult)
        reduce_(ph)
        sk = act(T(), ph, AF.Sin)
        ts(ph, ph, float(np.pi / 2), ALU.add)
        wrap(ph)
        ck = act(T(), ph, AF.Sin)
        tt(lr[k], ek, ck, ALU.mult)
        tt(li[k], ek, sk, ALU.mult)
        if k == 8:
            ek8, ck8, sk8 = ek, ck, sk
    den = tt(T(), AR, AR, ALU.mult)
    t0 = tt(T(), AI, AI, ALU.mult)
    tt(den, den, t0, ALU.add)
    p.op("dve", lambda e: e.reciprocal(out=den[:], in_=den[:]), [den], [den])
    lm1 = ts(T(), lr[1], -1.0, ALU.add)
    fr = tt(T(), lm1, AR, ALU.mult); tt(t0, li[1], AI, ALU.mult); tt(fr, fr, t0, ALU.add); tt(fr, fr, den, ALU.mult)
    fi = tt(T(), li[1], AR, ALU.mult); tt(t0, lm1, AI, ALU.mult); tt(fi, fi, t0, ALU.subtract); tt(fi, fi, den, ALU.mult)
    al, be = [None] * 8, [None] * 8
    wr, wi_, t1 = T(), T(), T()
    for k in range(8):
        tt(wr, lr[k], fr, ALU.mult); tt(t0, li[k], fi, ALU.mult); tt(wr, wr, t0, ALU.subtract)
        tt(wi_, lr[k], fi, ALU.mult); tt(t0, li[k], fr, ALU.mult); tt(wi_, wi_, t0, ALU.add)
        al[k], be[k] = T(), T()
        ts(t1, wi_, sb_, ALU.mult, rd=[SEL]); stt(al[k], wr, sa, t1, ALU.mult, ALU.add, rd=[SEL])
        ts(t1, wi_, sa, ALU.mult, rd=[SEL]); stt(be[k], wr, sb_, t1, ALU.mult, ALU.subtract, rd=[SEL])
    nlr, nli = [None] * 9, [None] * 9
    for k in range(1, 9):
        nlr[k] = ts(T(), lr[k], -1.0, ALU.mult)
        nli[k] = ts(T(), li[k], -1.0, ALU.mult)
    cst = p.sb("cst", [128, 64, 16], BF16)
    ctmp = p.sb("ctmp", [128, 64, 16], F32)
    ts(ctmp, CI, sb_, ALU.mult, rd=[SEL])
    stt(cst, CR, sa, ctmp, ALU.mult, ALU.subtract, rd=[SEL])
    Mr, Mi_ = [None] * 10, [None] * 10
    Mr[0] = ck8
    Mi_[0] = ts(T(), sk8, -1.0, ALU.mult)
    for k in range(1, 10):
        Mr[k], Mi_[k] = T(), T()
        tt(t0, Mi_[k - 1], Mi_[k - 1], ALU.mult)
        tt(Mr[k], Mr[k - 1], Mr[k - 1], ALU.mult)
        tt(Mr[k], Mr[k], t0, ALU.subtract)
        tt(Mi_[k], Mr[k - 1], Mi_[k - 1], ALU.mult)
        ts(Mi_[k], Mi_[k], 2.0, ALU.mult)

    NG = 8
    NP = NG // 2
    def pairpack(X):
        Xp = T((128, 32))
        Xv = X[:].rearrange("p (g two) -> p g two", two=2)
        p.op("dve", lambda e: e.tensor_copy(out=Xp[0:64, :], in_=Xv[0:64, :, 0]), [X], [Xp])
        p.op("dve", lambda e: e.tensor_copy(out=Xp[64:128, :], in_=Xv[64:128, :, 1]), [X], [Xp])
        return Xp

    ek8p = pairpack(ek8)
    Mrp = [pairpack(Mr[k]) for k in range(10)]
    Mip = [pairpack(Mi_[k]) for k in range(10)]
    zpp = p.sb("zpp", [128, NG, 15, 16], BF16)
    p.op("pool", lambda e: e.memset(zpp[:], 0.0), [], [zpp])
    tz = [p.sb("tz%d" % i, [128, NG, 16], F32) for i in range(4)]
    Md = p.sb("Md", [128, NG, 256], BF16)
    p.op("pool", lambda e: e.memset(Md[:], 0.0), [], [Md])
    Mi = p.sb("Mi", [128, NG, 128], BF16)
    MoR = p.sb("MoR", [128, NG, 128], BF16)
    MoI = p.sb("MoI", [128, NG, 128], BF16)
    G = p.sb("G", [128, 2, NP, NCH], F32)
    Hb = p.sb("Hb", [128, 2, NP, 512], BF16)
    Er = p.sb("Er", [128, NP, NCH], F32)
    Ei = p.sb("Ei", [128, NP, NCH], F32)
    tE = [p.sb("tE%d" % i, [128, NP, 256], F32) for i in range(2)]
    gm = [p.sb("gm%d" % i, [128, 2, NCH], F32) for i in range(2)]
    gsn = [p.sb("gsn%d" % i, [128, 2, NCH], F32) for i in range(2)]
    ta = [p.sb("ta%d" % i, [128, NCH], F32) for i in range(4)]
    uf = [p.sb("uf%d" % i, [128, NCH], F32) for i in range(2)]
    ub = [p.sb("ub%d" % i, [128, NCH], BF16) for i in range(NG)]
    yo = [p.sb("yo%d" % i, [128, 512], F32) for i in range(2)]
    ptp = p.ps("ptp", [128, 128], BF16)
    psI = p.ps("psI", [128, 128], F32)
    pg = [p.ps("pg%d" % i, [128, 512], F32) for i in range(4)]
    pgi = [0]

    for ps_ in range(64 // NG):
        g0 = ps_ * NG
        pp0 = g0 // 2
        gs_ = slice(g0, g0 + NG)

        def bc(t_):
            return t_[:, gs_].unsqueeze(2).broadcast_to([128, NG, 16])

        for m in range(8):
            k = 7 - m
            p.op("dve", lambda e: e.tensor_tensor(out=tz[0][:], in0=BR[:, gs_, :], in1=bc(al[k]), op=ALU.mult), [BR, al[k]], [tz[0]])
            p.op("pool", lambda e: e.tensor_tensor(out=tz[1][:], in0=BI[:, gs_, :], in1=bc(be[k]), op=ALU.mult), [BI, be[k]], [tz[1]])
            p.op("dve", lambda e: e.tensor_tensor(out=zpp[:, :, m, :], in0=tz[0][:], in1=tz[1][:], op=ALU.add), [tz[0], tz[1]], [zpp])
        for t in range(8):
            k = t + 1
            mo_r = MoR[:, :, t * 16:(t + 1) * 16]
            mo_i = MoI[:, :, t * 16:(t + 1) * 16]
            p.op("dve", lambda e: e.tensor_tensor(out=tz[0][:], in0=CR[:, gs_, :], in1=bc(lr[k]), op=ALU.mult), [CR, lr[k]], [tz[0]])
            p.op("pool", lambda e: e.tensor_tensor(out=tz[1][:], in0=CI[:, gs_, :], in1=bc(nli[k]), op=ALU.mult), [CI, nli[k]], [tz[1]])
            p.op("dve", lambda e: e.tensor_tensor(out=tz[0][:], in0=tz[0][:], in1=tz[1][:], op=ALU.add), [tz[0], tz[1]], [tz[0]])
            p.op("dve", lambda e: e.tensor_tensor(out=mo_r, in0=tz[0][:], in1=bc(MK), op=ALU.mult), [tz[0], MK], [MoR])
            p.op("pool", lambda e: e.tensor_tensor(out=tz[2][:], in0=CR[:, gs_, :], in1=bc(nli[k]), op=ALU.mult), [CR, nli[k]], [tz[2]])
            p.op("dve", lambda e: e.tensor_tensor(out=tz[3][:], in0=CI[:, gs_, :], in1=bc(nlr[k]), op=ALU.mult), [CI, nlr[k]], [tz[3]])
            p.op("pool", lambda e: e.tensor_tensor(out=tz[2][:], in0=tz[2][:], in1=tz[3][:], op=ALU.add), [tz[2], tz[3]], [tz[2]])
            p.op("pool", lambda e: e.tensor_tensor(out=mo_i, in0=tz[2][:], in1=bc(MK), op=ALU.mult), [tz[2], MK], [MoI])
        for gl in range(NG):
            g = g0 + gl
            gp, h = gl // 2, gl % 2
            z = zpp
            zf = zpp[:, gl].rearrange("p m j -> p (m j)")
            p.op("pe", lambda e: e.transpose(out=ptp[:], in_=zf[:, 0:128], identity=IDB[:]), [z, IDB], [ptp])
            p.op("act", lambda e: e.copy(out=Md[:, gl, 64:128], in_=ptp[:, 0:64]), [ptp], [Md])
            p.op("act", lambda e: e.copy(out=Md[:, gl, 192:256], in_=ptp[:, 64:128]), [ptp], [Md])
            for t in range(8):
                p.op("pe", lambda e, t=t: e.matmul(psI[:, t * 16:(t + 1) * 16], lhsT=zf[:, (7 - t) * 16:(15 - t) * 16], rhs=cst[:, g, :],
                                                  start=True, stop=True), [z, cst], [psI])
            p.op("dve", lambda e: e.scalar_tensor_tensor(out=Mi[:, gl, :], in0=IDF[:], scalar=DQ[:, g:g + 1], in1=psI[:],
                                                         op0=ALU.mult, op1=ALU.add), [IDF, DQ, psI], [Mi])
            u_f, u_b = uf[gl % 2], ub[gl]
            p.dma("sp", u_f[:], U[g], [U], [u_f])
            p.op("act", lambda e: e.copy(out=u_b[:], in_=u_f[:]), [u_f], [u_b])
            for c in range(2):
                for (n0, nn) in ((0, 512), (512, 32)):
                    ps = pg[pgi[0] % 4]
                    pgi[0] += 1
                    if h == 0:
                        p.op("pe", lambda e: e.matmul(ps[0:64, 0:nn], lhsT=Md[:, gl, 64 + 128 * c:128 + 128 * c], rhs=u_b[:, n0:n0 + nn],
                                                      start=True, stop=True), [Md, u_b], [ps])
                        p.op("act", lambda e: e.copy(out=G[0:64, c, gp, n0:n0 + nn], in_=ps[0:64, 0:nn]), [ps], [G])
                    else:
                        p.op("pe", lambda e: e.matmul(ps[:, 0:nn], lhsT=Md[:, gl, 128 * c:128 * c + 128], rhs=u_b[:, n0:n0 + nn],
                                                      start=True, stop=True), [Md, u_b], [ps])
                        p.op("act", lambda e: e.copy(out=G[64:128, c, gp, n0:n0 + nn], in_=ps[64:128, 0:nn]), [ps], [G])
        p.op("pool", lambda e: e.memset(Er[:, :, 0:1], 1.0), [], [Er])
        p.op("pool", lambda e: e.memset(Ei[:, :, 0:1], 0.0), [], [Ei])
        for k in range(10):
            ln = 1 << k
            cn = min(ln, NCH - ln)
            if cn <= 0:
                break
            mr = Mrp[k][:, pp0:pp0 + NP].unsqueeze(2).broadcast_to([128, NP, cn])
            mi = Mip[k][:, pp0:pp0 + NP].unsqueeze(2).broadcast_to([128, NP, cn])
            p.op("dve", lambda e: e.tensor_tensor(out=tE[0][:, :, 0:cn], in0=Ei[:, :, 0:cn], in1=mi, op=ALU.mult), [Ei, Mip[k]], [tE[0]])
            p.op("pool", lambda e: e.tensor_tensor(out=tE[1][:, :, 0:cn], in0=Er[:, :, 0:cn], in1=mi, op=ALU.mult), [Er, Mip[k]], [tE[1]])
            p.op("dve", lambda e: e.tensor_tensor(out=Er[:, :, ln:ln + cn], in0=Er[:, :, 0:cn], in1=mr, op=ALU.mult), [Er, Mrp[k]], [Er])
            p.op("pool", lambda e: e.tensor_tensor(out=Ei[:, :, ln:ln + cn], in0=Ei[:, :, 0:cn], in1=mr, op=ALU.mult), [Ei, Mrp[k]], [Ei])
            p.op("dve", lambda e: e.tensor_tensor(out=Er[:, :, ln:ln + cn], in0=Er[:, :, ln:ln + cn], in1=tE[0][:, :, 0:cn], op=ALU.subtract), [Er, tE[0]], [Er])
            p.op("pool", lambda e: e.tensor_tensor(out=Ei[:, :, ln:ln + cn], in0=Ei[:, :, ln:ln + cn], in1=tE[1][:, :, 0:cn], op=ALU.add), [Ei, tE[1]], [Ei])
        for gp in range(NP):
            gm_, gsc = gm[gp % 2], gsn[gp % 2]
            er, ei = Er[:, gp, :], Ei[:, gp, :]
            gre, gim = G[:, 0, gp, :], G[:, 1, gp, :]
            p.op("dve", lambda e: e.tensor_tensor(out=ta[0][:], in0=er, in1=gre, op=ALU.mult), [Er, G], [ta[0]])
            p.op("dve", lambda e: e.tensor_tensor(out=ta[1][:], in0=ei, in1=gim, op=ALU.mult), [Ei, G], [ta[1]])
            p.op("dve", lambda e: e.tensor_tensor(out=gm_[:, 0, :], in0=ta[0][:], in1=ta[1][:], op=ALU.subtract), [ta[0], ta[1]], [gm_])
            p.op("pool", lambda e: e.tensor_tensor(out=ta[2][:], in0=er, in1=gim, op=ALU.mult), [Er, G], [ta[2]])
            p.op("pool", lambda e: e.tensor_tensor(out=ta[3][:], in0=ei, in1=gre, op=ALU.mult), [Ei, G], [ta[3]])
            p.op("pool", lambda e: e.tensor_tensor(out=gm_[:, 1, :], in0=ta[2][:], in1=ta[3][:], op=ALU.add), [ta[2], ta[3]], [gm_])
            rb = ek8p[:, pp0 + gp:pp0 + gp + 1].broadcast_to([128, NCH])
            for c in range(2):
                p.op("dve", lambda e, c=c: e.tensor_tensor_scan(out=gsc[:, c, :], data0=rb, data1=gm_[:, c, :], initial=0.0,
                                                              op0=ALU.mult, op1=ALU.add), [gm_, ek8p], [gsc])
            sl = slice(31, 543)
            p.op("dve", lambda e: e.tensor_tensor(out=ta[0][:, sl], in0=er[:, sl], in1=gsc[:, 0, sl], op=ALU.mult), [Er, gsc], [ta[0]])
            p.op("dve", lambda e: e.tensor_tensor(out=ta[1][:, sl], in0=ei[:, sl], in1=gsc[:, 1, sl], op=ALU.mult), [Ei, gsc], [ta[1]])
            p.op("dve", lambda e: e.tensor_tensor(out=Hb[:, 0, gp, :], in0=ta[0][:, sl], in1=ta[1][:, sl], op=ALU.add), [ta[0], ta[1]], [Hb])
            p.op("pool", lambda e: e.tensor_tensor(out=ta[2][:, sl], in0=er[:, sl], in1=gsc[:, 1, sl], op=ALU.mult), [Er, gsc], [ta[2]])
            p.op("pool", lambda e: e.tensor_tensor(out=ta[3][:, sl], in0=ei[:, sl], in1=gsc[:, 0, sl], op=ALU.mult), [Ei, gsc], [ta[3]])
            p.op("pool", lambda e: e.tensor_tensor(out=Hb[:, 1, gp, :], in0=ta[2][:, sl], in1=ta[3][:, sl], op=ALU.subtract), [ta[2], ta[3]], [Hb])
        for gl in range(NG):
            g = g0 + gl
            gp = gl // 2
            ps = pg[pgi[0] % 4]
            pgi[0] += 1
            p.op("pe", lambda e: e.matmul(ps[:, :], lhsT=Mi[:, gl, :], rhs=ub[gl][:, 32:NCH], start=True, stop=False), [Mi, ub[gl]], [ps])
            p.op("pe", lambda e: e.matmul(ps[:, :], lhsT=MoR[:, gl, :], rhs=Hb[:, 0, gp, :], start=False, stop=False), [MoR, Hb], [ps])
            p.op("pe", lambda e: e.matmul(ps[:, :], lhsT=MoI[:, gl, :], rhs=Hb[:, 1, gp, :], start=False, stop=True), [MoI, Hb], [ps])
            y_ = yo[gl % 2]
            p.op("act", lambda e: e.copy(out=y_[:], in_=ps[:, :]), [ps], [y_])
            p.dma("pool", Y[g], y_[:], [y_], [Y])
    return p.finish()


def l6_inputs(useq, d, od_a_re, od_a_im, od_log_step, od_b_re, od_b_im, od_c_re, od_c_im, od_d, with_skip):
    U = np.ascontiguousarray(f32(useq).reshape(NCH, 8, 64, 16).transpose(2, 1, 3, 0).reshape(64, 128, NCH))
    dup = lambda a: np.concatenate([a, a], axis=0)
    ar = dup(f32(od_a_re)[d].T)
    ai = dup(f32(od_a_im)[d].T)
    ls = np.broadcast_to(f32(od_log_step)[d][None, :], (128, 64))
    prm = np.ascontiguousarray(np.stack([ar, ai, ls]))
    br = dup(f32(od_b_re)[d].transpose(1, 0, 2))
    bi = dup(f32(od_b_im)[d].transpose(1, 0, 2))
    cr = dup(f32(od_c_re)[d].transpose(2, 0, 1))
    ci = dup(f32(od_c_im)[d].transpose(2, 0, 1))
    sel = np.zeros((128, 2), np.float32)
    sel[:64, 0] = 1.0
    sel[64:, 1] = 1.0
    dq = np.zeros((128, 64), np.float32)
    if with_skip:
        dq[:] = np.tile(f32(od_d).reshape(64, 16).T, (8, 1))
    mk = np.zeros((128, 64), np.float32)
    mk[:64, 0::2] = 1.0
    mk[64:, 1::2] = 1.0
    return {"mk": mk, "U": U, "prm": prm, "bri": np.ascontiguousarray(np.stack([br, bi])), "cri": np.ascontiguousarray(np.stack([cr, ci])),
            "sel": sel, "dq": dq, "identf": np.eye(128, dtype=np.float32), "ident": np.eye(128, dtype=np.float32).astype(NPBF)}


def l6_unpack(Y):
    return np.ascontiguousarray(np.asarray(Y).reshape(64, 8, 16, 512).transpose(3, 1, 0, 2).reshape(4096, 1024))


T7 = 2048


def build_l7():
    p = Prog()
    nc = p.nc
    x = p.dram("x", [T7, D], F32, "ExternalInput")
    yf = p.dram("yf", [T7, D], F32, "ExternalInput")
    yr = p.dram("yr", [T7, D], F32, "ExternalInput")
    wglu = p.dram("wglu", [D, 2048], F32, "ExternalInput")
    g1_d = p.dram("g1bc", [128, D], F32, "ExternalInput")
    g2_d = p.dram("g2bc", [128, D], F32, "ExternalInput")
    fg_d = p.dram("fgbc", [128, D], F32, "ExternalInput")
    gsT_d = p.dram("gsT", [128, 8], F32, "ExternalInput")
    shT_d = p.dram("shT", [128, 8], F32, "ExternalInput")
    wr_d = p.dram("wr", [128, 8, 8], F32, "ExternalInput")
    wge = p.dram("wge", [8, 28, 128, 8, 128], F32, "ExternalInput")
    wue = p.dram("wue", [8, 28, 128, 8, 128], F32, "ExternalInput")
    wde = p.dram("wde", [8, 3584, D], F32, "ExternalInput")
    ident_d = p.dram("ident", [128, 128], BF16, "ExternalInput")
    identf_d = p.dram("identf", [128, 128], F32, "ExternalInput")
    xm = p.dram("xm", [T7, D], F32, "Internal")
    out = p.dram("out", [T7, D], F32, "ExternalOutput")
    xmb = [Buf("xm%d" % i) for i in range(4)]
    pt = [p.ps("pt%d" % i, [128, 1024], BF16) for i in range(2)]
    ptf = p.ps("ptf", [128, 1024], F32)
    pp = [p.ps("pp%d" % i, [128, 512], F32) for i in range(4)]
    ppi = [0]

    def banks():
        ppi[0] += 1
        return pp[ppi[0] % 4]

    outer = p.es
    p.es = ExitStack()
    ident = p.sb("ident", [128, 128], BF16)
    p.dma("sp", ident[:], ident_d[:], [ident_d], [ident])
    g1 = p.sb("g1", [128, D], F32)
    p.dma("sp", g1[:], g1_d[:], [g1_d], [g1])
    stg = Stage(p, 2048)
    WG = load_w(p, stg, "WGLU", wglu, 8, 2048)
    ya = [p.sb("ya%d" % i, [128, D], F32) for i in range(2)]
    yb = [p.sb("yb%d" % i, [128, D], F32) for i in range(2)]
    y2s = [p.sb("y2_%d" % i, [128, D], F32) for i in range(2)]
    gls = [p.sb("gl_%d" % i, [128, D], BF16) for i in range(2)]
    glTs = [p.sb("glT_%d" % i, [128, 8, 128], BF16) for i in range(2)]
    xss = [p.sb("xsA_%d" % i, [128, D], F32) for i in range(2)]
    sgms = [p.sb("sgm_%d" % i, [128, 512], F32) for i in range(2)]
    ots = [p.sb("ot_%d" % i, [128, 512], F32) for i in range(2)]
    for ti in range(T7 // 128):
        a, b_ = ya[ti % 2], yb[ti % 2]
        y2, gl, glT, xs = y2s[ti % 2], gls[ti % 2], glTs[ti % 2], xss[ti % 2]
        rs = slice(ti * 128, (ti + 1) * 128)
        p.dma("sp", a[:], yf[rs, :], [yf], [a])
        p.dma("sp", b_[:], yr[rs, :], [yr], [b_])
        p.dma("sp", xs[:], x[rs, :], [x], [xs])
        p.op("pool", lambda e: e.tensor_tensor(out=a[:], in0=a[:], in1=b_[:], op=ALU.add), [a, b_], [a])
        p.op("pool", lambda e: e.tensor_tensor(out=y2[:], in0=a[:], in1=a[:], op=ALU.mult), [a], [y2])
        p.op("dve", lambda e: e.tensor_scalar(out=y2[:], in0=y2[:], scalar1=0.044715, scalar2=1.0, op0=ALU.mult, op1=ALU.add), [y2], [y2])
        p.op("dve", lambda e: e.tensor_tensor(out=y2[:], in0=y2[:], in1=a[:], op=ALU.mult), [y2, a], [y2])
        p.op("act", lambda e: e.activation(out=y2[:], in_=y2[:], func=AF.Tanh, scale=0.7978845608028654), [y2], [y2])
        p.op("dve", lambda e: e.scalar_tensor_tensor(out=gl[:], in0=y2[:], scalar=1.0, in1=a[:], op0=ALU.add, op1=ALU.mult), [y2, a], [gl])
        ptt = pt[ti % 2]
        for c in range(8):
            p.op("pe", lambda e, c=c: e.transpose(out=ptt[:, c * 128:(c + 1) * 128], in_=gl[:, c * 128:(c + 1) * 128], identity=ident[:]), [gl, ident], [ptt])
        p.op("act", lambda e: e.copy(out=glT[:, 0:4, :], in_=ptt[:, 0:512].rearrange("p (c t) -> p c t", c=4)), [ptt], [glT])
        p.op("dve", lambda e: e.tensor_copy(out=glT[:, 4:8, :], in_=ptt[:, 512:1024].rearrange("p (c t) -> p c t", c=4)), [ptt], [glT])
        for cb in range(2):
            sgm, ot = sgms[cb], ots[cb]
            pa, pb = banks(), banks()
            for (ps, off) in ((pa, cb * 512), (pb, 1024 + cb * 512)):
                for c in range(8):
                    p.op("pe", lambda e, c=c, ps=ps, off=off: e.matmul(ps[:], lhsT=glT[:, c, :], rhs=WG[:, c, off:off + 512],
                                                                      start=(c == 0), stop=(c == 7)), [glT, WG], [ps])
            p.op("act", lambda e: e.activation(out=sgm[:], in_=pb[:], func=AF.Sigmoid, scale=0.5), [pb], [sgm])
            p.op("dve", lambda e: e.scalar_tensor_tensor(out=ot[:], in0=pa[:], scalar=0.5, in1=sgm[:], op0=ALU.mult, op1=ALU.mult), [pa, sgm], [ot])
            p.op("pool", lambda e: e.tensor_tensor(out=ot[:], in0=ot[:], in1=g1[:, cb * 512:(cb + 1) * 512], op=ALU.mult), [ot, g1], [ot])
            p.op("pool", lambda e: e.tensor_tensor(out=xs[:, cb * 512:(cb + 1) * 512], in0=xs[:, cb * 512:(cb + 1) * 512], in1=ot[:], op=ALU.add), [xs, ot], [xs])
        p.dma("pool", xm[rs, :], xs[:], [xs], [xmb[ti // 4]])
    p.barrier()
    p.es.close()
    p.es = ExitStack()
    identf = p.sb("identf", [128, 128], F32)
    p.dma("sp", identf[:], identf_d[:], [identf_d], [identf])
    g2 = p.sb("g2", [128, D], F32)
    fg = p.sb("fg", [128, D], F32)
    gsT = p.sb("gsT", [128, 8], F32)
    shT = p.sb("shT", [128, 8], F32)
    wr = p.sb("wr", [128, 8, 8], F32)
    for a, b_ in ((g2, g2_d), (fg, fg_d), (gsT, gsT_d), (shT, shT_d), (wr, wr_d)):
        p.dma("sp", a[:], b_[:], [b_], [a])
    eps = mk_eps(p)
    stg = Stage(p, 512, n=3, name="stgB")
    pan = Panels(p)
    NTB = 8
    actT = p.sb("actT", [128, 14, 128 * NTB], BF16)
    WDh = [p.sb("WDh%d" % i, [128, 14, 512], BF16) for i in range(2)]
    hT = p.sb("hTB", [128, 8, 128 * NTB], BF16)
    hT32 = p.sb("hT32", [128, 8, 128], F32)
    xs = p.sb("xsB", [128, NTB, D], F32)
    xn = p.sb("xnB", [128, D], F32)
    junk = p.sb("junkB", [128, D], BF16)
    ss = p.sb("ssB", [128, 4], F32)
    lg = p.sb("lg", [128, 8], F32)
    mx = p.sb("mx", [128, 8], F32)
    e1 = p.sb("e1", [128, 8], F32)
    e2 = p.sb("e2", [128, 8], F32)
    w12 = p.sb("w12", [128, 4], F32)
    comb = p.sb("comb", [128, NTB, 8], F32)
    sg = [p.sb("sgB%d" % i, [128, 512], F32) for i in range(2)]
    tm = [p.sb("tmB%d" % i, [128, 512], F32) for i in range(2)]
    ob = [p.sb("ob%d" % i, [128, D], F32) for i in range(1)]
    tmi = [0]
    wdi = [0]
    for blk in range(T7 // (128 * NTB)):
        c0 = blk * 128 * NTB
        for q4 in range(NTB // 4):
            p.dma("sp", xs[:, q4 * 4:(q4 + 1) * 4, :], xm[c0 + q4 * 512:c0 + (q4 + 1) * 512, :].rearrange("(t p) f -> p t f", p=128),
                  [xmb[(c0 + q4 * 512) // 512]], [xs])
        for ti in range(NTB):
            xt = xs[:, ti, :]
            p.op("act", lambda e: e.activation(out=junk[:], in_=xt, func=AF.Square, accum_out=ss[:, 0:1]), [xs], [junk, ss])
            p.op("act", lambda e: e.activation(out=ss[:, 1:2], in_=ss[:, 0:1], func=AF.Sqrt, scale=1.0 / D, bias=eps[:, 0:1]), [ss, eps], [ss])
            p.op("dve", lambda e: e.reciprocal(out=ss[:, 2:3], in_=ss[:, 1:2]), [ss], [ss])
            p.op("dve", lambda e: e.tensor_scalar(out=xn[:], in0=xt, scalar1=ss[:, 2:3], scalar2=None, op0=ALU.mult), [xs, ss], [xn])
            for c in range(8):
                p.op("pe", lambda e, c=c: e.transpose(out=ptf[:, c * 128:(c + 1) * 128], in_=xn[:, c * 128:(c + 1) * 128], identity=identf[:]), [xn, identf], [ptf])
            for c in range(8):
                p.op("dve", lambda e, c=c: e.tensor_scalar(out=hT32[:, c, :], in0=ptf[:, c * 128:(c + 1) * 128], scalar1=gsT[:, c:c + 1],
                                                           scalar2=shT[:, c:c + 1], op0=ALU.mult, op1=ALU.add), [ptf, gsT, shT], [hT32])
            p.op("pool", lambda e: e.tensor_copy(out=hT[:, :, ti * 128:(ti + 1) * 128], in_=hT32[:]), [hT32], [hT])
            ps = banks()
            for c in range(8):
                p.op("pe", lambda e, c=c: e.matmul(ps[:, 0:8], lhsT=hT32[:, c, :], rhs=wr[:, c, :], start=(c == 0), stop=(c == 7)), [hT32, wr], [ps])
            p.op("dve", lambda e: e.tensor_copy(out=lg[:], in_=ps[:, 0:8]), [ps], [lg])
            p.op("dve", lambda e: e.max(out=mx[:], in_=lg[:]), [lg], [mx])
            p.op("dve", lambda e: e.tensor_tensor(out=w12[:, 0:1], in0=mx[:, 0:1], in1=mx[:, 1:2], op=ALU.subtract), [mx], [w12])
            p.op("act", lambda e: e.activation(out=w12[:, 1:2], in_=w12[:, 0:1], func=AF.Sigmoid), [w12], [w12])
            p.op("dve", lambda e: e.tensor_scalar(out=w12[:, 2:3], in0=w12[:, 1:2], scalar1=-1.0, scalar2=1.0, op0=ALU.mult, op1=ALU.add), [w12], [w12])
            p.op("dve", lambda e: e.tensor_scalar(out=e1[:], in0=lg[:], scalar1=mx[:, 0:1], scalar2=w12[:, 1:2], op0=ALU.is_equal, op1=ALU.mult), [lg, mx, w12], [e1])
            p.op("dve", lambda e: e.tensor_scalar(out=e2[:], in0=lg[:], scalar1=mx[:, 1:2], scalar2=w12[:, 2:3], op0=ALU.is_equal, op1=ALU.mult), [lg, mx, w12], [e2])
            p.op("dve", lambda e: e.tensor_tensor(out=comb[:, ti, :], in0=e1[:], in1=e2[:], op=ALU.add), [e1, e2], [comb])
        for ex in range(8):
            for fh in range(2):
                wd0, wd1 = WDh[0], WDh[1]

                def ldwd(f, wd_, cb):
                    r0 = (fh * 14 + f) * 128
                    stg.load(wd_[:, f, :], wd_.b, wde[ex, r0:r0 + 128, cb * 512:(cb + 1) * 512], wde.b, 128, 512)

                ffn_phase1(p, pan, banks, hT, 128 * NTB, Tile(wge[ex], wge.b), Tile(wue[ex], wue.b), 14, actT, sg, f0=fh * 14,
                           between=lambda f: ldwd(f, wd0, 0))
                for cb in range(2):
                    wd_ = WDh[cb]
                    if cb == 1:
                        for f in range(14):
                            ldwd(f, wd1, 1)
                    for ti in range(NTB):
                        ps = banks()
                        for f in range(14):
                            p.op("pe", lambda e, f=f, ps=ps: e.matmul(ps[:], lhsT=actT[:, f, ti * 128:(ti + 1) * 128], rhs=wd_[:, f, :],
                                                                     start=(f == 0), stop=(f == 13)), [actT, wd_], [ps])
                        t = tm[tmi[0] % 2]
                        tmi[0] += 1
                        p.op("dve", lambda e: e.scalar_tensor_tensor(out=t[:], in0=ps[:], scalar=comb[:, ti, ex:ex + 1], in1=g2[:, cb * 512:(cb + 1) * 512],
                                                                     op0=ALU.mult, op1=ALU.mult), [ps, comb, g2], [t])
                        p.op("pool", lambda e: e.tensor_tensor(out=xs[:, ti, cb * 512:(cb + 1) * 512], in0=xs[:, ti, cb * 512:(cb + 1) * 512],
                                                               in1=t[:], op=ALU.add), [xs, t], [xs])
        for ti in range(NTB):
            xt = xs[:, ti, :]
            o_ = ob[ti % len(ob)]
            p.op("act", lambda e: e.activation(out=junk[:], in_=xt, func=AF.Square, accum_out=ss[:, 0:1]), [xs], [junk, ss])
            p.op("act", lambda e: e.activation(out=ss[:, 1:2], in_=ss[:, 0:1], func=AF.Sqrt, scale=1.0 / D, bias=eps[:, 0:1]), [ss, eps], [ss])
            p.op("dve", lambda e: e.reciprocal(out=ss[:, 2:3], in_=ss[:, 1:2]), [ss], [ss])
            p.op("dve", lambda e: e.scalar_tensor_tensor(out=o_[:], in0=xt, scalar=ss[:, 2:3], in1=fg[:], op0=ALU.mult, op1=ALU.mult), [xs, ss, fg], [o_])
            p.dma("pool", out[c0 + ti * 128:c0 + (ti + 1) * 128, :], o_[:], [o_], [out])
    p.es.close()
    p.es = outer
    return p.finish()


def kernel(x, c, ctx, c_ctx, mod_w, mod_b, norm1_g, norm2_g,
           ev_w_in, ev_q_norm_g, ev_w_qb, ev_kv_norm_g, ev_w_kvb, ev_na_rpb, ev_w_out,
           ev_ffn_w_gate, ev_ffn_w_up, ev_ffn_w_down,
           od_w_in, od_a_re, od_a_im, od_log_step, od_b_re, od_b_im, od_c_re, od_c_im, od_d, od_w_glu,
           moe_w_router, moe_w_gate, moe_w_up, moe_w_down, final_g):
    x = f32(x)
    ctx = f32(ctx)
    m, gs = run_adaln(c, c_ctx, mod_w, mod_b, norm1_g, norm2_g)
    identb = np.eye(128, dtype=np.float32).astype(NPBF)
    identf = np.eye(128, dtype=np.float32)
    cores = [(i // 2, i % 2) for i in range(NCORES)]

    def mods(layer, b, lo_g, lo_s):
        return (np.ascontiguousarray(np.stack([fm(gs[layer, b, lo_g:lo_g + D], 8), fm(gs[layer, 4, lo_g:lo_g + D], 8)], axis=2)),
                np.ascontiguousarray(np.stack([fm(m[layer, b, lo_s:lo_s + D], 8), fm(m[layer, 4, lo_s:lo_s + D], 8)], axis=2)))

    in2 = []
    for (b, hf) in cores:
        xtok = np.concatenate([x[b, hf * 2048:(hf + 1) * 2048], ctx[b, hf * 128:(hf + 1) * 128]], 0)
        pos = np.concatenate([np.arange(hf * 2048, (hf + 1) * 2048), -np.ones(128, np.int64)])
        in2.append(l2_inputs(xtok, pos, gs[0, b, 1024:2048], m[0, b, 0:1024], gs[0, 4, 1024:2048], m[0, 4, 0:1024],
                             ev_w_in[0], ev_q_norm_g[0], ev_w_qb[0], ev_kv_norm_g[0], ev_w_kvb[0]))
    res2 = run_prog(build_l2(), in2)
    res2 = [{k: np.asarray(v) for k, v in r.items()} for r in res2]
    in3 = [l3_inputs(b, hf, res2, ev_na_rpb[0]) for (b, hf) in cores]
    res3 = run_prog(build_l3(), in3)
    in4 = []
    for i, (b, hf) in enumerate(cores):
        gsT, shT = mods(0, b, 4096, 3072)
        in4.append({"x": in2[i]["x"], "aT": np.ascontiguousarray(np.asarray(res3[i]["AO"]).T), "wo": f32(ev_w_out[0]),
                    "wg": pretile(ev_ffn_w_gate[0]), "wu": pretile(ev_ffn_w_up[0]), "wd": f32(ev_ffn_w_down[0]),
                    "g1bc": np.stack([bc128(m[0, b, 2048:3072]), bc128(m[0, 4, 2048:3072])]),
                    "g2bc": np.stack([bc128(m[0, b, 5120:6144]), bc128(m[0, 4, 5120:6144])]),
                    "gsT": gsT, "shT": shT, "ident": identb})
    res4 = run_prog(build_l4(), in4)
    xo = [np.asarray(r["xo"]) for r in res4]
    in5 = []
    for i, (b, hf) in enumerate(cores):
        gsT, shT = mods(1, b, 1024, 0)
        in5.append({"x": xo[i], "wi": f32(od_w_in[0]), "gsT": gsT, "shT": shT, "ident": identb})
    res5 = run_prog(build_l5(), in5)
    u = [np.asarray(r["u"]) for r in res5]
    in6 = []
    for i in range(NCORES):
        b, dr = i // 2, i % 2
        u0, u1 = u[2 * b], u[2 * b + 1]
        lat = np.concatenate([u0[:2048], u1[:2048]], 0)
        cx = np.concatenate([u0[2048:], u1[2048:]], 0)
        seq = np.concatenate([cx, lat], 0) if dr == 0 else np.concatenate([cx[::-1], lat[::-1]], 0)
        in6.append(l6_inputs(seq, dr, od_a_re[0], od_a_im[0], od_log_step[0], od_b_re[0], od_b_im[0],
                             od_c_re[0], od_c_im[0], od_d[0], dr == 0))
    res6 = run_prog(build_l6(), in6)
    Y = [l6_unpack(r["Y"]) for r in res6]
    in7 = []
    wr = np.ascontiguousarray(f32(moe_w_router[0]).reshape(8, 128, 8).transpose(1, 0, 2))
    wge_t = np.stack([pretile(moe_w_gate[0][e]) for e in range(8)])
    wue_t = np.stack([pretile(moe_w_up[0][e]) for e in range(8)])
    for i, (b, hf) in enumerate(cores):
        yf = Y[2 * b][hf * 2048:(hf + 1) * 2048]
        yr = Y[2 * b + 1][::-1][hf * 2048:(hf + 1) * 2048]
        in7.append({"x": np.ascontiguousarray(xo[i][:2048]), "yf": np.ascontiguousarray(yf), "yr": np.ascontiguousarray(yr),
                    "wglu": f32(od_w_glu[0]), "g1bc": bc128(m[1, b, 2048:3072]), "g2bc": bc128(m[1, b, 5120:6144]),
                    "fgbc": bc128(final_g), "gsT": fm(gs[1, b, 4096:5120], 8), "shT": fm(m[1, b, 3072:4096], 8),
                    "wr": wr, "wge": wge_t, "wue": wue_t, "wde": f32(moe_w_down[0]),
                    "ident": identb, "identf": identf})
    res7 = run_prog(build_l7(), in7)
    out = np.zeros((B, L, D), np.float32)
    for i, (b, hf) in enumerate(cores):
        out[b, hf * 2048:(hf + 1) * 2048] = np.asarray(res7[i]["out"])
    return out
```

```python
import numpy as np
import concourse.bass as bass
import concourse.mybir as mybir
from concourse.bass_utils import run_bass_kernel_spmd
from contextlib import ExitStack
import ml_dtypes

F32 = mybir.dt.float32
BF16 = mybir.dt.bfloat16
I32 = mybir.dt.int32
U32 = mybir.dt.uint32
AF = mybir.ActivationFunctionType
ALU = mybir.AluOpType
AX = mybir.AxisListType
NPBF = ml_dtypes.bfloat16

NCORES = 8
D = 1024
B = 4
L = 4096
LC = 256
EPS = 1e-6


class Buf:
    __slots__ = ("w", "rs", "name")

    def __init__(self, name=""):
        self.w = None
        self.rs = {}
        self.name = name


class Tile:
    __slots__ = ("t", "b")

    def __init__(self, t, b):
        self.t = t
        self.b = b

    def __getitem__(self, idx):
        return self.t[idx]


COMPUTE = ("pe", "act", "dve", "pool")


class View:
    def __init__(self, ap, b):
        self.ap = ap
        self.b = b

    def __getitem__(self, idx):
        return self.ap


class Prog:
    def __init__(self, ndma=8):
        self.nc = bass.Bass("TRN2", target_bir_lowering=False)
        nc = self.nc
        self.es = ExitStack()
        self.eng = {"pe": nc.tensor, "act": nc.scalar, "dve": nc.vector,
                    "pool": nc.gpsimd, "sp": nc.sync}
        self.sems = {}
        self.cnt = {}
        for e in COMPUTE:
            self.sems[e] = self.es.enter_context(nc.semaphore("c_" + e))
            self.cnt[e] = 0
        self.waited = {e: {} for e in self.eng}
        self.dpool = {}
        self.didx = {}
        self.dval = {}
        for q in ("sp", "act", "pool"):
            n = ndma if q == "sp" else 4
            self.dpool[q] = []
            for i in range(n):
                k = ("d", q, i)
                self.sems[k] = self.es.enter_context(nc.semaphore("d_%s_%d" % (q, i)))
                self.dval[k] = 0
                self.dpool[q].append(k)
            self.didx[q] = 0
        self.ndram = 0

    def sb(self, name, shape, dt):
        t = self.es.enter_context(self.nc.sbuf_tensor("s_" + name, list(shape), dt))
        return Tile(t, Buf(name))

    def ps(self, name, shape, dt):
        t = self.es.enter_context(self.nc.psum_tensor("p_" + name, list(shape), dt))
        return Tile(t, Buf(name))

    def dram(self, name, shape, dt, kind):
        t = self.nc.dram_tensor(name, list(shape), dt, kind=kind)
        return Tile(t.ap(), Buf(name))

    def _wait(self, e, reads, writes):
        need = {}
        for b in reads:
            if b.w is not None:
                k, v = b.w
                if not (k == e and e == "pe"):
                    need[k] = max(need.get(k, 0), v)
        for b in writes:
            if b.w is not None:
                k, v = b.w
                if not (k == e and e == "pe"):
                    need[k] = max(need.get(k, 0), v)
            for k, v in b.rs.items():
                if not (k == e and e == "pe"):
                    need[k] = max(need.get(k, 0), v)
        w = self.waited[e]
        for k, v in need.items():
            if w.get(k, 0) < v:
                self.eng[e].wait_ge(self.sems[k], v)
                w[k] = v

    def _mark(self, tok, reads, writes):
        k, v = tok
        for b in reads:
            if b.rs.get(k, 0) < v:
                b.rs[k] = v
        for b in writes:
            b.w = tok
            b.rs = {}

    def op(self, e, fn, reads=(), writes=()):
        reads = [r.b if isinstance(r, (Tile, View)) else r for r in reads]
        writes = [r.b if isinstance(r, (Tile, View)) else r for r in writes]
        self._wait(e, reads, writes)
        inst = fn(self.eng[e])
        self.cnt[e] += 1
        inst.then_inc(self.sems[e], 1)
        self._mark((e, self.cnt[e]), reads, writes)

    def dma(self, q, out, in_, reads=(), writes=(), **kw):
        reads = [r.b if isinstance(r, (Tile, View)) else r for r in reads]
        writes = [r.b if isinstance(r, (Tile, View)) else r for r in writes]
        pool = self.dpool[q]
        k = pool[self.didx[q] % len(pool)]
        self.didx[q] += 1
        pv = self.dval[k]
        w = self.waited[q]
        if pv and w.get(k, 0) < pv:
            self.eng[q].wait_ge(self.sems[k], pv)
            w[k] = pv
        self._wait(q, reads, writes)
        self.eng[q].dma_start(out=out, in_=in_, **kw).then_inc(self.sems[k], 16)
        self.dval[k] = pv + 16
        self._mark((k, pv + 16), reads, writes)

    def barrier(self):
        for e in self.eng:
            w = self.waited[e]
            for k in COMPUTE:
                v = self.cnt[k]
                if v and k != e and w.get(k, 0) < v:
                    self.eng[e].wait_ge(self.sems[k], v)
                    w[k] = v
            for k, v in self.dval.items():
                if v and w.get(k, 0) < v:
                    self.eng[e].wait_ge(self.sems[k], v)
                    w[k] = v

    def finish(self):
        for k, v in self.dval.items():
            if v and self.waited["sp"].get(k, 0) < v:
                self.nc.sync.wait_ge(self.sems[k], v)
        self.es.close()
        return self.nc


TIMES = []


def run_prog(nc, in_maps, trace=False):
    res = run_bass_kernel_spmd(nc, in_maps, core_ids=list(range(len(in_maps))), trace=trace)
    if trace:
        TIMES.append(res.exec_time_ns)
    return res.results


def f32(a):
    return np.ascontiguousarray(a, dtype=np.float32)


def build_adaln():
    p = Prog()
    cond = p.dram("cond", [128, 8, 5], F32, "ExternalInput")
    w = p.dram("w", [2, 1024, 768], F32, "ExternalInput")
    bias5 = p.dram("bias5", [2, 5, 768], F32, "ExternalInput")
    gain5 = p.dram("gain5", [2, 5, 768], F32, "ExternalInput")
    m_out = p.dram("m", [2, 5, 768], F32, "ExternalOutput")
    gs_out = p.dram("gs", [2, 5, 768], F32, "ExternalOutput")
    ct = p.sb("ct", [128, 8, 5], F32)
    st = p.sb("st", [128, 8, 5], F32)
    p.dma("sp", ct[:], cond[:], [cond], [ct])
    p.op("act", lambda e: e.activation(out=st[:], in_=ct[:], func=AF.Silu), [ct], [st])
    pss = [p.ps("ps%d" % i, [128, 512], F32) for i in range(2)]
    for l in range(2):
        wt = p.sb("wt%d" % l, [128, 8, 768], F32)
        bt = p.sb("bt%d" % l, [5, 768], F32)
        gt = p.sb("gt%d" % l, [5, 768], F32)
        mt = p.sb("mt%d" % l, [5, 768], F32)
        gst = p.sb("gst%d" % l, [5, 768], F32)
        p.dma("sp", wt[:], w[l].rearrange("(c p) n -> p c n", p=128), [w], [wt])
        p.dma("sp", bt[:], bias5[l], [bias5], [bt])
        p.dma("sp", gt[:], gain5[l], [gain5], [gt])
        for nb in range(2):
            ps = pss[nb]
            cs = slice(nb * 384, (nb + 1) * 384)
            for c in range(8):
                p.op("pe", lambda e, c=c, cs=cs, ps=ps: e.matmul(
                    ps[0:5, 0:384], lhsT=st[:, c, :], rhs=wt[:, c, cs],
                    start=(c == 0), stop=(c == 7)), [st, wt], [ps])
            p.op("dve", lambda e, cs=cs, ps=ps: e.tensor_tensor(
                out=mt[:, cs], in0=ps[0:5, 0:384], in1=bt[:, cs], op=ALU.add), [ps, bt], [mt])
        p.op("dve", lambda e: e.scalar_tensor_tensor(
            out=gst[:], in0=mt[:], scalar=1.0, in1=gt[:], op0=ALU.add, op1=ALU.mult), [mt, gt], [gst])
        p.dma("sp", m_out[l], mt[:], [mt], [m_out])
        p.dma("sp", gs_out[l], gst[:], [gst], [gs_out])
    return p.finish()


def run_adaln(c, c_ctx, mod_w, mod_b, norm1_g, norm2_g):
    cond_all = np.concatenate([f32(c), f32(c_ctx)[None]], 0)
    condT = np.ascontiguousarray(cond_all.T.reshape(8, 128, 5).transpose(1, 0, 2))
    gain = np.zeros((2, 6144), np.float32)
    gain[:, 1024:2048] = f32(norm1_g)
    gain[:, 4096:5120] = f32(norm2_g)
    in_maps = []
    for j in range(NCORES):
        cs = slice(768 * j, 768 * j + 768)
        in_maps.append({
            "cond": condT,
            "w": np.ascontiguousarray(f32(mod_w)[:, :, cs]),
            "bias5": np.ascontiguousarray(np.broadcast_to(f32(mod_b)[:, None, cs], (2, 5, 768))),
            "gain5": np.ascontiguousarray(np.broadcast_to(gain[:, None, cs], (2, 5, 768))),
        })
    res = run_prog(build_adaln(), in_maps)
    m = np.concatenate([r["m"] for r in res], axis=2)
    gs = np.concatenate([r["gs"] for r in res], axis=2)
    return m, gs


class Stage:
    def __init__(self, p, width, n=2, name="stg"):
        self.p = p
        self.slots = [p.sb("%s%d" % (name, i), [128, width], F32) for i in range(n)]
        self.i = 0
        self.ce = 0

    def load(self, dst_ap, dst_buf, src_ap, src_buf, rows, n, eng=None, scale=None):
        p = self.p
        s = self.slots[self.i % len(self.slots)]
        self.i += 1
        p.dma("sp", s[0:rows, 0:n], src_ap, [src_buf], [s])
        if scale is not None:
            sc_ap, sc_t = scale
            p.op("dve", lambda e: e.tensor_scalar(out=dst_ap, in0=s[0:rows, 0:n], scalar1=sc_ap, scalar2=None,
                                                  op0=ALU.mult), [s, sc_t], [dst_buf])
            return
        if eng is None:
            eng = ("pool", "dve")[self.ce % 2]
            self.ce += 1
        p.op(eng, lambda e: e.tensor_copy(out=dst_ap, in_=s[0:rows, 0:n]), [s], [dst_buf])


def load_w(p, stg, name, src, kc, n, eng=None, rowscale=None):
    dst = p.sb(name, [128, kc, n], BF16)
    for c in range(kc):
        sc = None if rowscale is None else (rowscale[:, c:c + 1], rowscale)
        stg.load(dst[:, c, :], dst.b, src[c * 128:(c + 1) * 128, :], src.b, 128, n, eng, sc)
    return dst


class NormT:
    def __init__(self, p, ident, nslots=2):
        self.p = p
        self.ident = ident
        self.xt = [p.sb("nx%d" % i, [128, 1024], F32) for i in range(nslots)]
        self.xn = [p.sb("nn%d" % i, [128, 1024], BF16) for i in range(nslots)]
        self.junk = p.sb("njunk", [128, 1024], BF16)
        self.ss = [p.sb("nss%d" % i, [128, 2], F32) for i in range(nslots)]
        self.pt = [p.ps("npt%d" % i, [128, 1024], BF16) for i in range(2)]
        self.i = 0
        self.eps = mk_eps(p)

    def run(self, x_ap, x_buf, gsT, shT, cond, hT, col0, x_loaded=None):
        p = self.p
        k = self.i % len(self.xt)
        self.i += 1
        xt, xn, ss, pt = self.xt[k], self.xn[k], self.ss[k], self.pt[k % 2]
        if x_loaded is None:
            p.dma("sp", xt[:], x_ap, [x_buf], [xt])
        else:
            xt = x_loaded
        p.op("act", lambda e: e.activation(out=self.junk[:], in_=xt[:], func=AF.Square,
                                           accum_out=ss[:, 0:1]), [xt], [self.junk, ss])
        p.op("act", lambda e: e.activation(out=ss[:, 1:2], in_=ss[:, 0:1], func=AF.Sqrt,
                                           scale=1.0 / D, bias=self.eps[:, 0:1]), [ss, self.eps], [ss])
        p.op("dve", lambda e: e.reciprocal(out=ss[:, 0:1], in_=ss[:, 1:2]), [ss], [ss])
        p.op("dve", lambda e: e.tensor_scalar(out=xn[:], in0=xt[:], scalar1=ss[:, 0:1], scalar2=None,
                                              op0=ALU.mult), [xt, ss], [xn])
        for c in range(8):
            p.op("pe", lambda e, c=c: e.transpose(out=pt[:, c * 128:(c + 1) * 128],
                                                   in_=xn[:, c * 128:(c + 1) * 128],
                                                   identity=self.ident[:]), [xn, self.ident], [pt])
        for c in range(8):
            p.op("dve" if c % 2 == 0 else "act",
                 (lambda e, c=c: e.tensor_scalar(out=hT[:, c, col0:col0 + 128], in0=pt[:, c * 128:(c + 1) * 128],
                                                 scalar1=gsT[:, c, cond:cond + 1], scalar2=shT[:, c, cond:cond + 1],
                                                 op0=ALU.mult, op1=ALU.add)) if c % 2 == 0 else
                 (lambda e, c=c: e.activation(out=hT[:, c, col0:col0 + 128], in_=pt[:, c * 128:(c + 1) * 128],
                                              func=AF.Identity, scale=gsT[:, c, cond:cond + 1],
                                              bias=shT[:, c, cond:cond + 1])),
                 [pt, gsT, shT], [hT])


def mk_eps(p, val=EPS):
    t = p.sb("epsc", [128, 1], F32)
    p.op("pool", lambda e: e.memset(t[:], val), [], [t])
    return t


T2 = 2176
BLK2 = [(0, 512), (512, 512), (1024, 512), (1536, 512), (2048, 128)]
NA_SCALE = 64 ** -0.5
MLA_SCALE = 96 ** -0.5


def build_l2(stop=None):
    p = Prog()
    x = p.dram("x", [T2, D], F32, "ExternalInput")
    w1 = p.dram("w1", [D, 2368], F32, "ExternalInput")
    wq = p.dram("wq", [384, 1536], F32, "ExternalInput")
    wkv = p.dram("wkv", [256, 1024], F32, "ExternalInput")
    cos_d = p.dram("cos96", [96, T2], F32, "ExternalInput")
    sin_d = p.dram("sin96", [96, T2], F32, "ExternalInput")
    gsT_d = p.dram("gsT", [128, 8, 2], F32, "ExternalInput")
    shT_d = p.dram("shT", [128, 8, 2], F32, "ExternalInput")
    gq_d = p.dram("gq", [128, 3], F32, "ExternalInput")
    gkv_d = p.dram("gkv", [128, 2], F32, "ExternalInput")
    ident_d = p.dram("ident", [128, 128], BF16, "ExternalInput")
    QT = p.dram("QT", [96, 8, T2], BF16, "ExternalOutput")
    KT = p.dram("KT", [96, 8, T2], BF16, "ExternalOutput")
    V = p.dram("V", [T2, 512], BF16, "ExternalOutput")
    NQT = p.dram("NQT", [64, 8, T2], BF16, "ExternalOutput")
    NKT = p.dram("NKT", [64, 8, T2], BF16, "ExternalOutput")
    NV = p.dram("NV", [T2, 512], BF16, "ExternalOutput")

    ident = p.sb("ident", [128, 128], BF16)
    p.dma("sp", ident[:], ident_d[:], [ident_d], [ident])
    ones = p.sb("ones", [128, 128], BF16)
    p.op("pool", lambda e: e.memset(ones[:], 1.0), [], [ones])
    cos = p.sb("cos", [96, T2], F32)
    sin = p.sb("sin", [96, T2], F32)
    p.dma("sp", cos[:], cos_d[:], [cos_d], [cos])
    p.dma("sp", sin[:], sin_d[:], [sin_d], [sin])
    gsT = p.sb("gsT", [128, 8, 2], F32)
    shT = p.sb("shT", [128, 8, 2], F32)
    gq = p.sb("gq", [128, 3], F32)
    gkv = p.sb("gkv", [128, 2], F32)
    for a, b_ in ((gsT, gsT_d), (shT, shT_d), (gq, gq_d), (gkv, gkv_d)):
        p.dma("sp", a[:], b_[:], [b_], [a])
    stg = Stage(p, 2368)
    W1 = load_w(p, stg, "W1", w1, 8, 2368)
    WQ = load_w(p, stg, "WQ", wq, 3, 1536, rowscale=gq)
    WKV = load_w(p, stg, "WKV", wkv, 2, 1024, rowscale=gkv)
    O_CQ, O_CKV, O_KR, O_KRR, O_NQ, O_NK, O_NV = 0, 384, 640, 736, 832, 1344, 1856
    nt = NormT(p, ident)
    eps = nt.eps
    hT = p.sb("hT", [128, 8, 512], BF16)
    cqg = p.sb("cqg", [128, 3, 512], BF16)
    sqq = p.sb("sqq", [128, 3, 512], BF16)
    ckvg = p.sb("ckvg", [128, 2, 512], BF16)
    sqkv = p.sb("sqkv", [128, 2, 512], BF16)
    rq = p.sb("rq", [128, 512], F32)
    rkv = p.sb("rkv", [128, 512], F32)
    rtok = p.sb("rtok", [128, 8], F32)
    tmpa = [p.sb("tmpa%d" % i, [128, 512], F32) for i in range(2)]
    tmpb = [p.sb("tmpb%d" % i, [128, 512], F32) for i in range(2)]
    krt = p.sb("krt", [96, 512], BF16)
    Qb = p.sb("Qb", [96, 8, 512], BF16)
    Kb = p.sb("Kb", [96, 8, 512], BF16)
    NQb = p.sb("NQb", [64, 8, 512], BF16)
    NKb = p.sb("NKb", [64, 8, 512], BF16)
    Vb = p.sb("Vb", [128, 4, 512], BF16)
    NVb = p.sb("NVb", [128, 4, 512], BF16)
    pp = [p.ps("pp%d" % i, [128, 512], F32) for i in range(6)]
    ppi = [0]

    def bank():
        ppi[0] += 1
        return pp[ppi[0] % 6]

    def mm(ps_ap, ps_t, pairs, rd):
        n = len(pairs)
        for i, (l, r) in enumerate(pairs):
            p.op("pe", lambda e, l=l, r=r, i=i: e.matmul(ps_ap, lhsT=l, rhs=r, start=(i == 0), stop=(i == n - 1)),
                 rd, [ps_t])

    for (c0, n) in BLK2:
        ntile = n // 128
        for ti in range(ntile):
            t0 = c0 + ti * 128
            cond = 0 if t0 < 2048 else 1
            nt.run(x[t0:t0 + 128, :], x.b, gsT, shT, cond, hT, ti * 128)
        if stop == 'norm':
            break
        for (dst, sq, g, off, nch, rbc, dim) in ((cqg, sqq, gq, O_CQ, 3, rq, 384), (ckvg, sqkv, gkv, O_CKV, 2, rkv, 256)):
            for c3 in range(nch):
                ps = bank()
                mm(ps[:, 0:n], ps, [(W1[:, c, off + c3 * 128: off + (c3 + 1) * 128], hT[:, c, 0:n]) for c in range(8)], [W1, hT])
                if stop == 'cq_mm':
                    continue
                p.op("dve", lambda e, ps=ps, c3=c3, dst=dst: e.tensor_copy(out=dst[:, c3, 0:n], in_=ps[:, 0:n]), [ps], [dst])
                p.op("act", lambda e, c3=c3, sq=sq, dst=dst: e.activation(out=sq[:, c3, 0:n], in_=dst[:, c3, 0:n], func=AF.Square), [dst], [sq])
            if stop in ('cq_mm', 'cq_dve', 'cq_act', 'cq_act2'):
                continue
            ps = bank()
            mm(ps[:, 0:n], ps, [(ones[:], sq[:, c3, 0:n]) for c3 in range(nch)], [ones, sq])
            if stop == 'cq_ones':
                continue
            p.op("act", lambda e, ps=ps, rbc=rbc, dim=dim: e.activation(out=rbc[:, 0:n], in_=ps[:, 0:n], func=AF.Sqrt,
                                                                      scale=1.0 / dim, bias=eps[:, 0:1]), [ps, eps], [rbc])
            p.op("dve", lambda e, rbc=rbc: e.reciprocal(out=rbc[:, 0:n], in_=rbc[:, 0:n]), [rbc], [rbc])
        if stop in ('cq', 'cq_mm', 'cq_dve', 'cq_act', 'cq_ones', 'cq_act2'):
            break
        ps = bank()
        for ti in range(ntile):
            mm(ps[:, ti:ti + 1], ps, [(sqkv[:, c2, ti * 128:(ti + 1) * 128], ones[:, 0:1]) for c2 in range(2)], [sqkv, ones])
        p.op("act", lambda e, ps=ps: e.activation(out=rtok[:, 0:ntile], in_=ps[:, 0:ntile], func=AF.Sqrt,
                                                  scale=1.0 / 256, bias=eps[:, 0:1]), [ps, eps], [rtok])
        p.op("dve", lambda e: e.reciprocal(out=rtok[:, 0:ntile], in_=rtok[:, 0:ntile]), [rtok], [rtok])
        if stop == 'rtok':
            break
        def rope(pa, pb, out_ap, out_t, scale_ap=None, scale_t=None, k=0):
            ta, tb = tmpa[k % 2], tmpb[k % 2]
            p.op("dve", lambda e: e.tensor_tensor(out=ta[0:96, 0:n], in0=pa[0:96, 0:n], in1=cos[:, c0:c0 + n], op=ALU.mult), [pa, cos], [ta])
            p.op("dve", lambda e: e.tensor_tensor(out=tb[0:96, 0:n], in0=pb[0:96, 0:n], in1=sin[:, c0:c0 + n], op=ALU.mult), [pb, sin], [tb])
            if scale_ap is None:
                p.op("pool", lambda e: e.tensor_tensor(out=out_ap, in0=ta[0:96, 0:n], in1=tb[0:96, 0:n], op=ALU.add), [ta, tb], [out_t])
            else:
                p.op("pool", lambda e: e.tensor_tensor(out=ta[0:96, 0:n], in0=ta[0:96, 0:n], in1=tb[0:96, 0:n], op=ALU.add), [ta, tb], [ta])
                p.op("pool", lambda e: e.tensor_tensor(out=out_ap, in0=ta[0:96, 0:n], in1=scale_ap, op=ALU.mult), [ta, scale_t], [out_t])
        for h in range(8):
            pa, pb = bank(), bank()
            mm(pa[0:96, 0:n], pa, [(WQ[:, c3, h * 96:(h + 1) * 96], cqg[:, c3, 0:n]) for c3 in range(3)], [WQ, cqg])
            mm(pb[0:96, 0:n], pb, [(WQ[:, c3, 768 + h * 96: 768 + (h + 1) * 96], cqg[:, c3, 0:n]) for c3 in range(3)], [WQ, cqg])
            rope(pa, pb, Qb[:, h, 0:n], Qb, rq[0:96, 0:n], rq, k=h)
        if stop == 'q':
            break
        pa, pb = bank(), bank()
        mm(pa[0:96, 0:n], pa, [(W1[:, c, O_KR:O_KR + 96], hT[:, c, 0:n]) for c in range(8)], [W1, hT])
        mm(pb[0:96, 0:n], pb, [(W1[:, c, O_KRR:O_KRR + 96], hT[:, c, 0:n]) for c in range(8)], [W1, hT])
        rope(pa, pb, krt[:, 0:n], krt)
        if stop == 'kr':
            break
        for h in range(8):
            ps = bank()
            mm(ps[0:64, 0:n], ps, [(WKV[:, c2, h * 64:(h + 1) * 64], ckvg[:, c2, 0:n]) for c2 in range(2)], [WKV, ckvg])
            p.op("dve", lambda e, ps=ps, h=h: e.tensor_tensor(out=Kb[0:64, h, 0:n], in0=ps[0:64, 0:n], in1=rkv[0:64, 0:n], op=ALU.mult), [ps, rkv], [Kb])
            p.op("pool", lambda e, h=h: e.tensor_copy(out=Kb[64:96, h, 0:n], in_=krt[64:96, 0:n]), [krt], [Kb])
        for ti in range(ntile):
            ps = bank()
            mm(ps[:, :], ps, [(ckvg[:, c2, ti * 128:(ti + 1) * 128], WKV[:, c2, 512:1024]) for c2 in range(2)], [WKV, ckvg])
            p.op("dve", lambda e, ps=ps, ti=ti: e.tensor_scalar(out=Vb[:, ti, :], in0=ps[:, :], scalar1=rtok[:, ti:ti + 1], scalar2=None, op0=ALU.mult), [ps, rtok], [Vb])
        if stop == 'kv':
            break
        for h in range(8):
            ps = bank()
            mm(ps[0:64, 0:n], ps, [(W1[:, c, O_NQ + h * 64:O_NQ + (h + 1) * 64], hT[:, c, 0:n]) for c in range(8)], [W1, hT])
            p.op("act", lambda e, ps=ps, h=h: e.mul(out=NQb[:, h, 0:n], in_=ps[0:64, 0:n], mul=NA_SCALE), [ps], [NQb])
            ps = bank()
            mm(ps[0:64, 0:n], ps, [(W1[:, c, O_NK + h * 64:O_NK + (h + 1) * 64], hT[:, c, 0:n]) for c in range(8)], [W1, hT])
            p.op("dve", lambda e, ps=ps, h=h: e.tensor_copy(out=NKb[:, h, 0:n], in_=ps[0:64, 0:n]), [ps], [NKb])
        for ti in range(ntile):
            ps = bank()
            mm(ps[:, :], ps, [(hT[:, c, ti * 128:(ti + 1) * 128], W1[:, c, O_NV:O_NV + 512]) for c in range(8)], [W1, hT])
            p.op("act", lambda e, ps=ps, ti=ti: e.copy(out=NVb[:, ti, :], in_=ps[:, :]), [ps], [NVb])
        if stop == 'na':
            break
        p.dma("pool", QT[:, :, c0:c0 + n], Qb[:, :, 0:n], [Qb], [QT])
        p.dma("pool", KT[:, :, c0:c0 + n], Kb[:, :, 0:n], [Kb], [KT])
        p.dma("pool", NQT[:, :, c0:c0 + n], NQb[:, :, 0:n], [NQb], [NQT])
        p.dma("pool", NKT[:, :, c0:c0 + n], NKb[:, :, 0:n], [NKb], [NKT])
        p.dma("pool", V[c0:c0 + n, :].rearrange("(t p) f -> p t f", p=128), Vb[:, 0:ntile, :], [Vb], [V])
        p.dma("pool", NV[c0:c0 + n, :].rearrange("(t p) f -> p t f", p=128), NVb[:, 0:ntile, :], [NVb], [NV])
    return p.finish()


def rope_tables(pos):
    T = len(pos)
    cos = np.ones((96, T), np.float64)
    sin = np.zeros((96, T), np.float64)
    invf = 10000.0 ** (-np.arange(8) / 8.0)
    valid = pos >= 0
    row = (pos // 64).astype(np.float64)
    col = (pos % 64).astype(np.float64)
    for j in range(32):
        pp_ = row if j < 16 else col
        ang = (pp_.astype(np.float32) * invf[j % 8].astype(np.float32)).astype(np.float64)
        cj = np.where(valid, np.cos(ang), 1.0)
        sj = np.where(valid, np.sin(ang), 0.0)
        cos[64 + j] = cj
        sin[64 + j] = -sj if (j % 16) < 8 else sj
    return cos.astype(np.float32), sin.astype(np.float32)


ROPE_PERM = np.array([j + 8 if (j % 16) < 8 else j - 8 for j in range(32)])


def fm(vec, nch):
    return np.ascontiguousarray(f32(vec).reshape(nch, 128).T)


def l2_inputs(xtok, pos, gs_lat, sh_lat, gs_ctx, sh_ctx, ev_w_in, q_norm_g, w_qb, kv_norm_g, w_kvb):
    w_in = f32(ev_w_in)
    kr = w_in[:, 640:672]
    z64 = np.zeros((D, 64), np.float32)
    w1 = np.concatenate([w_in[:, 0:640], z64, kr, z64, kr[:, ROPE_PERM], w_in[:, 672:2208]], axis=1)
    wqb = f32(w_qb).reshape(384, 8, 96)
    wq_rot = np.concatenate([np.zeros((384, 8, 64), np.float32), wqb[:, :, 64:][:, :, ROPE_PERM]], axis=2)
    wq = np.concatenate([wqb.reshape(384, 768), wq_rot.reshape(384, 768)], axis=1)
    wkvb = f32(w_kvb).reshape(256, 8, 128)
    wkv = np.concatenate([wkvb[:, :, :64].reshape(256, 512), wkvb[:, :, 64:].reshape(256, 512)], axis=1)
    cos96, sin96 = rope_tables(pos)
    ident = np.eye(128, dtype=np.float32).astype(NPBF)
    return {
        "x": f32(xtok), "w1": np.ascontiguousarray(w1), "wq": np.ascontiguousarray(wq), "wkv": np.ascontiguousarray(wkv),
        "cos96": cos96, "sin96": sin96,
        "gsT": np.ascontiguousarray(np.stack([fm(gs_lat, 8), fm(gs_ctx, 8)], axis=2)),
        "shT": np.ascontiguousarray(np.stack([fm(sh_lat, 8), fm(sh_ctx, 8)], axis=2)),
        "gq": fm(q_norm_g, 3), "gkv": fm(kv_norm_g, 2), "ident": ident,
    }


NKEY = 4352
NAK = 40 * 64 + 256


def build_l3():
    p = Prog()
    QT = p.dram("QT", [96, 8, T2], BF16, "ExternalInput")
    KT = p.dram("KT", [96, 8, NKEY], BF16, "ExternalInput")
    VA = p.dram("VA", [NKEY, 8, 65], BF16, "ExternalInput")
    NQT = p.dram("NQT", [64, 8, T2], BF16, "ExternalInput")
    NKT = p.dram("NKT", [64, 8, NAK], BF16, "ExternalInput")
    NVA = p.dram("NVA", [NAK, 8, 65], BF16, "ExternalInput")
    NB = p.dram("NB", [8, 128, 18, 256], F32, "ExternalInput")
    AO = p.dram("AO", [T2, D], BF16, "ExternalOutput")
    attn = p.sb("attn", [128, 17, D], BF16)
    S = [p.ps("S%d" % i, [128, 512], F32) for i in range(2)]
    O = [p.ps("O%d" % i, [128, 512], F32) for i in range(4)]
    PT = [p.sb("PT%d" % i, [128, 512], BF16) for i in range(3)]
    rden = [p.sb("rden%d" % i, [128, 1], F32) for i in range(4)]
    tmp = [p.sb("tmpf%d" % i, [128, 256], F32) for i in range(2)]
    cnt = {"s": 0, "pt": 0, "tm": 0}

    def attend(kt, ktoff, q, q0, nq, keytiles, v, scale, bias=None, tile0=0, col0=0):
        nqs = nq // 128
        nk = len(keytiles)

        def score(i):
            kb, bj = keytiles[i]
            ps = S[cnt["s"] % 2]
            cnt["s"] += 1
            pt = PT[cnt["pt"] % 3]
            cnt["pt"] += 1
            p.op("pe", lambda e: e.matmul(ps[:, 0:nq], lhsT=kt[:, kb * 128:(kb + 1) * 128], rhs=q[:, q0:q0 + nq],
                                          start=True, stop=True), [kt, q], [ps])
            if bj is None:
                p.op("act", lambda e: e.activation(out=pt[:, 0:nq], in_=ps[:, 0:nq], func=AF.Exp, scale=scale), [ps], [pt])
            else:
                tm = tmp[cnt["tm"] % 2]
                cnt["tm"] += 1
                p.op("dve", lambda e: e.tensor_tensor(out=tm[:, 0:nq], in0=ps[:, 0:nq], in1=bias[:, bj, 0:nq], op=ALU.add), [ps, bias], [tm])
                p.op("act", lambda e: e.activation(out=pt[:, 0:nq], in_=tm[:, 0:nq], func=AF.Exp, scale=scale), [tm], [pt])
            return pt

        pts = [score(0)]
        for i, (kb, bj) in enumerate(keytiles):
            if i + 1 < nk:
                pts.append(score(i + 1))
            pt = pts[i]
            for qs in range(nqs):
                p.op("pe", lambda e, qs=qs: e.matmul(O[qs][:, 0:65], lhsT=pt[:, qs * 128:(qs + 1) * 128], rhs=v[:, kb, :],
                                                     start=(i == 0), stop=(i == nk - 1)), [pt, v], [O[qs]])
        for qs in range(nqs):
            p.op("dve", lambda e, qs=qs: e.reciprocal(out=rden[qs][:], in_=O[qs][:, 64:65]), [O[qs]], [rden[qs]])
            p.op("dve", lambda e, qs=qs: e.tensor_scalar(out=attn[:, tile0 + qs, col0:col0 + 64], in0=O[qs][:, 0:64],
                                                         scalar1=rden[qs][:, 0:1], scalar2=None, op0=ALU.mult),
                 [O[qs], rden[qs]], [attn])

    kth = [p.sb("kth%d" % i, [96, NKEY], BF16) for i in range(2)]
    vh = [p.sb("vh%d" % i, [128, 34, 65], BF16) for i in range(2)]
    qh = [p.sb("qh%d" % i, [96, T2], BF16) for i in range(2)]
    for h in range(8):
        k_, v_, q_ = kth[h % 2], vh[h % 2], qh[h % 2]
        p.dma("sp", k_[:], KT[:, h, :], [KT], [k_])
        p.dma("sp", v_[:], VA[:, h, :].rearrange("(t p) f -> p t f", p=128), [VA], [v_])
        p.dma("sp", q_[:], QT[:, h, :], [QT], [q_])
        for qb in range(4):
            attend(k_, 0, q_, qb * 512, 512, [(kb, None) for kb in range(34)], v_, MLA_SCALE, tile0=qb * 4, col0=h * 64)
        attend(k_, 0, q_, 2048, 128, [(32, None), (33, None)], v_, MLA_SCALE, tile0=16, col0=h * 64)
    nkh = [p.sb("nkh%d" % i, [64, NAK], BF16) for i in range(2)]
    nvh = [p.sb("nvh%d" % i, [128, 22, 65], BF16) for i in range(2)]
    nqh = [p.sb("nqh%d" % i, [64, T2], BF16) for i in range(2)]
    nbh = [p.sb("nbh%d" % i, [128, 18, 256], F32) for i in range(2)]
    for h in range(8):
        k_, v_, q_, b_ = nkh[h % 2], nvh[h % 2], nqh[h % 2], nbh[h % 2]
        p.dma("sp", k_[:], NKT[:, h, :], [NKT], [k_])
        p.dma("sp", v_[:], NVA[:, h, :].rearrange("(t p) f -> p t f", p=128), [NVA], [v_])
        p.dma("sp", q_[:], NQT[:, h, :], [NQT], [q_])
        p.dma("sp", b_[:], NB[h], [NB], [b_])
        for qt in range(8):
            slot = 0 if qt == 0 else (2 if qt == 7 else 1)
            kts = [(2 * qt + j, slot * 6 + j) for j in range(6)] + [(20, None), (21, None)]
            attend(k_, 0, q_, qt * 256, 256, kts, v_, 1.0, bias=b_, tile0=qt * 2, col0=512 + h * 64)
        attend(k_, 0, q_, 2048, 128, [(20, None), (21, None)], v_, 1.0, tile0=16, col0=512 + h * 64)
    p.dma("sp", AO.t.rearrange("(t p) f -> p t f", p=128), attn[:], [attn], [AO])
    return p.finish()


def na_bias(rpb, hf, qt):
    rpb = f32(rpb)
    j = np.arange(6)[:, None, None, None, None]
    krl = np.arange(2)[None, :, None, None, None]
    kc = np.arange(64)[None, None, :, None, None]
    qrl = np.arange(4)[None, None, None, :, None]
    qc = np.arange(64)[None, None, None, None, :]
    kr = 32 * hf + 4 * qt - 4 + 2 * j + krl
    r = 32 * hf + 4 * qt + qrl
    rs = np.clip(r - 4, 0, 56)
    cs = np.clip(qc - 8, 0, 48)
    ok = (kr >= 0) & (kr < 64) & (kr >= rs) & (kr < rs + 8) & (kc >= cs) & (kc < cs + 16)
    ro = np.clip(kr - r + 7, 0, 14) + 0 * kc + 0 * qc
    co = np.clip(kc - qc + 15, 0, 30) + 0 * kr + 0 * r
    ok = np.broadcast_to(ok, ro.shape)
    out = np.where(ok[None], rpb[:, ro, co], np.float32(-30000.0))
    return out.reshape(8, 6, 128, 256).astype(np.float32)


def l3_inputs(b, hf, l2res, rpb):
    r0, r1 = l2res[2 * b], l2res[2 * b + 1]
    own = l2res[2 * b + hf]
    KT = np.concatenate([r0["KT"][:, :, :2048], r1["KT"][:, :, :2048], r0["KT"][:, :, 2048:], r1["KT"][:, :, 2048:]], axis=2)
    Vall = np.concatenate([r0["V"][:2048], r1["V"][:2048], r0["V"][2048:], r1["V"][2048:]], axis=0).reshape(NKEY, 8, 64)
    VA = np.concatenate([Vall, np.ones((NKEY, 8, 1), NPBF)], axis=2)
    nk_lat = np.concatenate([r0["NKT"][:, :, :2048], r1["NKT"][:, :, :2048]], axis=2)
    nk_ctx = np.concatenate([r0["NKT"][:, :, 2048:], r1["NKT"][:, :, 2048:]], axis=2)
    nv_lat = np.concatenate([r0["NV"][:2048], r1["NV"][:2048]], axis=0).reshape(4096, 8, 64)
    nv_ctx = np.concatenate([r0["NV"][2048:], r1["NV"][2048:]], axis=0).reshape(256, 8, 64)
    NK = np.zeros((64, 8, NAK), NPBF)
    NVv = np.zeros((NAK, 8, 64), NPBF)
    for i in range(40):
        gr = 32 * hf - 4 + i
        if 0 <= gr < 64:
            NK[:, :, i * 64:(i + 1) * 64] = nk_lat[:, :, gr * 64:(gr + 1) * 64]
            NVv[i * 64:(i + 1) * 64] = nv_lat[gr * 64:(gr + 1) * 64]
    NK[:, :, 2560:] = nk_ctx
    NVv[2560:] = nv_ctx
    NVA = np.concatenate([NVv, np.ones((NAK, 8, 1), NPBF)], axis=2)
    nb = np.stack([na_bias(rpb, hf, qt) for qt in (0, 3, 7)], axis=1)
    NB = np.ascontiguousarray(nb.reshape(8, 18, 128, 256).transpose(0, 2, 1, 3))
    return {"QT": own["QT"], "KT": np.ascontiguousarray(KT), "VA": np.ascontiguousarray(VA), "NQT": own["NQT"],
            "NKT": NK, "NVA": np.ascontiguousarray(NVA), "NB": NB}


class View:
    def __init__(self, ap, b):
        self.ap = ap
        self.b = b

    def __getitem__(self, idx):
        return self.ap


class Panels:
    def __init__(self, p, nslots=4):
        self.p = p
        self.st = [p.sb("pst%d" % i, [128, 8, 128], F32) for i in range(nslots)]
        self.bf = [p.sb("pbf%d" % i, [128, 8, 128], BF16) for i in range(nslots)]
        self.i = 0

    def get(self, w, col0):
        p = self.p
        k = self.i % len(self.st)
        self.i += 1
        st, bf = self.st[k], self.bf[k]
        if len(w.t.shape) == 4:
            p.dma("sp", st[:], w[col0 // 128], [w], [st])
        else:
            p.dma("sp", st[:], w[:, col0:col0 + 128].rearrange("(c p) n -> p c n", p=128), [w], [st])
        p.op("pool" if k % 2 == 0 else "dve", lambda e: e.tensor_copy(out=bf[:], in_=st[:]), [st], [bf])
        return bf


def ffn_phase1(p, pan, banks, hT, n, wg, wu, nf, actT, sg, f0=0, between=None):
    for f in range(f0, f0 + nf):
        g_, u_ = pan.get(wg, f * 128), pan.get(wu, f * 128)
        if between is not None:
            between(f - f0)
        for n0 in range(0, n, 512):
            nn = min(512, n - n0)
            pg, pu = banks(), banks()
            for (ps, w_) in ((pg, g_), (pu, u_)):
                for c in range(8):
                    p.op("pe", lambda e, ps=ps, w_=w_, c=c: e.matmul(ps[:, 0:nn], lhsT=w_[:, c, :], rhs=hT[:, c, n0:n0 + nn],
                                                                   start=(c == 0), stop=(c == 7)), [w_, hT], [ps])
            s = sg[(f + n0 // 512) % 2]
            p.op("act", lambda e, s=s, pg=pg: e.activation(out=s[:, 0:nn], in_=pg[:, 0:nn], func=AF.Silu), [pg], [s])
            p.op("dve", lambda e, s=s, pu=pu, f=f: e.tensor_tensor(out=actT[:, f - f0, n0:n0 + nn], in0=s[:, 0:nn], in1=pu[:, 0:nn],
                                                                 op=ALU.mult), [s, pu], [actT])


def build_l4():
    p = Prog()
    x = p.dram("x", [T2, D], F32, "ExternalInput")
    aT = p.dram("aT", [D, T2], BF16, "ExternalInput")
    wo = p.dram("wo", [D, D], F32, "ExternalInput")
    wg = p.dram("wg", [22, 128, 8, 128], F32, "ExternalInput")
    wu = p.dram("wu", [22, 128, 8, 128], F32, "ExternalInput")
    wd = p.dram("wd", [2816, D], F32, "ExternalInput")
    g1_d = p.dram("g1bc", [2, 128, D], F32, "ExternalInput")
    g2_d = p.dram("g2bc", [2, 128, D], F32, "ExternalInput")
    gsT_d = p.dram("gsT", [128, 8, 2], F32, "ExternalInput")
    shT_d = p.dram("shT", [128, 8, 2], F32, "ExternalInput")
    ident_d = p.dram("ident", [128, 128], BF16, "ExternalInput")
    xo = p.dram("xo", [T2, D], F32, "ExternalOutput")
    ident = p.sb("ident", [128, 128], BF16)
    p.dma("sp", ident[:], ident_d[:], [ident_d], [ident])
    gsT = p.sb("gsT", [128, 8, 2], F32)
    shT = p.sb("shT", [128, 8, 2], F32)
    p.dma("sp", gsT[:], gsT_d[:], [gsT_d], [gsT])
    p.dma("sp", shT[:], shT_d[:], [shT_d], [shT])
    stg = Stage(p, 1024)
    WO = load_w(p, stg, "WO", wo, 8, 1024)
    WD = load_w(p, stg, "WD", wd, 22, 1024)
    nt = NormT(p, ident, nslots=2)
    pan = Panels(p)
    hT = p.sb("hT", [128, 8, 640], BF16)
    at = p.sb("at", [128, 8, 640], BF16)
    actT = p.sb("actT", [128, 22, 640], BF16)
    xs = p.sb("xs", [128, 5, D], F32)
    g1 = p.sb("g1", [128, 2, D], F32)
    g2 = p.sb("g2", [128, 2, D], F32)
    for cnd in range(2):
        p.dma("sp", g1[:, cnd, :], g1_d[cnd], [g1_d], [g1])
        p.dma("sp", g2[:, cnd, :], g2_d[cnd], [g2_d], [g2])
    sg = [p.sb("sg%d" % i, [128, 512], F32) for i in range(2)]
    tm = [p.sb("tm%d" % i, [128, 512], F32) for i in range(2)]
    pp = [p.ps("pp%d" % i, [128, 512], F32) for i in range(6)]
    ppi = [0]

    def banks():
        ppi[0] += 1
        return pp[ppi[0] % 6]

    tmi = [0]

    def resid(ps, gt, ti, cb, cond):
        t = tm[tmi[0] % 2]
        tmi[0] += 1
        p.op("dve", lambda e: e.tensor_tensor(out=t[:], in0=ps[:], in1=gt[:, cond, cb * 512:(cb + 1) * 512], op=ALU.mult), [ps, gt], [t])
        p.op("pool", lambda e: e.tensor_tensor(out=xs[:, ti, cb * 512:(cb + 1) * 512], in0=xs[:, ti, cb * 512:(cb + 1) * 512],
                                               in1=t[:], op=ALU.add), [xs, t], [xs])

    for (c0, n) in [(0, 512), (512, 512), (1024, 512), (1536, 640)]:
        ntile = n // 128
        p.dma("sp", xs[:, 0:ntile, :], x[c0:c0 + n, :].rearrange("(t p) f -> p t f", p=128), [x], [xs])
        p.dma("sp", at[:, :, 0:n], aT[:, c0:c0 + n].rearrange("(c p) t -> p c t", p=128), [aT], [at])
        for ti in range(ntile):
            cond = 0 if (c0 + ti * 128) < 2048 else 1
            for cb in range(2):
                ps = banks()
                for c in range(8):
                    p.op("pe", lambda e, c=c, ps=ps: e.matmul(ps[:], lhsT=at[:, c, ti * 128:(ti + 1) * 128],
                                                             rhs=WO[:, c, cb * 512:(cb + 1) * 512], start=(c == 0), stop=(c == 7)),
                         [at, WO], [ps])
                resid(ps, g1, ti, cb, cond)
            nt.run(None, None, gsT, shT, cond, hT, ti * 128, x_loaded=View(xs[:, ti, :], xs.b))
        ffn_phase1(p, pan, banks, hT, n, wg, wu, 22, actT, sg)
        for ti in range(ntile):
            cond = 0 if (c0 + ti * 128) < 2048 else 1
            for cb in range(2):
                ps = banks()
                for f in range(22):
                    p.op("pe", lambda e, f=f, ps=ps: e.matmul(ps[:], lhsT=actT[:, f, ti * 128:(ti + 1) * 128],
                                                             rhs=WD[:, f, cb * 512:(cb + 1) * 512], start=(f == 0), stop=(f == 21)),
                         [actT, WD], [ps])
                resid(ps, g2, ti, cb, cond)
        p.dma("pool", xo[c0:c0 + n, :].rearrange("(t p) f -> p t f", p=128), xs[:, 0:ntile, :], [xs], [xo])
    return p.finish()


def pretile(w):
    w = f32(w)
    F = w.shape[1]
    return np.ascontiguousarray(w.reshape(8, 128, F // 128, 128).transpose(2, 1, 0, 3))


def bc128(v):
    return np.ascontiguousarray(np.broadcast_to(f32(v)[None, :], (128, len(v))))


def build_l5():
    p = Prog()
    x = p.dram("x", [T2, D], F32, "ExternalInput")
    wi = p.dram("wi", [D, D], F32, "ExternalInput")
    gsT_d = p.dram("gsT", [128, 8, 2], F32, "ExternalInput")
    shT_d = p.dram("shT", [128, 8, 2], F32, "ExternalInput")
    ident_d = p.dram("ident", [128, 128], BF16, "ExternalInput")
    uo = p.dram("u", [T2, D], F32, "ExternalOutput")
    ident = p.sb("ident", [128, 128], BF16)
    p.dma("sp", ident[:], ident_d[:], [ident_d], [ident])
    gsT = p.sb("gsT", [128, 8, 2], F32)
    shT = p.sb("shT", [128, 8, 2], F32)
    p.dma("sp", gsT[:], gsT_d[:], [gsT_d], [gsT])
    p.dma("sp", shT[:], shT_d[:], [shT_d], [shT])
    stg = Stage(p, 1024)
    WI = load_w(p, stg, "WI", wi, 8, 1024)
    nt = NormT(p, ident)
    hT = [p.sb("hT%d" % i, [128, 8, 128], BF16) for i in range(2)]
    us = [p.sb("us%d" % i, [128, D], F32) for i in range(2)]
    pp = [p.ps("pp%d" % i, [128, 512], F32) for i in range(4)]
    k = 0
    for ti in range(T2 // 128):
        cond = 0 if ti < 16 else 1
        h_, u_ = hT[ti % 2], us[ti % 2]
        nt.run(x[ti * 128:(ti + 1) * 128, :], x.b, gsT, shT, cond, h_, 0)
        for cb in range(2):
            ps = pp[k % 4]
            k += 1
            for c in range(8):
                p.op("pe", lambda e, c=c, ps=ps: e.matmul(ps[:], lhsT=h_[:, c, :], rhs=WI[:, c, cb * 512:(cb + 1) * 512],
                                                         start=(c == 0), stop=(c == 7)), [h_, WI], [ps])
            p.op("act" if cb else "dve", (lambda e, ps=ps: e.copy(out=u_[:, cb * 512:(cb + 1) * 512], in_=ps[:])) if cb else
                 (lambda e, ps=ps: e.tensor_copy(out=u_[:, cb * 512:(cb + 1) * 512], in_=ps[:])), [ps], [u_])
        p.dma("pool", uo[ti * 128:(ti + 1) * 128, :], u_[:], [u_], [uo])
    return p.finish()


NCH = 544
TWO_PI = 2.0 * np.pi


def build_l6():
    p = Prog()
    U = p.dram("U", [64, 128, NCH], F32, "ExternalInput")
    prm = p.dram("prm", [3, 128, 64], F32, "ExternalInput")
    bri = p.dram("bri", [2, 128, 64, 16], F32, "ExternalInput")
    cri = p.dram("cri", [2, 128, 64, 16], F32, "ExternalInput")
    sel = p.dram("sel", [128, 2], F32, "ExternalInput")
    dq_d = p.dram("dq", [128, 64], F32, "ExternalInput")
    mk_d = p.dram("mk", [128, 64], F32, "ExternalInput")
    identf_d = p.dram("identf", [128, 128], F32, "ExternalInput")
    ident_d = p.dram("ident", [128, 128], BF16, "ExternalInput")
    Y = p.dram("Y", [64, 128, 512], F32, "ExternalOutput")

    def ld(name, shape, src, dt=F32):
        t = p.sb(name, shape, dt)
        p.dma("sp", t[:], src, [src] if isinstance(src, Tile) else [], [t])
        return t

    AR = ld("AR", [128, 64], prm[0]); AI = ld("AI", [128, 64], prm[1]); LS = ld("LS", [128, 64], prm[2])
    BR = ld("BR", [128, 64, 16], bri[0]); BI = ld("BI", [128, 64, 16], bri[1])
    CR = ld("CR", [128, 64, 16], cri[0]); CI = ld("CI", [128, 64, 16], cri[1])
    SEL = ld("SEL", [128, 2], sel[:]); DQ = ld("DQ", [128, 64], dq_d[:]); MK = ld("MK", [128, 64], mk_d[:])
    IDF = ld("IDF", [128, 128], identf_d[:]); IDB = ld("IDB", [128, 128], ident_d[:], BF16)
    sa, sb_ = SEL[:, 0:1], SEL[:, 1:2]
    n_ = [0]

    def T(shape=(128, 64), dt=F32):
        n_[0] += 1
        return p.sb("g%d" % n_[0], list(shape), dt)

    def tt(out, a, b, op, eng="dve"):
        p.op(eng, lambda e: e.tensor_tensor(out=out[:], in0=a[:], in1=b[:], op=op), [a, b], [out])
        return out

    def ts(out, a, s1, op0, s2=None, op1=None, rd=()):
        if op1 is None:
            p.op("dve", lambda e: e.tensor_scalar(out=out[:], in0=a[:], scalar1=s1, scalar2=None, op0=op0), [a] + list(rd), [out])
        else:
            p.op("dve", lambda e: e.tensor_scalar(out=out[:], in0=a[:], scalar1=s1, scalar2=s2, op0=op0, op1=op1), [a] + list(rd), [out])
        return out

    def stt(out, a, s, b, op0, op1, rd=()):
        p.op("dve", lambda e: e.scalar_tensor_tensor(out=out[:], in0=a[:], scalar=s, in1=b[:], op0=op0, op1=op1), [a, b] + list(rd), [out])
        return out

    def act(out, a, func, scale=1.0):
        p.op("act", lambda e: e.activation(out=out[:], in_=a[:], func=func, scale=scale), [a], [out])
        return out

    dt_ = act(T(), LS, AF.Exp)
    xd = tt(T(), AR, dt_, ALU.mult)
    th = tt(T(), AI, dt_, ALU.mult)
    ki = p.sb("ki", [128, 64], I32)
    kf, m1 = T(), T()

    def reduce_(r):
        ts(kf, r, 1.0 / TWO_PI, ALU.mult)
        p.op("dve", lambda e: e.tensor_copy(out=ki[:], in_=kf[:]), [kf], [ki])
        p.op("dve", lambda e: e.tensor_copy(out=kf[:], in_=ki[:]), [ki], [kf])
        stt(r, kf, -TWO_PI, r, ALU.mult, ALU.add)
        wrap(r)

    def wrap(r):
        ts(m1, r, float(np.pi), ALU.is_gt)
        stt(r, m1, -TWO_PI, r, ALU.mult, ALU.add)
        ts(m1, r, -float(np.pi), ALU.is_lt)
        stt(r, m1, TWO_PI, r, ALU.mult, ALU.add)

    lr, li = [None] * 9, [None] * 9
    for k in range(9):
        lr[k], li[k] = T(), T()
        if k == 0:
            p.op("pool", lambda e: e.memset(lr[0][:], 1.0), [], [lr[0]])
            p.op("pool", lambda e: e.memset(li[0][:], 0.0), [], [li[0]])
            continue
        ek = act(T(), xd, AF.Exp, scale=float(k))
        ph = ts(T(), th, float(k), ALU.mult)
        reduce_(ph)
        sk = act(T(), ph, AF.Sin)
        ts(ph, ph, float(np.pi / 2), ALU.add)
        wrap(ph)
        ck = act(T(), ph, AF.Sin)
        tt(lr[k], ek, ck, ALU.mult)
        tt(li[k], ek, sk, ALU.mult)
        if k == 8:
            ek8, ck8, sk8 = ek, ck, sk
    den = tt(T(), AR, AR, ALU.mult)
    t0 = tt(T(), AI, AI, ALU.mult)
    tt(den, den, t0, ALU.add)
    p.op("dve", lambda e: e.reciprocal(out=den[:], in_=den[:]), [den], [den])
    lm1 = ts(T(), lr[1], -1.0, ALU.add)
    fr = tt(T(), lm1, AR, ALU.mult); tt(t0, li[1], AI, ALU.mult); tt(fr, fr, t0, ALU.add); tt(fr, fr, den, ALU.mult)
    fi = tt(T(), li[1], AR, ALU.mult); tt(t0, lm1, AI, ALU.mult); tt(fi, fi, t0, ALU.subtract); tt(fi, fi, den, ALU.mult)
    al, be = [None] * 8, [None] * 8
    wr, wi_, t1 = T(), T(), T()
    for k in range(8):
        tt(wr, lr[k], fr, ALU.mult); tt(t0, li[k], fi, ALU.mult); tt(wr, wr, t0, ALU.subtract)
        tt(wi_, lr[k], fi, ALU.mult); tt(t0, li[k], fr, ALU.mult); tt(wi_, wi_, t0, ALU.add)
        al[k], be[k] = T(), T()
        ts(t1, wi_, sb_, ALU.mult, rd=[SEL]); stt(al[k], wr, sa, t1, ALU.mult, ALU.add, rd=[SEL])
        ts(t1, wi_, sa, ALU.mult, rd=[SEL]); stt(be[k], wr, sb_, t1, ALU.mult, ALU.subtract, rd=[SEL])
    nlr, nli = [None] * 9, [None] * 9
    for k in range(1, 9):
        nlr[k] = ts(T(), lr[k], -1.0, ALU.mult)
        nli[k] = ts(T(), li[k], -1.0, ALU.mult)
    cst = p.sb("cst", [128, 64, 16], BF16)
    ctmp = p.sb("ctmp", [128, 64, 16], F32)
    ts(ctmp, CI, sb_, ALU.mult, rd=[SEL])
    stt(cst, CR, sa, ctmp, ALU.mult, ALU.subtract, rd=[SEL])
    Mr, Mi_ = [None] * 10, [None] * 10
    Mr[0] = ck8
    Mi_[0] = ts(T(), sk8, -1.0, ALU.mult)
    for k in range(1, 10):
        Mr[k], Mi_[k] = T(), T()
        tt(t0, Mi_[k - 1], Mi_[k - 1], ALU.mult)
        tt(Mr[k], Mr[k - 1], Mr[k - 1], ALU.mult)
        tt(Mr[k], Mr[k], t0, ALU.subtract)
        tt(Mi_[k], Mr[k - 1], Mi_[k - 1], ALU.mult)
        ts(Mi_[k], Mi_[k], 2.0, ALU.mult)

    NG = 8
    NP = NG // 2
    def pairpack(X):
        Xp = T((128, 32))
        Xv = X[:].rearrange("p (g two) -> p g two", two=2)
        p.op("dve", lambda e: e.tensor_copy(out=Xp[0:64, :], in_=Xv[0:64, :, 0]), [X], [Xp])
        p.op("dve", lambda e: e.tensor_copy(out=Xp[64:128, :], in_=Xv[64:128, :, 1]), [X], [Xp])
        return Xp

    ek8p = pairpack(ek8)
    Mrp = [pairpack(Mr[k]) for k in range(10)]
    Mip = [pairpack(Mi_[k]) for k in range(10)]
    zpp = p.sb("zpp", [128, NG, 15, 16], BF16)
    p.op("pool", lambda e: e.memset(zpp[:], 0.0), [], [zpp])
    tz = [p.sb("tz%d" % i, [128, NG, 16], F32) for i in range(4)]
    Md = p.sb("Md", [128, NG, 256], BF16)
    p.op("pool", lambda e: e.memset(Md[:], 0.0), [], [Md])
    Mi = p.sb("Mi", [128, NG, 128], BF16)
    MoR = p.sb("MoR", [128, NG, 128], BF16)
    MoI = p.sb("MoI", [128, NG, 128], BF16)
    G = p.sb("G", [128, 2, NP, NCH], F32)
    Hb = p.sb("Hb", [128, 2, NP, 512], BF16)
    Er = p.sb("Er", [128, NP, NCH], F32)
    Ei = p.sb("Ei", [128, NP, NCH], F32)
    tE = [p.sb("tE%d" % i, [128, NP, 256], F32) for i in range(2)]
    gm = [p.sb("gm%d" % i, [128, 2, NCH], F32) for i in range(2)]
    gsn = [p.sb("gsn%d" % i, [128, 2, NCH], F32) for i in range(2)]
    ta = [p.sb("ta%d" % i, [128, NCH], F32) for i in range(4)]
    uf = [p.sb("uf%d" % i, [128, NCH], F32) for i in range(2)]
    ub = [p.sb("ub%d" % i, [128, NCH], BF16) for i in range(NG)]
    yo = [p.sb("yo%d" % i, [128, 512], F32) for i in range(2)]
    ptp = p.ps("ptp", [128, 128], BF16)
    psI = p.ps("psI", [128, 128], F32)
    pg = [p.ps("pg%d" % i, [128, 512], F32) for i in range(4)]
    pgi = [0]

    for ps_ in range(64 // NG):
        g0 = ps_ * NG
        pp0 = g0 // 2
        gs_ = slice(g0, g0 + NG)

        def bc(t_):
            return t_[:, gs_].unsqueeze(2).broadcast_to([128, NG, 16])

        for m in range(8):
            k = 7 - m
            p.op("dve", lambda e: e.tensor_tensor(out=tz[0][:], in0=BR[:, gs_, :], in1=bc(al[k]), op=ALU.mult), [BR, al[k]], [tz[0]])
            p.op("pool", lambda e: e.tensor_tensor(out=tz[1][:], in0=BI[:, gs_, :], in1=bc(be[k]), op=ALU.mult), [BI, be[k]], [tz[1]])
            p.op("dve", lambda e: e.tensor_tensor(out=zpp[:, :, m, :], in0=tz[0][:], in1=tz[1][:], op=ALU.add), [tz[0], tz[1]], [zpp])
        for t in range(8):
            k = t + 1
            mo_r = MoR[:, :, t * 16:(t + 1) * 16]
            mo_i = MoI[:, :, t * 16:(t + 1) * 16]
            p.op("dve", lambda e: e.tensor_tensor(out=tz[0][:], in0=CR[:, gs_, :], in1=bc(lr[k]), op=ALU.mult), [CR, lr[k]], [tz[0]])
            p.op("pool", lambda e: e.tensor_tensor(out=tz[1][:], in0=CI[:, gs_, :], in1=bc(nli[k]), op=ALU.mult), [CI, nli[k]], [tz[1]])
            p.op("dve", lambda e: e.tensor_tensor(out=tz[0][:], in0=tz[0][:], in1=tz[1][:], op=ALU.add), [tz[0], tz[1]], [tz[0]])
            p.op("dve", lambda e: e.tensor_tensor(out=mo_r, in0=tz[0][:], in1=bc(MK), op=ALU.mult), [tz[0], MK], [MoR])
            p.op("pool", lambda e: e.tensor_tensor(out=tz[2][:], in0=CR[:, gs_, :], in1=bc(nli[k]), op=ALU.mult), [CR, nli[k]], [tz[2]])
            p.op("dve", lambda e: e.tensor_tensor(out=tz[3][:], in0=CI[:, gs_, :], in1=bc(nlr[k]), op=ALU.mult), [CI, nlr[k]], [tz[3]])
            p.op("pool", lambda e: e.tensor_tensor(out=tz[2][:], in0=tz[2][:], in1=tz[3][:], op=ALU.add), [tz[2], tz[3]], [tz[2]])
            p.op("pool", lambda e: e.tensor_tensor(out=mo_i, in0=tz[2][:], in1=bc(MK), op=ALU.mult), [tz[2], MK], [MoI])
        for gl in range(NG):
            g = g0 + gl
            gp, h = gl // 2, gl % 2
            z = zpp
            zf = zpp[:, gl].rearrange("p m j -> p (m j)")
            p.op("pe", lambda e: e.transpose(out=ptp[:], in_=zf[:, 0:128], identity=IDB[:]), [z, IDB], [ptp])
            p.op("act", lambda e: e.copy(out=Md[:, gl, 64:128], in_=ptp[:, 0:64]), [ptp], [Md])
            p.op("act", lambda e: e.copy(out=Md[:, gl, 192:256], in_=ptp[:, 64:128]), [ptp], [Md])
            for t in range(8):
                p.op("pe", lambda e, t=t: e.matmul(psI[:, t * 16:(t + 1) * 16], lhsT=zf[:, (7 - t) * 16:(15 - t) * 16], rhs=cst[:, g, :],
                                                  start=True, stop=True), [z, cst], [psI])
            p.op("dve", lambda e: e.scalar_tensor_tensor(out=Mi[:, gl, :], in0=IDF[:], scalar=DQ[:, g:g + 1], in1=psI[:],
                                                         op0=ALU.mult, op1=ALU.add), [IDF, DQ, psI], [Mi])
            u_f, u_b = uf[gl % 2], ub[gl]
            p.dma("sp", u_f[:], U[g], [U], [u_f])
            p.op("act", lambda e: e.copy(out=u_b[:], in_=u_f[:]), [u_f], [u_b])
            for c in range(2):
                for (n0, nn) in ((0, 512), (512, 32)):
                    ps = pg[pgi[0] % 4]
                    pgi[0] += 1
                    if h == 0:
                        p.op("pe", lambda e: e.matmul(ps[0:64, 0:nn], lhsT=Md[:, gl, 64 + 128 * c:128 + 128 * c], rhs=u_b[:, n0:n0 + nn],
                                                      start=True, stop=True), [Md, u_b], [ps])
                        p.op("act", lambda e: e.copy(out=G[0:64, c, gp, n0:n0 + nn], in_=ps[0:64, 0:nn]), [ps], [G])
                    else:
                        p.op("pe", lambda e: e.matmul(ps[:, 0:nn], lhsT=Md[:, gl, 128 * c:128 * c + 128], rhs=u_b[:, n0:n0 + nn],
                                                      start=True, stop=True), [Md, u_b], [ps])
                        p.op("act", lambda e: e.copy(out=G[64:128, c, gp, n0:n0 + nn], in_=ps[64:128, 0:nn]), [ps], [G])
        p.op("pool", lambda e: e.memset(Er[:, :, 0:1], 1.0), [], [Er])
        p.op("pool", lambda e: e.memset(Ei[:, :, 0:1], 0.0), [], [Ei])
        for k in range(10):
            ln = 1 << k
            cn = min(ln, NCH - ln)
            if cn <= 0:
                break
            mr = Mrp[k][:, pp0:pp0 + NP].unsqueeze(2).broadcast_to([128, NP, cn])
            mi = Mip[k][:, pp0:pp0 + NP].unsqueeze(2).broadcast_to([128, NP, cn])
            p.op("dve", lambda e: e.tensor_tensor(out=tE[0][:, :, 0:cn], in0=Ei[:, :, 0:cn], in1=mi, op=ALU.mult), [Ei, Mip[k]], [tE[0]])
            p.op("pool", lambda e: e.tensor_tensor(out=tE[1][:, :, 0:cn], in0=Er[:, :, 0:cn], in1=mi, op=ALU.mult), [Er, Mip[k]], [tE[1]])
            p.op("dve", lambda e: e.tensor_tensor(out=Er[:, :, ln:ln + cn], in0=Er[:, :, 0:cn], in1=mr, op=ALU.mult), [Er, Mrp[k]], [Er])
            p.op("pool", lambda e: e.tensor_tensor(out=Ei[:, :, ln:ln + cn], in0=Ei[:, :, 0:cn], in1=mr, op=ALU.mult), [Ei, Mrp[k]], [Ei])
            p.op("dve", lambda e: e.tensor_tensor(out=Er[:, :, ln:ln + cn], in0=Er[:, :, ln:ln + cn], in1=tE[0][:, :, 0:cn], op=ALU.subtract), [Er, tE[0]], [Er])
            p.op("pool", lambda e: e.tensor_tensor(out=Ei[:, :, ln:ln + cn], in0=Ei[:, :, ln:ln + cn], in1=tE[1][:, :, 0:cn], op=ALU.add), [Ei, tE[1]], [Ei])
        for gp in range(NP):
            gm_, gsc = gm[gp % 2], gsn[gp % 2]
            er, ei = Er[:, gp, :], Ei[:, gp, :]
            gre, gim = G[:, 0, gp, :], G[:, 1, gp, :]
            p.op("dve", lambda e: e.tensor_tensor(out=ta[0][:], in0=er, in1=gre, op=ALU.mult), [Er, G], [ta[0]])
            p.op("dve", lambda e: e.tensor_tensor(out=ta[1][:], in0=ei, in1=gim, op=ALU.mult), [Ei, G], [ta[1]])
            p.op("dve", lambda e: e.tensor_tensor(out=gm_[:, 0, :], in0=ta[0][:], in1=ta[1][:], op=ALU.subtract), [ta[0], ta[1]], [gm_])
            p.op("pool", lambda e: e.tensor_tensor(out=ta[2][:], in0=er, in1=gim, op=ALU.mult), [Er, G], [ta[2]])
            p.op("pool", lambda e: e.tensor_tensor(out=ta[3][:], in0=ei, in1=gre, op=ALU.mult), [Ei, G], [ta[3]])
            p.op("pool", lambda e: e.tensor_tensor(out=gm_[:, 1, :], in0=ta[2][:], in1=ta[3][:], op=ALU.add), [ta[2], ta[3]], [gm_])
            rb = ek8p[:, pp0 + gp:pp0 + gp + 1].broadcast_to([128, NCH])
            for c in range(2):
                p.op("dve", lambda e, c=c: e.tensor_tensor_scan(out=gsc[:, c, :], data0=rb, data1=gm_[:, c, :], initial=0.0,
                                                              op0=ALU.mult, op1=ALU.add), [gm_, ek8p], [gsc])
            sl = slice(31, 543)
            p.op("dve", lambda e: e.tensor_tensor(out=ta[0][:, sl], in0=er[:, sl], in1=gsc[:, 0, sl], op=ALU.mult), [Er, gsc], [ta[0]])
            p.op("dve", lambda e: e.tensor_tensor(out=ta[1][:, sl], in0=ei[:, sl], in1=gsc[:, 1, sl], op=ALU.mult), [Ei, gsc], [ta[1]])
            p.op("dve", lambda e: e.tensor_tensor(out=Hb[:, 0, gp, :], in0=ta[0][:, sl], in1=ta[1][:, sl], op=ALU.add), [ta[0], ta[1]], [Hb])
            p.op("pool", lambda e: e.tensor_tensor(out=ta[2][:, sl], in0=er[:, sl], in1=gsc[:, 1, sl], op=ALU.mult), [Er, gsc], [ta[2]])
            p.op("pool", lambda e: e.tensor_tensor(out=ta[3][:, sl], in0=ei[:, sl], in1=gsc[:, 0, sl], op=ALU.mult), [Ei, gsc], [ta[3]])
            p.op("pool", lambda e: e.tensor_tensor(out=Hb[:, 1, gp, :], in0=ta[2][:, sl], in1=ta[3][:, sl], op=ALU.subtract), [ta[2], ta[3]], [Hb])
        for gl in range(NG):
            g = g0 + gl
            gp = gl // 2
            ps = pg[pgi[0] % 4]
            pgi[0] += 1
            p.op("pe", lambda e: e.matmul(ps[:, :], lhsT=Mi[:, gl, :], rhs=ub[gl][:, 32:NCH], start=True, stop=False), [Mi, ub[gl]], [ps])
            p.op("pe", lambda e: e.matmul(ps[:, :], lhsT=MoR[:, gl, :], rhs=Hb[:, 0, gp, :], start=False, stop=False), [MoR, Hb], [ps])
            p.op("pe", lambda e: e.matmul(ps[:, :], lhsT=MoI[:, gl, :], rhs=Hb[:, 1, gp, :], start=False, stop=True), [MoI, Hb], [ps])
            y_ = yo[gl % 2]
            p.op("act", lambda e: e.copy(out=y_[:], in_=ps[:, :]), [ps], [y_])
            p.dma("pool", Y[g], y_[:], [y_], [Y])
    return p.finish()


def l6_inputs(useq, d, od_a_re, od_a_im, od_log_step, od_b_re, od_b_im, od_c_re, od_c_im, od_d, with_skip):
    U = np.ascontiguousarray(f32(useq).reshape(NCH, 8, 64, 16).transpose(2, 1, 3, 0).reshape(64, 128, NCH))
    dup = lambda a: np.concatenate([a, a], axis=0)
    ar = dup(f32(od_a_re)[d].T)
    ai = dup(f32(od_a_im)[d].T)
    ls = np.broadcast_to(f32(od_log_step)[d][None, :], (128, 64))
    prm = np.ascontiguousarray(np.stack([ar, ai, ls]))
    br = dup(f32(od_b_re)[d].transpose(1, 0, 2))
    bi = dup(f32(od_b_im)[d].transpose(1, 0, 2))
    cr = dup(f32(od_c_re)[d].transpose(2, 0, 1))
    ci = dup(f32(od_c_im)[d].transpose(2, 0, 1))
    sel = np.zeros((128, 2), np.float32)
    sel[:64, 0] = 1.0
    sel[64:, 1] = 1.0
    dq = np.zeros((128, 64), np.float32)
    if with_skip:
        dq[:] = np.tile(f32(od_d).reshape(64, 16).T, (8, 1))
    mk = np.zeros((128, 64), np.float32)
    mk[:64, 0::2] = 1.0
    mk[64:, 1::2] = 1.0
    return {"mk": mk, "U": U, "prm": prm, "bri": np.ascontiguousarray(np.stack([br, bi])), "cri": np.ascontiguousarray(np.stack([cr, ci])),
            "sel": sel, "dq": dq, "identf": np.eye(128, dtype=np.float32), "ident": np.eye(128, dtype=np.float32).astype(NPBF)}


def l6_unpack(Y):
    return np.ascontiguousarray(np.asarray(Y).reshape(64, 8, 16, 512).transpose(3, 1, 0, 2).reshape(4096, 1024))


T7 = 2048


def build_l7():
    p = Prog()
    nc = p.nc
    x = p.dram("x", [T7, D], F32, "ExternalInput")
    yf = p.dram("yf", [T7, D], F32, "ExternalInput")
    yr = p.dram("yr", [T7, D], F32, "ExternalInput")
    wglu = p.dram("wglu", [D, 2048], F32, "ExternalInput")
    g1_d = p.dram("g1bc", [128, D], F32, "ExternalInput")
    g2_d = p.dram("g2bc", [128, D], F32, "ExternalInput")
    fg_d = p.dram("fgbc", [128, D], F32, "ExternalInput")
    gsT_d = p.dram("gsT", [128, 8], F32, "ExternalInput")
    shT_d = p.dram("shT", [128, 8], F32, "ExternalInput")
    wr_d = p.dram("wr", [128, 8, 8], F32, "ExternalInput")
    wge = p.dram("wge", [8, 28, 128, 8, 128], F32, "ExternalInput")
    wue = p.dram("wue", [8, 28, 128, 8, 128], F32, "ExternalInput")
    wde = p.dram("wde", [8, 3584, D], F32, "ExternalInput")
    ident_d = p.dram("ident", [128, 128], BF16, "ExternalInput")
    identf_d = p.dram("identf", [128, 128], F32, "ExternalInput")
    xm = p.dram("xm", [T7, D], F32, "Internal")
    out = p.dram("out", [T7, D], F32, "ExternalOutput")
    xmb = [Buf("xm%d" % i) for i in range(4)]
    pt = [p.ps("pt%d" % i, [128, 1024], BF16) for i in range(2)]
    ptf = p.ps("ptf", [128, 1024], F32)
    pp = [p.ps("pp%d" % i, [128, 512], F32) for i in range(4)]
    ppi = [0]

    def banks():
        ppi[0] += 1
        return pp[ppi[0] % 4]

    outer = p.es
    p.es = ExitStack()
    ident = p.sb("ident", [128, 128], BF16)
    p.dma("sp", ident[:], ident_d[:], [ident_d], [ident])
    g1 = p.sb("g1", [128, D], F32)
    p.dma("sp", g1[:], g1_d[:], [g1_d], [g1])
    stg = Stage(p, 2048)
    WG = load_w(p, stg, "WGLU", wglu, 8, 2048)
    ya = [p.sb("ya%d" % i, [128, D], F32) for i in range(2)]
    yb = [p.sb("yb%d" % i, [128, D], F32) for i in range(2)]
    y2s = [p.sb("y2_%d" % i, [128, D], F32) for i in range(2)]
    gls = [p.sb("gl_%d" % i, [128, D], BF16) for i in range(2)]
    glTs = [p.sb("glT_%d" % i, [128, 8, 128], BF16) for i in range(2)]
    xss = [p.sb("xsA_%d" % i, [128, D], F32) for i in range(2)]
    sgms = [p.sb("sgm_%d" % i, [128, 512], F32) for i in range(2)]
    ots = [p.sb("ot_%d" % i, [128, 512], F32) for i in range(2)]
    for ti in range(T7 // 128):
        a, b_ = ya[ti % 2], yb[ti % 2]
        y2, gl, glT, xs = y2s[ti % 2], gls[ti % 2], glTs[ti % 2], xss[ti % 2]
        rs = slice(ti * 128, (ti + 1) * 128)
        p.dma("sp", a[:], yf[rs, :], [yf], [a])
        p.dma("sp", b_[:], yr[rs, :], [yr], [b_])
        p.dma("sp", xs[:], x[rs, :], [x], [xs])
        p.op("pool", lambda e: e.tensor_tensor(out=a[:], in0=a[:], in1=b_[:], op=ALU.add), [a, b_], [a])
        p.op("pool", lambda e: e.tensor_tensor(out=y2[:], in0=a[:], in1=a[:], op=ALU.mult), [a], [y2])
        p.op("dve", lambda e: e.tensor_scalar(out=y2[:], in0=y2[:], scalar1=0.044715, scalar2=1.0, op0=ALU.mult, op1=ALU.add), [y2], [y2])
        p.op("dve", lambda e: e.tensor_tensor(out=y2[:], in0=y2[:], in1=a[:], op=ALU.mult), [y2, a], [y2])
        p.op("act", lambda e: e.activation(out=y2[:], in_=y2[:], func=AF.Tanh, scale=0.7978845608028654), [y2], [y2])
        p.op("dve", lambda e: e.scalar_tensor_tensor(out=gl[:], in0=y2[:], scalar=1.0, in1=a[:], op0=ALU.add, op1=ALU.mult), [y2, a], [gl])
        ptt = pt[ti % 2]
        for c in range(8):
            p.op("pe", lambda e, c=c: e.transpose(out=ptt[:, c * 128:(c + 1) * 128], in_=gl[:, c * 128:(c + 1) * 128], identity=ident[:]), [gl, ident], [ptt])
        p.op("act", lambda e: e.copy(out=glT[:, 0:4, :], in_=ptt[:, 0:512].rearrange("p (c t) -> p c t", c=4)), [ptt], [glT])
        p.op("dve", lambda e: e.tensor_copy(out=glT[:, 4:8, :], in_=ptt[:, 512:1024].rearrange("p (c t) -> p c t", c=4)), [ptt], [glT])
        for cb in range(2):
            sgm, ot = sgms[cb], ots[cb]
            pa, pb = banks(), banks()
            for (ps, off) in ((pa, cb * 512), (pb, 1024 + cb * 512)):
                for c in range(8):
                    p.op("pe", lambda e, c=c, ps=ps, off=off: e.matmul(ps[:], lhsT=glT[:, c, :], rhs=WG[:, c, off:off + 512],
                                                                      start=(c == 0), stop=(c == 7)), [glT, WG], [ps])
            p.op("act", lambda e: e.activation(out=sgm[:], in_=pb[:], func=AF.Sigmoid, scale=0.5), [pb], [sgm])
            p.op("dve", lambda e: e.scalar_tensor_tensor(out=ot[:], in0=pa[:], scalar=0.5, in1=sgm[:], op0=ALU.mult, op1=ALU.mult), [pa, sgm], [ot])
            p.op("pool", lambda e: e.tensor_tensor(out=ot[:], in0=ot[:], in1=g1[:, cb * 512:(cb + 1) * 512], op=ALU.mult), [ot, g1], [ot])
            p.op("pool", lambda e: e.tensor_tensor(out=xs[:, cb * 512:(cb + 1) * 512], in0=xs[:, cb * 512:(cb + 1) * 512], in1=ot[:], op=ALU.add), [xs, ot], [xs])
        p.dma("pool", xm[rs, :], xs[:], [xs], [xmb[ti // 4]])
    p.barrier()
    p.es.close()
    p.es = ExitStack()
    identf = p.sb("identf", [128, 128], F32)
    p.dma("sp", identf[:], identf_d[:], [identf_d], [identf])
    g2 = p.sb("g2", [128, D], F32)
    fg = p.sb("fg", [128, D], F32)
    gsT = p.sb("gsT", [128, 8], F32)
    shT = p.sb("shT", [128, 8], F32)
    wr = p.sb("wr", [128, 8, 8], F32)
    for a, b_ in ((g2, g2_d), (fg, fg_d), (gsT, gsT_d), (shT, shT_d), (wr, wr_d)):
        p.dma("sp", a[:], b_[:], [b_], [a])
    eps = mk_eps(p)
    stg = Stage(p, 512, n=3, name="stgB")
    pan = Panels(p)
    NTB = 8
    actT = p.sb("actT", [128, 14, 128 * NTB], BF16)
    WDh = [p.sb("WDh%d" % i, [128, 14, 512], BF16) for i in range(2)]
    hT = p.sb("hTB", [128, 8, 128 * NTB], BF16)
    hT32 = p.sb("hT32", [128, 8, 128], F32)
    xs = p.sb("xsB", [128, NTB, D], F32)
    xn = p.sb("xnB", [128, D], F32)
    junk = p.sb("junkB", [128, D], BF16)
    ss = p.sb("ssB", [128, 4], F32)
    lg = p.sb("lg", [128, 8], F32)
    mx = p.sb("mx", [128, 8], F32)
    e1 = p.sb("e1", [128, 8], F32)
    e2 = p.sb("e2", [128, 8], F32)
    w12 = p.sb("w12", [128, 4], F32)
    comb = p.sb("comb", [128, NTB, 8], F32)
    sg = [p.sb("sgB%d" % i, [128, 512], F32) for i in range(2)]
    tm = [p.sb("tmB%d" % i, [128, 512], F32) for i in range(2)]
    ob = [p.sb("ob%d" % i, [128, D], F32) for i in range(1)]
    tmi = [0]
    wdi = [0]
    for blk in range(T7 // (128 * NTB)):
        c0 = blk * 128 * NTB
        for q4 in range(NTB // 4):
            p.dma("sp", xs[:, q4 * 4:(q4 + 1) * 4, :], xm[c0 + q4 * 512:c0 + (q4 + 1) * 512, :].rearrange("(t p) f -> p t f", p=128),
                  [xmb[(c0 + q4 * 512) // 512]], [xs])
        for ti in range(NTB):
            xt = xs[:, ti, :]
            p.op("act", lambda e: e.activation(out=junk[:], in_=xt, func=AF.Square, accum_out=ss[:, 0:1]), [xs], [junk, ss])
            p.op("act", lambda e: e.activation(out=ss[:, 1:2], in_=ss[:, 0:1], func=AF.Sqrt, scale=1.0 / D, bias=eps[:, 0:1]), [ss, eps], [ss])
            p.op("dve", lambda e: e.reciprocal(out=ss[:, 2:3], in_=ss[:, 1:2]), [ss], [ss])
            p.op("dve", lambda e: e.tensor_scalar(out=xn[:], in0=xt, scalar1=ss[:, 2:3], scalar2=None, op0=ALU.mult), [xs, ss], [xn])
            for c in range(8):
                p.op("pe", lambda e, c=c: e.transpose(out=ptf[:, c * 128:(c + 1) * 128], in_=xn[:, c * 128:(c + 1) * 128], identity=identf[:]), [xn, identf], [ptf])
            for c in range(8):
                p.op("dve", lambda e, c=c: e.tensor_scalar(out=hT32[:, c, :], in0=ptf[:, c * 128:(c + 1) * 128], scalar1=gsT[:, c:c + 1],
                                                           scalar2=shT[:, c:c + 1], op0=ALU.mult, op1=ALU.add), [ptf, gsT, shT], [hT32])
            p.op("pool", lambda e: e.tensor_copy(out=hT[:, :, ti * 128:(ti + 1) * 128], in_=hT32[:]), [hT32], [hT])
            ps = banks()
            for c in range(8):
                p.op("pe", lambda e, c=c: e.matmul(ps[:, 0:8], lhsT=hT32[:, c, :], rhs=wr[:, c, :], start=(c == 0), stop=(c == 7)), [hT32, wr], [ps])
            p.op("dve", lambda e: e.tensor_copy(out=lg[:], in_=ps[:, 0:8]), [ps], [lg])
            p.op("dve", lambda e: e.max(out=mx[:], in_=lg[:]), [lg], [mx])
            p.op("dve", lambda e: e.tensor_tensor(out=w12[:, 0:1], in0=mx[:, 0:1], in1=mx[:, 1:2], op=ALU.subtract), [mx], [w12])
            p.op("act", lambda e: e.activation(out=w12[:, 1:2], in_=w12[:, 0:1], func=AF.Sigmoid), [w12], [w12])
            p.op("dve", lambda e: e.tensor_scalar(out=w12[:, 2:3], in0=w12[:, 1:2], scalar1=-1.0, scalar2=1.0, op0=ALU.mult, op1=ALU.add), [w12], [w12])
            p.op("dve", lambda e: e.tensor_scalar(out=e1[:], in0=lg[:], scalar1=mx[:, 0:1], scalar2=w12[:, 1:2], op0=ALU.is_equal, op1=ALU.mult), [lg, mx, w12], [e1])
            p.op("dve", lambda e: e.tensor_scalar(out=e2[:], in0=lg[:], scalar1=mx[:, 1:2], scalar2=w12[:, 2:3], op0=ALU.is_equal, op1=ALU.mult), [lg, mx, w12], [e2])
            p.op("dve", lambda e: e.tensor_tensor(out=comb[:, ti, :], in0=e1[:], in1=e2[:], op=ALU.add), [e1, e2], [comb])
        for ex in range(8):
            for fh in range(2):
                wd0, wd1 = WDh[0], WDh[1]

                def ldwd(f, wd_, cb):
                    r0 = (fh * 14 + f) * 128
                    stg.load(wd_[:, f, :], wd_.b, wde[ex, r0:r0 + 128, cb * 512:(cb + 1) * 512], wde.b, 128, 512)

                ffn_phase1(p, pan, banks, hT, 128 * NTB, Tile(wge[ex], wge.b), Tile(wue[ex], wue.b), 14, actT, sg, f0=fh * 14,
                           between=lambda f: ldwd(f, wd0, 0))
                for cb in range(2):
                    wd_ = WDh[cb]
                    if cb == 1:
                        for f in range(14):
                            ldwd(f, wd1, 1)
                    for ti in range(NTB):
                        ps = banks()
                        for f in range(14):
                            p.op("pe", lambda e, f=f, ps=ps: e.matmul(ps[:], lhsT=actT[:, f, ti * 128:(ti + 1) * 128], rhs=wd_[:, f, :],
                                                                     start=(f == 0), stop=(f == 13)), [actT, wd_], [ps])
                        t = tm[tmi[0] % 2]
                        tmi[0] += 1
                        p.op("dve", lambda e: e.scalar_tensor_tensor(out=t[:], in0=ps[:], scalar=comb[:, ti, ex:ex + 1], in1=g2[:, cb * 512:(cb + 1) * 512],
                                                                     op0=ALU.mult, op1=ALU.mult), [ps, comb, g2], [t])
                        p.op("pool", lambda e: e.tensor_tensor(out=xs[:, ti, cb * 512:(cb + 1) * 512], in0=xs[:, ti, cb * 512:(cb + 1) * 512],
                                                               in1=t[:], op=ALU.add), [xs, t], [xs])
        for ti in range(NTB):
            xt = xs[:, ti, :]
            o_ = ob[ti % len(ob)]
            p.op("act", lambda e: e.activation(out=junk[:], in_=xt, func=AF.Square, accum_out=ss[:, 0:1]), [xs], [junk, ss])
            p.op("act", lambda e: e.activation(out=ss[:, 1:2], in_=ss[:, 0:1], func=AF.Sqrt, scale=1.0 / D, bias=eps[:, 0:1]), [ss, eps], [ss])
            p.op("dve", lambda e: e.reciprocal(out=ss[:, 2:3], in_=ss[:, 1:2]), [ss], [ss])
            p.op("dve", lambda e: e.scalar_tensor_tensor(out=o_[:], in0=xt, scalar=ss[:, 2:3], in1=fg[:], op0=ALU.mult, op1=ALU.mult), [xs, ss, fg], [o_])
            p.dma("pool", out[c0 + ti * 128:c0 + (ti + 1) * 128, :], o_[:], [o_], [out])
    p.es.close()
    p.es = outer
    return p.finish()


def kernel(x, c, ctx, c_ctx, mod_w, mod_b, norm1_g, norm2_g,
           ev_w_in, ev_q_norm_g, ev_w_qb, ev_kv_norm_g, ev_w_kvb, ev_na_rpb, ev_w_out,
           ev_ffn_w_gate, ev_ffn_w_up, ev_ffn_w_down,
           od_w_in, od_a_re, od_a_im, od_log_step, od_b_re, od_b_im, od_c_re, od_c_im, od_d, od_w_glu,
           moe_w_router, moe_w_gate, moe_w_up, moe_w_down, final_g):
    x = f32(x)
    ctx = f32(ctx)
    m, gs = run_adaln(c, c_ctx, mod_w, mod_b, norm1_g, norm2_g)
    identb = np.eye(128, dtype=np.float32).astype(NPBF)
    identf = np.eye(128, dtype=np.float32)
    cores = [(i // 2, i % 2) for i in range(NCORES)]

    def mods(layer, b, lo_g, lo_s):
        return (np.ascontiguousarray(np.stack([fm(gs[layer, b, lo_g:lo_g + D], 8), fm(gs[layer, 4, lo_g:lo_g + D], 8)], axis=2)),
                np.ascontiguousarray(np.stack([fm(m[layer, b, lo_s:lo_s + D], 8), fm(m[layer, 4, lo_s:lo_s + D], 8)], axis=2)))

    in2 = []
    for (b, hf) in cores:
        xtok = np.concatenate([x[b, hf * 2048:(hf + 1) * 2048], ctx[b, hf * 128:(hf + 1) * 128]], 0)
        pos = np.concatenate([np.arange(hf * 2048, (hf + 1) * 2048), -np.ones(128, np.int64)])
        in2.append(l2_inputs(xtok, pos, gs[0, b, 1024:2048], m[0, b, 0:1024], gs[0, 4, 1024:2048], m[0, 4, 0:1024],
                             ev_w_in[0], ev_q_norm_g[0], ev_w_qb[0], ev_kv_norm_g[0], ev_w_kvb[0]))
    res2 = run_prog(build_l2(), in2)
    res2 = [{k: np.asarray(v) for k, v in r.items()} for r in res2]
    in3 = [l3_inputs(b, hf, res2, ev_na_rpb[0]) for (b, hf) in cores]
    res3 = run_prog(build_l3(), in3)
    in4 = []
    for i, (b, hf) in enumerate(cores):
        gsT, shT = mods(0, b, 4096, 3072)
        in4.append({"x": in2[i]["x"], "aT": np.ascontiguousarray(np.asarray(res3[i]["AO"]).T), "wo": f32(ev_w_out[0]),
                    "wg": pretile(ev_ffn_w_gate[0]), "wu": pretile(ev_ffn_w_up[0]), "wd": f32(ev_ffn_w_down[0]),
                    "g1bc": np.stack([bc128(m[0, b, 2048:3072]), bc128(m[0, 4, 2048:3072])]),
                    "g2bc": np.stack([bc128(m[0, b, 5120:6144]), bc128(m[0, 4, 5120:6144])]),
                    "gsT": gsT, "shT": shT, "ident": identb})
    res4 = run_prog(build_l4(), in4)
    xo = [np.asarray(r["xo"]) for r in res4]
    in5 = []
    for i, (b, hf) in enumerate(cores):
        gsT, shT = mods(1, b, 1024, 0)
        in5.append({"x": xo[i], "wi": f32(od_w_in[0]), "gsT": gsT, "shT": shT, "ident": identb})
    res5 = run_prog(build_l5(), in5)
    u = [np.asarray(r["u"]) for r in res5]
    in6 = []
    for i in range(NCORES):
        b, dr = i // 2, i % 2
        u0, u1 = u[2 * b], u[2 * b + 1]
        lat = np.concatenate([u0[:2048], u1[:2048]], 0)
        cx = np.concatenate([u0[2048:], u1[2048:]], 0)
        seq = np.concatenate([cx, lat], 0) if dr == 0 else np.concatenate([cx[::-1], lat[::-1]], 0)
        in6.append(l6_inputs(seq, dr, od_a_re[0], od_a_im[0], od_log_step[0], od_b_re[0], od_b_im[0],
                             od_c_re[0], od_c_im[0], od_d[0], dr == 0))
    res6 = run_prog(build_l6(), in6)
    Y = [l6_unpack(r["Y"]) for r in res6]
    in7 = []
    wr = np.ascontiguousarray(f32(moe_w_router[0]).reshape(8, 128, 8).transpose(1, 0, 2))
    wge_t = np.stack([pretile(moe_w_gate[0][e]) for e in range(8)])
    wue_t = np.stack([pretile(moe_w_up[0][e]) for e in range(8)])
    for i, (b, hf) in enumerate(cores):
        yf = Y[2 * b][hf * 2048:(hf + 1) * 2048]
        yr = Y[2 * b + 1][::-1][hf * 2048:(hf + 1) * 2048]
        in7.append({"x": np.ascontiguousarray(xo[i][:2048]), "yf": np.ascontiguousarray(yf), "yr": np.ascontiguousarray(yr),
                    "wglu": f32(od_w_glu[0]), "g1bc": bc128(m[1, b, 2048:3072]), "g2bc": bc128(m[1, b, 5120:6144]),
                    "fgbc": bc128(final_g), "gsT": fm(gs[1, b, 4096:5120], 8), "shT": fm(m[1, b, 3072:4096], 8),
                    "wr": wr, "wge": wge_t, "wue": wue_t, "wde": f32(moe_w_down[0]),
                    "ident": identb, "identf": identf})
    res7 = run_prog(build_l7(), in7)
    out = np.zeros((B, L, D), np.float32)
    for i, (b, hf) in enumerate(cores):
        out[b, hf * 2048:(hf + 1) * 2048] = np.asarray(res7[i]["out"])
    return out
```
